# Optimizing a Trainium2 kernel written in Bass

```python
import numpy as np
import jax
import jax.numpy as jnp
from jax import lax

D_MODEL = 4096
BATCH = 2
SEQ = 4096
DEPTH = 2

GRID_W = 64
CTX_LEN = 256
CHUNK = 64
EPS = 1e-6
ROPE_BASE = 10000.0
GLA_HEADS = 8
GLA_DK = 64
GLA_DV = 128
GLA_RANK = 16
GLA_TAU = 16.0
ML_HEADS = 8
ML_DH = 128
ML_CONV = 3
NA_HEADS = 16
NA_DH = 128
NA_KH = 8
NA_KW = 16
N_BRANCH = 3

A_QK = GLA_HEADS * GLA_DK
A_V = GLA_HEADS * GLA_DV
B_W = ML_HEADS * ML_DH
C_W = NA_HEADS * NA_DH
IN_SPLITS = (A_QK, A_QK, A_V, A_V, 2 * GLA_RANK, B_W, B_W, B_W, B_W, B_W, 4 * ML_HEADS, C_W, C_W, C_W, C_W)
D_IN = 2 * A_QK + 2 * A_V + 2 * GLA_RANK + 5 * B_W + 4 * ML_HEADS + 4 * C_W

kernel_name = 'hybrid_gla_mlstm_natten_dit'


def rmsnorm(x, g):
    xf = x.astype(jnp.float32)
    xf = xf * lax.rsqrt(jnp.mean(xf * xf, axis=-1, keepdims=True) + EPS)
    return xf.astype(x.dtype) * g


def split_cols(p):
    return jnp.split(p, np.cumsum(IN_SPLITS)[:-1].tolist(), axis=-1)


def flip_t(t):
    return jnp.flip(t, axis=1)


def to_chunks(t):
    b, l = t.shape[:2]
    t = t.reshape(b, l // CHUNK, CHUNK, *t.shape[2:])
    return jnp.moveaxis(t, 1, 0)


def from_chunks(t):
    t = jnp.moveaxis(t, 0, 1)
    return t.reshape(t.shape[0], t.shape[1] * t.shape[2], *t.shape[3:])


def rope_1d(x, pos):
    nf = x.shape[-1] // 2
    inv = ROPE_BASE ** (-jnp.arange(nf, dtype=jnp.float32) / nf)
    ang = pos.astype(jnp.float32)[:, None] * inv[None, :]
    cos = jnp.cos(ang)[None, :, None, :]
    sin = jnp.sin(ang)[None, :, None, :]
    xf = x.astype(jnp.float32)
    x1, x2 = xf[..., :nf], xf[..., nf:]
    return jnp.concatenate([x1 * cos - x2 * sin, x1 * sin + x2 * cos], axis=-1).astype(x.dtype)


def axial_rope(x):
    t = jnp.arange(x.shape[1])
    half = x.shape[-1] // 2
    return jnp.concatenate([rope_1d(x[..., :half], t // GRID_W), rope_1d(x[..., half:], t % GRID_W)], axis=-1)


def gla_scan(q, k, v, log_a, s0):
    dt = v.dtype
    mask = jnp.tril(jnp.ones((CHUNK, CHUNK), dtype=bool))[None, :, :, None, None]

    def step(S, inp):
        qc, kc, vc, ac = inp
        b = jnp.cumsum(ac, axis=1)
        diff = jnp.where(mask, b[:, :, None] - b[:, None, :], -jnp.inf)
        att = jnp.einsum('bthk,bshk,btshk->bhts', qc, kc, jnp.exp(diff))
        o = jnp.einsum('bhts,bshv->bthv', att, vc) + jnp.einsum('bthk,bhkv->bthv', qc * jnp.exp(b), S)
        b_last = b[:, -1]
        S = jnp.exp(b_last)[..., None] * S + jnp.einsum('bshk,bshv->bhkv', kc * jnp.exp(b_last[:, None] - b), vc)
        return S, o

    xs = tuple(to_chunks(t.astype(jnp.float32)) for t in (q, k, v, log_a))
    S, o = lax.scan(step, s0, xs)
    return from_chunks(o).astype(dt), S


def mlstm_scan(q, k, v, i_pre, log_f, state):
    dt = v.dtype
    mask = jnp.tril(jnp.ones((CHUNK, CHUNK), dtype=bool))[None, :, :, None]

    def step(carry, inp):
        Cm, nm, mm = carry
        qc, kc, vc, ic, fc = inp
        b = jnp.cumsum(fc, axis=1)
        d_log = jnp.where(mask, b[:, :, None] - b[:, None, :] + ic[:, None, :], -jnp.inf)
        inter_log = b + mm[:, None]
        m = jnp.maximum(inter_log, jnp.max(d_log, axis=2))
        w_inter = jnp.exp(inter_log - m)
        qk = jnp.einsum('bthd,bshd->btsh', qc, kc) * jnp.exp(d_log - m[:, :, None])
        num = jnp.einsum('btsh,bshv->bthv', qk, vc) + w_inter[..., None] * jnp.einsum('bthd,bhdv->bthv', qc, Cm)
        den = jnp.sum(qk, axis=2) + w_inter * jnp.einsum('bthd,bhd->bth', qc, nm)
        h = num / jnp.maximum(jnp.abs(den), jnp.exp(-m))[..., None]
        b_last = b[:, -1]
        log_w = b_last[:, None] - b + ic
        m_new = jnp.maximum(b_last + mm, jnp.max(log_w, axis=1))
        decay = jnp.exp(b_last + mm - m_new)
        w = jnp.exp(log_w - m_new[:, None])
        Cm = decay[..., None, None] * Cm + jnp.einsum('bsh,bshd,bshv->bhdv', w, kc, vc)
        nm = decay[..., None] * nm + jnp.einsum('bsh,bshd->bhd', w, kc)
        return (Cm, nm, m_new), h

    xs = tuple(to_chunks(t.astype(jnp.float32)) for t in (q, k, v, i_pre, log_f))
    st, h = lax.scan(step, state, xs)
    return from_chunks(h).astype(dt), st


def bidirectional(scan_fn, lat_f, lat_b, ctx_f, ctx_b, init):
    yc_f, st_f = scan_fn(*ctx_f, init)
    yc_b, st_b = scan_fn(*map(flip_t, ctx_b), init)
    y_f, _ = scan_fn(*lat_f, st_f)
    y_b, _ = scan_fn(*map(flip_t, lat_b), st_b)
    return y_f + flip_t(y_b), yc_f + flip_t(yc_b)


def centred_dwconv(x, w, bias):
    pad = (ML_CONV - 1) // 2
    L = x.shape[1]
    xp = jnp.pad(x, ((0, 0), (pad, pad), (0, 0)))
    return sum(xp[:, j:j + L] * w[j] for j in range(ML_CONV)) + bias


def neighbourhood_attention(q, k, v, k_ctx, v_ctx, rpb):
    B_, L, H, d = q.shape
    rows = L // GRID_W
    kh, kw = min(NA_KH, rows), NA_KW
    qg = q.reshape(B_, rows, GRID_W, H, d)
    kg = k.reshape(B_, rows, GRID_W, H, d)
    vg = v.reshape(B_, rows, GRID_W, H, d)
    cols = jnp.arange(GRID_W)
    col_idx = jnp.clip(cols - kw // 2, 0, GRID_W - kw)[:, None] + jnp.arange(kw)[None, :]
    dc = col_idx - cols[:, None]
    scale = d ** -0.5

    def row_block(r):
        rs = jnp.clip(r - kh // 2, 0, rows - kh)
        qr = lax.dynamic_index_in_dim(qg, r, axis=1, keepdims=False)
        k_win = lax.dynamic_slice_in_dim(kg, rs, kh, axis=1)[:, :, col_idx]
        v_win = lax.dynamic_slice_in_dim(vg, rs, kh, axis=1)[:, :, col_idx]
        dr = rs + jnp.arange(kh) - r
        bias = rpb[:, dr[:, None, None] + NA_KH - 1, dc[None] + NA_KW - 1]
        s_win = jnp.einsum('bwhd,bawkhd->bhwak', qr, k_win) * scale + jnp.transpose(bias, (0, 2, 1, 3))[None]
        s_win = s_win.reshape(B_, H, GRID_W, kh * kw)
        s_ctx = jnp.einsum('bwhd,bchd->bhwc', qr, k_ctx) * scale
        p = jax.nn.softmax(jnp.concatenate([s_win, s_ctx], axis=-1).astype(jnp.float32), axis=-1).astype(v.dtype)
        p_win = p[..., :kh * kw].reshape(B_, H, GRID_W, kh, kw)
        p_ctx = p[..., kh * kw:]
        return jnp.einsum('bhwak,bawkhd->bwhd', p_win, v_win) + jnp.einsum('bhwc,bchd->bwhd', p_ctx, v_ctx)

    out = lax.map(row_block, jnp.arange(rows))
    return jnp.moveaxis(out, 0, 1).reshape(B_, L, H * d)


def ctx_attention(q, k, v):
    s = jnp.einsum('bqhd,bkhd->bhqk', q, k) * (q.shape[-1] ** -0.5)
    p = jax.nn.softmax(s.astype(jnp.float32), axis=-1).astype(v.dtype)
    return jnp.einsum('bhqk,bkhd->bqhd', p, v).reshape(q.shape[0], q.shape[1], -1)


def hybrid_mixer(h, hc, w_in, w_alpha2, b_alpha, gla_norm_g, b_gates, conv_w, conv_b, rpb,
                 w_merge, b_merge, w_proj_a, w_proj_b, w_proj_c, w_out, ctx_out):
    B_, L, _ = h.shape
    Lc = hc.shape[1]
    lat = split_cols(h @ w_in)
    ctx = split_cols(hc @ w_in)

    def gla_inputs(p, n, rope):
        qa, ka, va, za, aa = p[0:5]
        q = qa.reshape(B_, n, GLA_HEADS, GLA_DK)
        k = ka.reshape(B_, n, GLA_HEADS, GLA_DK)
        if rope:
            q, k = axial_rope(q), axial_rope(k)
        q = q * GLA_DK ** -0.5
        v = va.reshape(B_, n, GLA_HEADS, GLA_DV)
        la = [(jax.nn.log_sigmoid((aa[..., r * GLA_RANK:(r + 1) * GLA_RANK] @ w_alpha2[r] + b_alpha[r]).astype(jnp.float32)) / GLA_TAU).reshape(B_, n, GLA_HEADS, GLA_DK) for r in range(2)]
        return (q, k, v, la[0]), (q, k, v, la[1]), za

    a_f, a_b, za = gla_inputs(lat, L, True)
    ac_f, ac_b, za_c = gla_inputs(ctx, Lc, False)
    s0 = jnp.zeros((B_, GLA_HEADS, GLA_DK, GLA_DV), jnp.float32)
    oa, oa_c = bidirectional(gla_scan, a_f, a_b, ac_f, ac_b, s0)

    def ml_inputs(p, n):
        qb, kb, vb, zb, ob, gb = p[5:11]
        q = jax.nn.silu(centred_dwconv(qb, conv_w[:, :B_W], conv_b[:B_W])).reshape(B_, n, ML_HEADS, ML_DH)
        k = jax.nn.silu(centred_dwconv(kb, conv_w[:, B_W:], conv_b[B_W:])).reshape(B_, n, ML_HEADS, ML_DH) * ML_DH ** -0.5
        v = vb.reshape(B_, n, ML_HEADS, ML_DH)
        g = (gb.reshape(B_, n, 4, ML_HEADS) + b_gates).astype(jnp.float32)
        fwd = (q, k, v, g[:, :, 0], jax.nn.log_sigmoid(g[:, :, 1]))
        bwd = (q, k, v, g[:, :, 2], jax.nn.log_sigmoid(g[:, :, 3]))
        return fwd, bwd, zb, ob

    b_f, b_b, zb, ob = ml_inputs(lat, L)
    bc_f, bc_b, zb_c, ob_c = ml_inputs(ctx, Lc)
    st0 = (jnp.zeros((B_, ML_HEADS, ML_DH, ML_DH), jnp.float32),
           jnp.zeros((B_, ML_HEADS, ML_DH), jnp.float32),
           jnp.zeros((B_, ML_HEADS), jnp.float32))
    hb, hb_c = bidirectional(mlstm_scan, b_f, b_b, bc_f, bc_b, st0)

    heads = lambda t, n: t.reshape(B_, n, NA_HEADS, NA_DH)
    qn, kn, vn, zn = lat[11:15]
    qn_c, kn_c, vn_c, zn_c = ctx[11:15]
    k_ctx, v_ctx = heads(kn_c, Lc), heads(vn_c, Lc)
    on = neighbourhood_attention(heads(qn, L), heads(kn, L), heads(vn, L), k_ctx, v_ctx, rpb)

    def merge(hh, ya, yb, yc):
        ga, gb_, gc = jnp.split(jax.nn.sigmoid(hh @ w_merge + b_merge), N_BRANCH, axis=-1)
        return (ga * (ya @ w_proj_a) + gb_ * (yb @ w_proj_b) + gc * (yc @ w_proj_c)) @ w_out

    g_norm = gla_norm_g.reshape(GLA_HEADS, GLA_DV)
    ya = rmsnorm(oa, g_norm).reshape(B_, L, A_V) * jax.nn.silu(za)
    yb = jax.nn.sigmoid(ob) * hb.reshape(B_, L, B_W) * jax.nn.silu(zb)
    yc = on * jax.nn.silu(zn)
    out = merge(h, ya, yb, yc)
    if not ctx_out:
        return out, None
    ya_c = rmsnorm(oa_c, g_norm).reshape(B_, Lc, A_V) * jax.nn.silu(za_c)
    yb_c = jax.nn.sigmoid(ob_c) * hb_c.reshape(B_, Lc, B_W) * jax.nn.silu(zb_c)
    yc_c = ctx_attention(heads(qn_c, Lc), k_ctx, v_ctx) * jax.nn.silu(zn_c)
    return out, merge(hc, ya_c, yb_c, yc_c)


def setup_inputs(seed: int = 0) -> dict:
    key = jax.random.key(seed)
    ks = jax.random.split(key, 24)
    f32 = jnp.float32
    nrm = lambda k, shape, s: jax.random.normal(k, shape, f32) * s
    f_bias = jnp.linspace(3.0, 6.0, ML_HEADS, dtype=f32)
    gate_sel = jnp.array([0.0, 1.0, 0.0, 1.0], f32)[:, None]
    return {
        'x': nrm(ks[0], (BATCH, SEQ, D_MODEL), 1.0),
        'c': nrm(ks[1], (BATCH, D_MODEL), 1.0),
        'ctx': nrm(ks[2], (BATCH, CTX_LEN, D_MODEL), 1.0),
        'c_ctx': nrm(ks[3], (D_MODEL,), 1.0),
        'w_mod': nrm(ks[4], (DEPTH, D_MODEL, 3 * D_MODEL), D_MODEL ** -0.5),
        'b_mod': nrm(ks[5], (DEPTH, 3 * D_MODEL), 0.02),
        'norm_g': 1.0 + nrm(ks[6], (DEPTH, D_MODEL), 0.02),
        'w_in': nrm(ks[7], (DEPTH, D_MODEL, D_IN), D_MODEL ** -0.5),
        'w_alpha2': nrm(ks[8], (DEPTH, 2, GLA_RANK, A_QK), GLA_RANK ** -0.5),
        'b_alpha': nrm(ks[9], (DEPTH, 2, A_QK), 0.1),
        'gla_norm_g': 1.0 + nrm(ks[10], (DEPTH, A_V), 0.02),
        'b_gates': nrm(ks[11], (DEPTH, 4, ML_HEADS), 0.1) + gate_sel * f_bias[None, :],
        'conv_w': nrm(ks[12], (DEPTH, ML_CONV, 2 * B_W), ML_CONV ** -0.5),
        'conv_b': nrm(ks[13], (DEPTH, 2 * B_W), 0.02),
        'rpb': nrm(ks[14], (DEPTH, NA_HEADS, 2 * NA_KH - 1, 2 * NA_KW - 1), 0.1),
        'w_merge': nrm(ks[15], (DEPTH, D_MODEL, N_BRANCH * D_MODEL), D_MODEL ** -0.5),
        'b_merge': nrm(ks[16], (DEPTH, N_BRANCH * D_MODEL), 0.02),
        'w_proj_a': nrm(ks[17], (DEPTH, A_V, D_MODEL), A_V ** -0.5),
        'w_proj_b': nrm(ks[18], (DEPTH, B_W, D_MODEL), B_W ** -0.5),
        'w_proj_c': nrm(ks[19], (DEPTH, C_W, D_MODEL), C_W ** -0.5),
        'w_out': nrm(ks[20], (DEPTH, D_MODEL, D_MODEL), D_MODEL ** -0.5),
        'final_g': 1.0 + nrm(ks[21], (D_MODEL,), 0.02),
    }


def reference(x, c, ctx, c_ctx, w_mod, b_mod, norm_g, w_in, w_alpha2, b_alpha, gla_norm_g, b_gates,
              conv_w, conv_b, rpb, w_merge, b_merge, w_proj_a, w_proj_b, w_proj_c, w_out, final_g):
    xl, xc = x, ctx
    for l in range(DEPTH):
        last = l == DEPTH - 1
        mod = jax.nn.silu(c) @ w_mod[l] + b_mod[l]
        mod_c = jax.nn.silu(c_ctx) @ w_mod[l] + b_mod[l]
        shift, scale, gate = jnp.split(mod[:, None, :], 3, axis=-1)
        shift_c, scale_c, gate_c = jnp.split(mod_c, 3, axis=-1)
        h = rmsnorm(xl, norm_g[l]) * (1 + scale) + shift
        hc = rmsnorm(xc, norm_g[l]) * (1 + scale_c) + shift_c
        out, out_c = hybrid_mixer(h, hc, w_in[l], w_alpha2[l], b_alpha[l], gla_norm_g[l], b_gates[l],
                                  conv_w[l], conv_b[l], rpb[l], w_merge[l], b_merge[l],
                                  w_proj_a[l], w_proj_b[l], w_proj_c[l], w_out[l], not last)
        xl = xl + gate * out
        if not last:
            xc = xc + gate_c * out_c
    return rmsnorm(xl, final_g)
```

```python
from contextlib import ExitStack
import numpy as np
import ml_dtypes
import concourse.bass as bass
import concourse.mybir as mybir
from concourse.bass_utils import run_bass_kernel_spmd

F32 = mybir.dt.float32
BF16 = mybir.dt.bfloat16
AF = mybir.ActivationFunctionType
ALU = mybir.AluOpType
AX = mybir.AxisListType

D = 4096
L = 4096
LC = 256
NT = 9
NTOK = 1088
EPS = 1e-6
D_IN = 16448
NCOL = 4136
C_GLA, C_ML, C_NA, C_DEC, C_GAT = 0, 768, 2048, 4096, 4128
NEG = -30000.0
GROUPS = [[0, 1, 2, 3], [4, 5, 6, 7]]

ENGS = ('pe', 'dve', 'act', 'pool', 'sp')
ENGOBJ = {'pe': 'tensor', 'dve': 'vector', 'act': 'scalar', 'pool': 'gpsimd', 'sp': 'sync'}
NDMASEM = 8
SEM_ROLL = 30000
import os
SAME_ENGINE_SYNC = not os.environ.get('NO_SES')


class Op:
    __slots__ = ('eng', 'fn', 'deps', 'signal', 'tok', 'is_dma', 'dsem', 'prev_dma', 'inc')

    def __init__(self, eng, fn, is_dma, inc):
        self.eng, self.fn, self.is_dma, self.inc = eng, fn, is_dma, inc
        self.deps = []
        self.signal = False
        self.tok = None
        self.dsem = None
        self.prev_dma = None


class Prog:
    def __init__(self, nc):
        self.nc = nc
        self.q = {e: [] for e in ENGS}
        self.state = {}
        self.ndma = {e: 0 for e in ENGS}
        self.dma_hist = {e: [] for e in ENGS}
        self.es = ExitStack()
        self.esem = {e: [] for e in ENGS}
        self.ecnt = {e: SEM_ROLL for e in ENGS}
        self.dsem = {}
        self.dcnt = {}
        self.seen = {e: {} for e in ENGS}
        self.last = {e: None for e in ENGS}
        self.nops = 0
        self.rec = None
        self.rec_prefix = None

    def sem(self, name):
        return self.es.enter_context(self.nc.semaphore(name))

    def add(self, eng, fn, reads=(), writes=(), dma=False, inc=16, semq=None):
        if self.rec is not None:
            pf = self.rec_prefix
            fix = lambda kk: kk if (isinstance(kk, tuple) and kk and kk[0] == 'glob') else (pf, kk)
            self.rec.append((eng, fn, tuple(fix(x) for x in reads), tuple(fix(x) for x in writes), dma, inc, semq))
            return None
        for kk in reads:
            if isinstance(kk, tuple) and kk and kk[0] == 'glob' and kk[1] == 'st':
                assert kk in self.state, ('stash read before write', kk)
        op = Op(eng, fn, dma, inc)
        deps = []
        for k in reads:
            st = self.state.get(k)
            if st is not None:
                deps.extend(st[0])
        for k in writes:
            st = self.state.get(k)
            if st is not None:
                deps.extend(st[0])
                deps.extend(st[1])
        seen = set()
        for d in deps:
            if id(d) in seen:
                continue
            seen.add(id(d))
            if (not d.is_dma) and d.eng == eng and (eng == 'pe' or not SAME_ENGINE_SYNC):
                continue
            op.deps.append(d)
            d.signal = True
        for k in reads:
            st = self.state.setdefault(k, [[], []])
            if not dma:
                st[1] = [r for r in st[1] if r.is_dma or r.eng != eng]
            st[1].append(op)
        for k in writes:
            self.state[k] = [[op], []]
        if dma:
            sq = semq or eng
            n = self.ndma.setdefault(sq, 0)
            self.ndma[sq] = n + 1
            op.dsem = ('dma_' + sq, n % NDMASEM)
            hist = self.dma_hist.setdefault(sq, [])
            if n >= NDMASEM:
                op.prev_dma = hist[n - NDMASEM]
            hist.append(op)
        self.q[eng].append(op)
        self.nops += 1
        return op

    def pe(self, fn, r=(), w=()):
        return self.add('pe', fn, r, w)

    def dve(self, fn, r=(), w=()):
        return self.add('dve', fn, r, w)

    def act(self, fn, r=(), w=()):
        return self.add('act', fn, r, w)

    def pool(self, fn, r=(), w=()):
        return self.add('pool', fn, r, w)

    def dma(self, out, in_, r=(), w=(), eng='sp'):
        return self.add(eng, lambda e: e.dma_start(out=out, in_=in_), r, w, dma=True)

    def flush(self):
        lasts = []
        for e in ENGS:
            ops = [o for o in self.q[e] if (not o.is_dma) and o.fn is not None]
            if ops:
                ops[-1].signal = True
                lasts.append(ops[-1])
        dmas = []
        for sq in self.dma_hist:
            hist = self.dma_hist[sq]
            dmas.extend(hist[-NDMASEM:])
        for e in ENGS:
            b = Op(e, None, False, 0)
            b.deps = [o for o in lasts if o.eng != e] + dmas
            self.q[e].append(b)
        nc = self.nc
        for e in ENGS:
            for op in self.q[e]:
                if op.fn is None:
                    continue
                if op.is_dma:
                    if op.dsem not in self.dsem:
                        self.dsem[op.dsem] = self.sem('d_%s_%d' % op.dsem)
                        self.dcnt[op.dsem] = 0
                    v = self.dcnt[op.dsem] + op.inc
                    self.dcnt[op.dsem] = v
                    op.tok = (op.dsem, v, self.dsem[op.dsem])
                elif op.signal:
                    if self.ecnt[e] >= SEM_ROLL:
                        self.esem[e].append(self.sem('s_%s_%d' % (e, len(self.esem[e]))))
                        self.ecnt[e] = 0
                    self.ecnt[e] += 1
                    op.tok = ((e, len(self.esem[e])), self.ecnt[e], self.esem[e][-1])
        with nc.Block() as block:
            for e in ENGS:
                ops = self.q[e]

                def body(eng, ops=ops, e=e):
                    self.body_cache = {}
                    seen = self.seen[e]
                    for op in ops:
                        deps = list(op.deps)
                        if op.prev_dma is not None:
                            deps.append(op.prev_dma)
                        for dd in deps:
                            key, val, sem = dd.tok
                            if seen.get(key, 0) >= val:
                                continue
                            seen[key] = val
                            eng.wait_ge(sem, val)
                        if op.fn is None:
                            continue
                        ins = op.fn(eng)
                        if op.tok is not None:
                            ins.then_inc(op.tok[2], op.inc if op.is_dma else 1)

                getattr(block, ENGOBJ[e])(body)
        self.q = {e: [] for e in ENGS}
        self.state = {}

    def interleave(self, fns):
        recs = []
        for i, fn in enumerate(fns):
            self.rec, self.rec_prefix = [], 'il%d' % i
            fn()
            recs.append(self.rec)
        self.rec = None
        n = max(len(r) for r in recs)
        for i in range(n):
            for r in recs:
                if i < len(r):
                    self.add(*r[i])

    def close(self):
        self.es.close()


class Scope:
    def __init__(self, p):
        self.p = p
        self.es = ExitStack()

    CNT = [0]

    def sb(self, name, shape, dt):
        Scope.CNT[0] += 1
        return self.es.enter_context(self.p.nc.sbuf_tensor('%s_%d' % (name, Scope.CNT[0]), list(shape), dt))

    def ps(self, name, shape, dt=F32):
        Scope.CNT[0] += 1
        full = 512 if dt == F32 else 1024
        t = self.es.enter_context(self.p.nc.psum_tensor('%s_%d' % (name, Scope.CNT[0]), [shape[0], full], dt))
        n = 1
        for d_ in shape[1:]:
            n *= d_
        v = t[:, 0:n]
        if len(shape) == 3:
            v = v.rearrange("p (a b) -> p a b", a=shape[1])
        return v

    def end(self):
        self.p.flush()
        self.es.close()


IN_SPLITS = (512, 512, 1024, 1024, 32, 1024, 1024, 1024, 1024, 1024, 32, 2048, 2048, 2048, 2048)
OFFS = np.concatenate([[0], np.cumsum(IN_SPLITS)])


def my_cols(j):
    A_q, A_k, A_v, A_z, A_d, B_q, B_k, B_v, B_z, B_o, B_g, C_q, C_k, C_v, C_z = OFFS[:15]
    cols = []
    hs = (2 * j, 2 * j + 1)
    for base in (A_q, A_k):
        for h in hs:
            cols.append(np.arange(64) + base + h * 64)
    for base in (A_v, A_z):
        for h in hs:
            cols.append(np.arange(128) + base + h * 128)
    for base in (B_q, B_k, B_v, B_z, B_o):
        for h in hs:
            cols.append(np.arange(128) + base + h * 128)
    for base in (C_q, C_k, C_v, C_z):
        for h in range(4 * j, 4 * j + 4):
            cols.append(np.arange(128) + base + h * 128)
    cols.append(np.arange(32) + A_d)
    for t in range(4):
        for h in hs:
            cols.append(np.array([B_g + t * 8 + h]))
    c = np.concatenate(cols)
    assert c.shape[0] == NCOL
    return c


def na_tables(rpb_l, j):
    out = np.full((4, 5, 128, 576), NEG, np.float32)
    reps = [0, 1, 2, 30, 31]
    cc = np.arange(64)
    cstart = np.clip(cc - 8, 0, 48)
    for ti, rp in enumerate(reps):
        rs_lo = int(np.clip(2 * rp - 4, 0, 55))
        for jj in range(2):
            r = 2 * rp + jj
            rs = int(np.clip(r - 4, 0, 56))
            for a in range(9):
                kr = rs_lo + a
                if kr < rs or kr >= rs + 8 or kr > 63:
                    continue
                dr = kr - r + 7
                for c in range(64):
                    cp = np.arange(cstart[c], cstart[c] + 16)
                    dc = cp - c + 15
                    for hh in range(4):
                        out[hh, ti, jj * 64 + c, a * 64 + cp] = rpb_l[4 * j + hh, dr, dc]
    return out


def rope_table():
    nf = 16
    inv = (10000.0 ** (-np.arange(nf, dtype=np.float32) / nf)).astype(np.float32)
    tab = np.zeros((64, 64, 2, 8, 16), np.float32)
    for c in range(64):
        ang_r = (np.float32(c) * inv).astype(np.float32)
        ang_c = (np.arange(64, dtype=np.float32)[:, None] * inv[None, :]).astype(np.float32)
        for b8 in range(8):
            if b8 % 2 == 0:
                tab[c, :, 0, b8, :] = np.cos(ang_r)[None, :]
                tab[c, :, 1, b8, :] = np.sin(ang_r)[None, :]
            else:
                tab[c, :, 0, b8, :] = np.cos(ang_c)
                tab[c, :, 1, b8, :] = np.sin(ang_c)
    return tab.reshape(64, 64, 256)


def const_tables():
    s = np.arange(64)[:, None]
    t = np.arange(64)[None, :]
    lo = (s <= t).astype(np.float32)
    up = (s >= t).astype(np.float32)
    c = np.zeros((64, 12, 64), np.float32)
    c[:, 0] = lo
    c[:, 1] = up
    c[:, 2] = lo * (-1.0 / 16)
    c[:, 3] = up * (-1.0 / 16)
    c[:, 4] = -1.0 / 16
    c[:, 5] = -lo
    c[:, 6] = -up
    c[:, 7] = np.where(t <= s, 0.0, NEG)
    c[:, 8] = np.where(t >= s, 0.0, NEG)
    c[:, 9] = np.eye(64, dtype=np.float32)
    c[:, 10] = 1.0
    c[:, 11] = -1.0
    return c


def prep(inp):
    f = np.float32
    x, c, ctx, c_ctx = inp['x'], inp['c'], inp['ctx'], inp['c_ctx']
    rope = rope_table()
    cst = const_tables()
    ident = np.eye(128, dtype=f)
    maps = []
    for i in range(8):
        b, j = i // 4, i % 4
        m = {}
        m['x_tok'] = np.ascontiguousarray(np.concatenate([x[b, j * 1024:(j + 1) * 1024], ctx[b, j * 64:(j + 1) * 64]], 0))
        c2 = np.stack([c[b], c_ctx], 0)
        m['c2T'] = np.ascontiguousarray(c2.reshape(2, 32, 128).transpose(2, 1, 0))
        mc = np.concatenate([np.arange(1024) + part * 4096 + j * 1024 for part in range(3)])
        m['w_mod_s'] = np.ascontiguousarray(inp['w_mod'][:, :, mc])
        m['b_mod_s'] = np.ascontiguousarray(np.repeat(inp['b_mod'][:, None, mc], 2, axis=1))
        m['norm_g'] = inp['norm_g']
        m['final_g'] = inp['final_g'].reshape(1, D)
        cols = my_cols(j)
        m['w_in_s'] = np.ascontiguousarray(inp['w_in'][:, :, cols])
        hc = np.concatenate([np.arange(64) + h * 64 for h in (2 * j, 2 * j + 1)])
        wal = np.zeros((2, 2, 17, 128), f)
        wal[:, :, :16, :] = inp['w_alpha2'][:, :, :, hc]
        wal[:, :, 16, :] = inp['b_alpha'][:, :, hc]
        m['w_al'] = wal
        m['gla_g'] = np.ascontiguousarray(inp['gla_norm_g'][:, 2 * j * 128:(2 * j + 2) * 128])
        m['b_gates_s'] = np.ascontiguousarray(inp['b_gates'][:, :, 2 * j:2 * j + 2].reshape(2, 8))
        qc = np.concatenate([np.arange(256) + 2 * j * 128, np.arange(256) + 1024 + 2 * j * 128])
        m['conv_w_s'] = np.ascontiguousarray(inp['conv_w'][:, :, qc])
        m['conv_b_s'] = np.ascontiguousarray(inp['conv_b'][:, qc])
        m['natab'] = np.stack([na_tables(inp['rpb'][l], j) for l in range(2)], 0)
        m['rope'] = rope
        m['cst'] = cst
        m['ident'] = ident
        m['w_merge'] = inp['w_merge']
        m['b_merge'] = inp['b_merge']
        m['w_pa'] = inp['w_proj_a']
        m['w_pb'] = inp['w_proj_b']
        m['w_pc'] = inp['w_proj_c']
        m['w_out'] = inp['w_out']
        maps.append(m)
    return maps


class K:
    def __getattr__(self, name):
        specs = self.__dict__.get('_specs', {})
        if name in specs:
            shape, dt = specs[name]
            ap = self.nc.dram_tensor(name, list(shape), dt, kind="ExternalInput").ap()
            self.__dict__[name] = ap
            self.declared.append(name)
            return ap
        raise AttributeError(name)


def build_nc(stop_after=None, dbg=False, nlayers=2, only=None):
    nc = bass.Bass("TRN2", target_bir_lowering=False)
    k = K()
    k.nc = nc
    k.dbg = dbg
    k.only = only

    order = ['M', 'N0', 'A0', 'G0', 'L0', 'C0', 'B0', 'N1', 'A1', 'G1', 'L1', 'C1', 'B1', 'F']
    upto = len(order) if stop_after is None else order.index(stop_after) + 1
    need = {'c2T': 'M', 'w_mod_s': 'M', 'b_mod_s': 'M', 'x_tok': 'N0', 'norm_g': 'N0', 'ident': 'N0', 'w_in_s': 'A0',
            'w_al': 'G0', 'gla_g': 'G0', 'rope': 'G0', 'cst': 'G0', 'b_gates_s': 'L0', 'conv_w_s': 'L0', 'conv_b_s': 'L0',
            'natab': 'C0', 'w_merge': 'B0', 'b_merge': 'B0', 'w_pa': 'B0', 'w_pb': 'B0', 'w_pc': 'B0', 'w_out': 'B0', 'final_g': 'F'}
    k.declared = []

    k._specs = {}

    def din(name, shape, dt=F32):
        k._specs[name] = (shape, dt)
        return None

    def dint(name, shape, dt=F32):
        return nc.dram_tensor(name, list(shape), dt, kind="ExternalOutput" if (dbg and name in DBG_OUT) else "Internal").ap()

    din('x_tok', [NTOK, D])
    din('c2T', [128, 32, 2])
    din('w_mod_s', [2, D, 3072])
    din('b_mod_s', [2, 2, 3072])
    din('norm_g', [2, D])
    din('final_g', [1, D])
    din('w_in_s', [2, D, NCOL])
    din('w_al', [2, 2, 17, 128])
    din('gla_g', [2, 256])
    din('b_gates_s', [2, 8])
    din('conv_w_s', [2, 3, 512])
    din('conv_b_s', [2, 512])
    din('natab', [2, 4, 5, 128, 576])
    din('rope', [64, 64, 256])
    din('cst', [64, 12, 64])
    din('ident', [128, 128])
    din('w_merge', [2, D, 3 * D])
    din('b_merge', [2, 3 * D])
    din('w_pa', [2, 1024, D])
    din('w_pb', [2, 1024, D])
    din('w_pc', [2, 2048, D])
    din('w_out', [2, D, D])
    k.out = nc.dram_tensor('out', [1024, D], F32, kind="ExternalOutput").ap()

    k.mod_loc = dint('mod_loc', [2, 2, 3, 1024])
    k.mod_all = dint('mod_all', [4 * 2, 2, 3, 1024])
    k.hT_loc = [dint('hT_loc%d' % t, [128, 32 * 128], BF16) for t in range(NT)]
    k.hT_all = [dint('hT_all%d' % t, [4 * 128, 32 * 128], BF16) for t in range(NT)]
    if only is not None:
        k.P_lat = nc.dram_tensor('P_lat', [L, NCOL], F32, kind="ExternalInput").ap()
        k.P_ctx = nc.dram_tensor('P_ctx', [LC, NCOL], F32, kind="ExternalInput").ap()
        k.declared += ['P_lat', 'P_ctx']
    else:
        k.P_lat = dint('P_lat', [L, NCOL])
        k.P_ctx = dint('P_ctx', [LC, NCOL])
    k.stash = dint('stash', [L + LC, 1024])
    k.y_loc = [dint('y_loc%d' % t, [512 if t < 8 else 256, 1024], BF16) for t in range(NT)]
    k.y_all = [dint('y_all%d' % t, [4 * (512 if t < 8 else 256), 1024], BF16) for t in range(NT)]
    if dbg:
        k.y_dbg = nc.dram_tensor('y_dbg', [NT * 512, 1024], BF16, kind='ExternalOutput').ap()
    k.G_scr = dint('G_scr', [NT * 3 * 8 * 128, 512], BF16)
    k.x_loc = dint('x_loc', [NTOK, D])
    k.u_scr = dint('u_scr', [NTOK, D], BF16)
    if dbg:
        k.xn_dbg = nc.dram_tensor('xn_dbg', [128, D], F32, kind='ExternalOutput').ap()
        k.hn_dbg = nc.dram_tensor('hn_dbg', [128, D], BF16, kind='ExternalOutput').ap()
        k.hTt_dbg = nc.dram_tensor('hTt_dbg', [128, D], BF16, kind='ExternalOutput').ap()
        k.A_dbg = nc.dram_tensor('A_dbg', [128, D], F32, kind='ExternalOutput').ap()
        k.mod_dbg = nc.dram_tensor('mod_dbg', [48, 1024], F32, kind='ExternalOutput').ap()
        k.hT_dbg = nc.dram_tensor('hT_dbg', [NT * 512, 4096], BF16, kind='ExternalOutput').ap()

    p = Prog(nc)
    k.p = p
    phases = []
    phases.append(('M', lambda: phase_mod(k)))
    for l in range(nlayers):
        phases.append(('N%d' % l, lambda l=l: phase_norm(k, l)))
        phases.append(('A%d' % l, lambda l=l: phase_proj(k, l)))
        phases.append(('G%d' % l, lambda l=l: phase_gla(k, l)))
        phases.append(('L%d' % l, lambda l=l: phase_mlstm(k, l)))
        phases.append(('C%d' % l, lambda l=l: phase_na(k, l)))
        phases.append(('B%d' % l, lambda l=l: phase_b(k, l, last=(l == 1))))
    phases.append(('F', lambda: phase_final(k)))
    for name, fn in phases:
        if only is not None and name != only:
            continue
        fn()
        if stop_after == name:
            break
    p.flush()
    p.close()
    nc.declared_inputs = k.declared
    return nc


DBG_OUT = ('P_lat', 'P_ctx', 'x_loc', 'stash', 'y_dbg')


def allgather(k, src, dst, rkeys, wkeys):
    k.p.add('pool', lambda e: e.collective_compute("AllGather", ALU.bypass, replica_groups=GROUPS, ins=[src], outs=[dst]),
            rkeys, wkeys, dma=True, inc=1, semq='cc')


def phase_mod(k):
    p = k.p
    s = Scope(p)
    c2 = s.sb('m_c2', [128, 32, 2], F32)
    cs = s.sb('m_cs', [128, 32, 2], BF16)
    bm = s.sb('m_bm', [2, 2 * 3072], F32)
    mo = s.sb('m_mo', [2, 2 * 3072], F32)
    wm = [s.sb('m_wm%d' % i, [128, 32, 512], BF16) for i in range(2)]
    ps = [s.ps('m_ps%d' % i, [2, 512], F32) for i in range(2)]
    p.dma(c2[:], k.c2T, w=['c2'])
    p.dma(bm[:].rearrange("m (l c) -> m l c", l=2), k.b_mod_s.rearrange("l m c -> m l c"), w=['bm'])
    p.act(lambda e: e.activation(cs[:], c2[:], AF.Silu), r=['c2'], w=['cs'])
    it = 0
    for l in range(2):
        for cb in range(6):
            b = it % 2
            it += 1
            p.dma(wm[b][:], k.w_mod_s[l][:, cb * 512:(cb + 1) * 512].rearrange("(kk pp) c -> pp kk c", pp=128),
                  w=['wm%d' % b], eng='pool')
            for kk in range(32):
                p.pe(lambda e, b=b, kk=kk: e.matmul(ps[b][:], cs[:, kk, :], wm[b][:, kk, :], start=(kk == 0), stop=(kk == 31)),
                     r=['cs', 'wm%d' % b], w=['mps%d' % b])
            o = l * 3072 + cb * 512
            p.dve(lambda e, b=b, o=o: e.tensor_tensor(mo[:, o:o + 512], ps[b][:], bm[:, o:o + 512], ALU.add),
                  r=['mps%d' % b, 'bm'], w=['mo'])
    p.dma(k.mod_loc.rearrange("l m a c -> m l (a c)"), mo[:].rearrange("m (l c) -> m l c", l=2), r=['mo'], w=['mod_loc'])
    allgather(k, k.mod_loc.rearrange("l m a c -> (l m a) c"), k.mod_all.rearrange("rl m a c -> (rl m a) c"), ['mod_loc'], ['mod_all'])
    s.end()


def modvec_bc(k, l, m, a):
    v = k.mod_all.rearrange("(r l) m a c -> l m a r c", l=2)[l, m, a]
    return v.partition_broadcast(128)


def phase_norm(k, l):
    p = k.p
    s = Scope(p)
    A = [s.sb('n_A%d' % m, [128, D], F32) for m in range(2)]
    S = [s.sb('n_S%d' % m, [128, D], F32) for m in range(2)]
    gbc = s.sb('n_g', [128, D], F32)
    xt = [s.sb('n_x%d' % i, [128, D], F32) for i in range(2)]
    xn = s.sb('n_xn', [128, D], F32)
    hn = s.sb('n_hn', [128, D], BF16)
    junk = s.sb('n_junk', [128, D], BF16)
    hT = [s.sb('n_hT%d' % i, [128, 32, 128], BF16) for i in range(2)]
    st = s.sb('n_st', [128, 4], F32)
    idb = s.sb('n_idb', [128, 128], BF16)
    pst = [s.ps('n_ps%d' % i, [128, 8, 128], BF16) for i in range(2)]
    p.dma(idb[:], k.ident, w=['idb'], eng='pool')
    p.dma(gbc[:], k.norm_g[l].partition_broadcast(128), w=['gbc'])
    for m in range(2):
        p.dma(A[m][:].rearrange("p (r c) -> p r c", r=4), modvec_bc(k, l, m, 1), w=['A%d' % m])
        p.dma(S[m][:].rearrange("p (r c) -> p r c", r=4), modvec_bc(k, l, m, 0), w=['S%d' % m])
        p.dve(lambda e, m=m: e.scalar_tensor_tensor(A[m][:], A[m][:], 1.0, gbc[:], ALU.add, ALU.mult), r=['A%d' % m, 'gbc'], w=['A%d' % m])
    for i in range(2):
        p.dve(lambda e, i=i: e.memset(hT[i][:], 0.0), w=['hT%d' % i])
    xsrc = k.x_tok if l == 0 else k.x_loc
    for t in range(NT):
        rows = 128 if t < 8 else 64
        m = 0 if t < 8 else 1
        b = t % 2
        p.dma(xt[b][:rows], xsrc[t * 128:t * 128 + rows, :], w=['xt%d' % b])
        p.dve(lambda e: e.memset(st[:], 0.0), w=['st'])
        p.act(lambda e, b=b, rows=rows: e.activation(junk[:rows], xt[b][:rows], AF.Square, accum_out=st[:rows, 0:1]), r=['xt%d' % b, 'st'], w=['junk', 'st'])
        p.dve(lambda e, rows=rows: e.tensor_scalar(st[:rows, 1:2], st[:rows, 0:1], 1.0 / D, EPS, ALU.mult, ALU.add), r=['st'], w=['st'])
        p.act(lambda e, rows=rows: e.activation(st[:rows, 1:2], st[:rows, 1:2], AF.Sqrt), r=['st'], w=['st'])
        p.dve(lambda e, rows=rows: e.reciprocal(st[:rows, 2:3], st[:rows, 1:2]), r=['st'], w=['st'])
        p.dve(lambda e, b=b, rows=rows, m=m: e.scalar_tensor_tensor(xn[:rows], xt[b][:rows], st[:rows, 2:3], A[m][:rows], ALU.mult, ALU.mult),
              r=['xt%d' % b, 'st', 'A%d' % m], w=['xn'])
        p.dve(lambda e, rows=rows, m=m: e.tensor_tensor(hn[:rows], xn[:rows], S[m][:rows], ALU.add), r=['xn', 'S%d' % m], w=['hn'])
        for g in range(4):
            pb = (t * 4 + g) % 2
            for i in range(8):
                kk = g * 8 + i
                p.pe(lambda e, pb=pb, i=i, kk=kk, rows=rows: e.transpose(pst[pb][:, i, :rows], hn[:rows, kk * 128:(kk + 1) * 128], idb[:rows, :rows]),
                     r=['hn', 'idb'], w=['pst%d' % pb])
            if g % 2 == 0:
                p.act(lambda e, pb=pb, g=g, b=b, rows=rows: e.copy(hT[b][:, g * 8:(g + 1) * 8, :rows], pst[pb][:, :, :rows]), r=['pst%d' % pb], w=['hT%d' % b])
            else:
                p.dve(lambda e, pb=pb, g=g, b=b, rows=rows: e.tensor_copy(hT[b][:, g * 8:(g + 1) * 8, :rows], pst[pb][:, :, :rows]), r=['pst%d' % pb], w=['hT%d' % b])
        p.dma(k.hT_loc[t], hT[b][:].rearrange("p a b -> p (a b)"), r=['hT%d' % b], w=[('hT_loc', t)])
        if k.dbg and l == 0 and t == 0:
            p.dma(k.xn_dbg, xn[:], r=['xn'], w=['d1'])
            p.dma(k.hn_dbg, hn[:], r=['hn'], w=['d2'])
            p.dma(k.hTt_dbg, hT[b][:].rearrange("p a b -> p (a b)"), r=['hT%d' % b], w=['d3'])
            p.dma(k.A_dbg, A[0][:], r=['A0'], w=['d4'])
        allgather(k, k.hT_loc[t], k.hT_all[t], [('hT_loc', t)], [('hT_all', t)])
        if k.dbg and l == 0:
            p.dma(k.hT_dbg[t * 512:(t + 1) * 512, :], k.hT_all[t], r=[('hT_all', t)], w=[('hTd', t)])
    if k.dbg and l == 0:
        p.dma(k.mod_dbg, k.mod_all.rearrange("rl m a c -> (rl m a) c"), w=['modd'])

    s.end()


def proj_stream(k, l, s, blocks, nps, tag):
    p = k.p
    wb = [s.sb('a_w%s%d' % (tag, i), [128, 32, 512], BF16) for i in range(2)]
    ht = [s.sb('a_h%s%d' % (tag, i), [128, 32, 128], BF16) for i in range(3)]
    ob = [s.sb('a_o%s%d' % (tag, i), [128, 512], F32) for i in range(3)]
    ps = [s.ps('a_ps%s%d' % (tag, i), [128, 512], F32) for i in range(nps)]
    it = 0
    for bi, nb in enumerate(blocks):
        c0 = nb * 512
        n = 512 if nb < 8 else NCOL - 4096
        wi = bi % 2
        p.dma(wb[wi][:, :, :n], k.w_in_s[l][:, c0:c0 + n].rearrange("(kk pp) c -> pp kk c", pp=128), w=['aw%d' % wi], eng='pool')
        for r in range(4):
            for t in range(NT):
                rows = 128 if t < 8 else 64
                hi, oi, pi = it % 3, it % 3, it % nps
                it += 1
                p.dma(ht[hi][:].rearrange("p a b -> p (a b)"), k.hT_all[t][r * 128:(r + 1) * 128, :], w=['ah%d' % hi])
                for kk in range(32):
                    p.pe(lambda e, pi=pi, hi=hi, wi=wi, kk=kk, rows=rows, n=n: e.matmul(ps[pi][:rows, :n], ht[hi][:, kk, :rows], wb[wi][:, kk, :n], start=(kk == 0), stop=(kk == 31)),
                         r=['ah%d' % hi, 'aw%d' % wi], w=['aps%d' % pi])
                if it % 2 == 0:
                    p.act(lambda e, oi=oi, pi=pi, rows=rows, n=n: e.copy(ob[oi][:rows, :n], ps[pi][:rows, :n]), r=[], w=['ao%d' % oi, 'aps%d' % pi])
                else:
                    p.dve(lambda e, oi=oi, pi=pi, rows=rows, n=n: e.tensor_copy(ob[oi][:rows, :n], ps[pi][:rows, :n]), r=[], w=['ao%d' % oi, 'aps%d' % pi])
                if t < 8:
                    dst = k.P_lat[r * 1024 + t * 128: r * 1024 + t * 128 + 128, c0:c0 + n]
                else:
                    dst = k.P_ctx[r * 64:(r + 1) * 64, c0:c0 + n]
                p.dma(dst, ob[oi][:rows, :n], r=['ao%d' % oi], w=[('P', r, t, nb)], eng='act')


def phase_proj(k, l):
    s = Scope(k.p)
    proj_stream(k, l, s, [0, 1, 2, 3, 8], 4, 'p1')
    s.end()


def gates_stream(k, l, s, ntiles):
    p = k.p
    hTs = s.sb('b_hTs', [128, 32, NT * 128], BF16)
    wb = [s.sb('b_wb%d' % i, [128, 32, 512], BF16) for i in range(2)]
    bms = [s.sb('b_bm%d' % i, [1, 512], BF16) for i in range(2)]
    onesr = s.sb('b_ones', [1, 128], BF16)
    Gsb = [s.sb('b_G%d' % i, [128, 512], BF16) for i in range(3)]
    ps = [s.ps('b_ps%d' % i, [128, 512], F32) for i in range(2)]
    p.dve(lambda e: e.memset(onesr[:], 1.0), w=['onesr'])
    for t in range(ntiles):
        p.dma(hTs[:, :, t * 128:(t + 1) * 128], k.hT_loc[t].rearrange("q (a b) -> q a b", a=32), w=[('hTs', t)])
    it = 0
    wi = 0
    for cb in range(8):
        for kb in range(3):
            c0 = kb * D + cb * 512
            w_ = wi % 2
            wi += 1
            p.dma(wb[w_][:], k.w_merge[l][:, c0:c0 + 512].rearrange("(kk pp) c -> pp kk c", pp=128), w=['bw%d' % w_], eng='pool')
            p.dma(bms[w_][:], k.b_merge[l:l + 1, c0:c0 + 512], w=['bm%d' % w_], eng='pool')
            for t in range(ntiles):
                rows = 128 if t < 8 else 64
                pi, gi = it % 2, it % 3
                it += 1
                for kk in range(32):
                    p.pe(lambda e, pi=pi, w_=w_, kk=kk, t=t, rows=rows: e.matmul(ps[pi][:rows, :], hTs[:, kk, t * 128:t * 128 + rows], wb[w_][:, kk, :], start=(kk == 0), stop=False),
                         r=[('hTs', t), 'bw%d' % w_], w=['bps%d' % pi])
                p.pe(lambda e, pi=pi, rows=rows, w_=w_: e.matmul(ps[pi][:rows, :], onesr[:, :rows], bms[w_][:], start=False, stop=True), r=['onesr', 'bm%d' % w_], w=['bps%d' % pi])
                p.act(lambda e, pi=pi, gi=gi, rows=rows: e.activation(Gsb[gi][:rows, :], ps[pi][:rows, :], AF.Sigmoid), r=[], w=['bG%d' % gi, 'bps%d' % pi])
                g0 = ((t * 3 + kb) * 8 + cb) * 128
                p.dma(k.G_scr[g0:g0 + rows, :], Gsb[gi][:rows, :], r=['bG%d' % gi], w=[('G', t, kb, cb)])


def yrows(k, kind, c, n=64):
    if kind == 'lat':
        t = (c % 16) // 2
        r0 = (c // 16) * 128 + (c % 2) * 64
        return k.y_loc[t][r0:r0 + n, :], ('y', t, c // 16, c % 2)
    return k.y_loc[8][c * 64:c * 64 + n, :], ('y', 8, c, 0)


def dbg_y(k, l, c0, c1):
    if k.dbg and l == 0:
        for t in range(NT):
            n = 512 if t < 8 else 256
            k.p.dma(k.y_dbg[t * 512:t * 512 + n, c0:c1], k.y_loc[t][:, c0:c1], w=[('yd', t)])
        k.p.flush()


def chunk_order(d):
    ctx = [('ctx', c) for c in range(4)]
    lat = [('lat', c) for c in range(64)]
    if d == 1:
        ctx.reverse()
        lat.reverse()
    return ctx + lat


def chunk_pos(d):
    return {kc: i for i, kc in enumerate(chunk_order(d))}


def phase_gla(k, l):
    s = Scope(k.p)
    fns = [lambda d=d: gla_sweep(k, l, d, s) for d in range(2)]
    if k.only is None:
        fns.append(lambda: gates_stream(k, l, s, 8 if l == 1 else NT))
    k.p.interleave(fns)
    s.end()
    dbg_y(k, l, 0, 256)


def gla_sweep(k, l, d, s):
    p = k.p
    mypos, otpos = chunk_pos(d), chunk_pos(1 - d)
    own0, oth0 = (0, 512) if d == 0 else (512, 0)
    cst = s.sb('g_cst', [64, 12, 64], F32)
    waug = s.sb('g_waug', [17, 128], F32)
    gng = s.sb('g_gng', [64, 256], F32)
    mask2 = s.sb('g_mask2', [64, 2, 64], F32)
    S32 = [s.sb('g_S32%d' % h, [64, 128], F32) for h in range(2)]
    Sbf = [s.sb('g_Sbf%d' % h, [64, 128], BF16) for h in range(2)]
    aaT = s.sb('g_aaT', [17, 64], F32)
    pa = [s.sb('g_pa%d' % i, [64, 768], F32) for i in range(2)]
    dec = [s.sb('g_dec%d' % i, [64, 16], F32) for i in range(2)]
    rt = [s.sb('g_rt%d' % i, [64, 256], F32) for i in range(2)]
    of = [s.sb('g_of%d' % i, [64, 256], F32) for i in range(2)]
    e1 = s.sb('g_e1', [64, 128], F32)
    sp = s.sb('g_sp', [64, 128], F32)
    bs = s.sb('g_bs', [64, 128], F32)
    Ep = s.sb('g_Ep', [64, 128], F32)
    Em = s.sb('g_Em', [64, 128], F32)
    dlt = s.sb('g_dlt', [64, 128], F32)
    Eh = s.sb('g_Eh', [64, 128], F32)
    decs = s.sb('g_decs', [64, 2], F32)
    tt = [s.sb('g_t%d' % i, [64, 8, 16], F32) for i in range(4)]
    qkr = s.sb('g_qkr', [64, 256], F32)
    qt = s.sb('g_qt', [64, 128], F32)
    kt = s.sb('g_kt', [64, 128], F32)
    kh = s.sb('g_kh', [64, 128], BF16)
    vbf = s.sb('g_vbf', [64, 256], BF16)
    qkT = s.sb('g_qkT', [64, 4, 64], BF16)
    attm = s.sb('g_attm', [64, 2, 64], BF16)
    osb = s.sb('g_osb', [64, 256], F32)
    junk = s.sb('g_junk', [64, 128], F32)
    ss = s.sb('g_ss', [64, 4], F32)
    sz = s.sb('g_sz', [64, 256], F32)
    tn = s.sb('g_tn', [64, 256], F32)
    ya = s.sb('g_ya', [64, 256], BF16)
    T1 = s.ps('g_T1', [64, 512], F32)
    PA = s.ps('g_PA', [64, 512], F32)
    psT = PA[:, 0:256].rearrange("p (a b) -> p a b", a=4)
    att = PA[:, 256:384].rearrange("p (a b) -> p a b", a=2)
    OK = s.ps('g_OK', [64, 512], F32)
    ops = OK[:, 0:256].rearrange("p (a b) -> p a b", a=2)
    kvp = OK[:, 256:512].rearrange("p (a b) -> p a b", a=2)

    p.dma(cst[:], k.cst, w=['cst'])
    p.dma(waug[:], k.w_al[l, d], w=['waug'])
    p.dma(gng[:], k.gla_g[l].partition_broadcast(64), w=['gng'])
    for h in range(2):
        p.dma(mask2[:, h, :], k.cst[:, d, :], w=['mask2'])
        p.dve(lambda e, h=h: e.memset(S32[h][:], 0.0), w=['S32%d' % h])
        p.dve(lambda e, h=h: e.memset(Sbf[h][:], 0.0), w=['Sbf%d' % h])
    p.dve(lambda e: e.memset(aaT[:], 1.0), w=['aaT'])
    TriD = cst[:, 2 + d, :]
    All16 = cst[:, 4, :]
    id64 = cst[:, 9, :]

    for it, (kind, c) in enumerate(chunk_order(d)):
        b = it % 2
        src = k.P_lat if kind == 'lat' else k.P_ctx
        r0 = c * 64
        srow = r0 if kind == 'lat' else L + r0
        P = 'pa%d' % b
        p.dma(pa[b][:], src[r0:r0 + 64, C_GLA:C_GLA + 768], w=[P])
        p.dma(dec[b][:], src[r0:r0 + 64, C_DEC + 16 * d:C_DEC + 16 * d + 16], w=['dec%d' % b])
        if kind == 'lat':
            p.dma(rt[b][:], k.rope[c], w=['rt%d' % b])
        epi = mypos[(kind, c)] > otpos[(kind, c)]
        p.pe(lambda e, b=b: e.transpose(T1[0:16, 448:512], dec[b][:], id64), r=['dec%d' % b, 'cst'], w=['T1'])
        p.act(lambda e: e.copy(aaT[0:16, :], T1[0:16, 448:512]), r=[], w=['aaT', 'T1'])
        p.pe(lambda e: e.matmul(T1[:, 0:128], aaT[:], waug[:], start=True, stop=True), r=['aaT', 'waug'], w=['T1'])
        p.act(lambda e: e.activation(e1[:], T1[:, 0:128], AF.Exp, scale=-1.0), r=[], w=['e1', 'T1'])
        p.act(lambda e: e.activation(sp[:], e1[:], AF.Ln, bias=1.0), r=['e1'], w=['sp'])
        p.pe(lambda e: e.matmul(T1[:, 128:256], TriD, sp[:], start=True, stop=True), r=['sp', 'cst'], w=['T1'])
        p.pe(lambda e: e.matmul(T1[:, 256:384], All16, sp[:], start=True, stop=True), r=['sp', 'cst'], w=['T1'])
        for h in range(2):
            p.pe(lambda e, h=h: e.matmul(T1[:, 384 + h:385 + h], sp[:, h * 64:(h + 1) * 64], cst[:, 4, 0:1], start=True, stop=True), r=['sp', 'cst'], w=['T1'])
        p.dve(lambda e: e.tensor_copy(bs[:], T1[:, 128:256]), r=[], w=['bs', 'T1'])
        p.act(lambda e: e.activation(Ep[:], bs[:], AF.Exp), r=['bs'], w=['Ep'])
        p.act(lambda e: e.activation(Em[:], bs[:], AF.Exp, scale=-1.0), r=['bs'], w=['Em'])
        p.dve(lambda e: e.tensor_tensor(dlt[:], T1[:, 256:384], bs[:], ALU.subtract), r=['bs'], w=['dlt', 'T1'])
        p.act(lambda e: e.activation(Eh[:], dlt[:], AF.Exp), r=['dlt'], w=['Eh'])
        p.act(lambda e: e.activation(decs[:], T1[:, 384:386], AF.Exp), r=[], w=['decs', 'T1'])
        if kind == 'lat':
            x4 = pa[b][:, 0:256].rearrange("p (a h f) -> p a h f", a=8, h=2)
            cos = rt[b][:, 0:128].rearrange("p (a f) -> p a f", a=8)
            sin = rt[b][:, 128:256].rearrange("p (a f) -> p a f", a=8)
            o4 = qkr[:].rearrange("p (a h f) -> p a h f", a=8, h=2)
            R = [P, 'rt%d' % b]
            p.dve(lambda e, x4=x4, cos=cos: e.tensor_tensor(tt[0][:], x4[:, :, 0, :], cos, ALU.mult), r=R, w=['t0'])
            p.dve(lambda e, x4=x4, sin=sin: e.tensor_tensor(tt[1][:], x4[:, :, 1, :], sin, ALU.mult), r=R, w=['t1'])
            p.dve(lambda e, o4=o4: e.tensor_tensor(o4[:, :, 0, :], tt[0][:], tt[1][:], ALU.subtract), r=['t0', 't1'], w=['qkr'])
            p.dve(lambda e, x4=x4, sin=sin: e.tensor_tensor(tt[2][:], x4[:, :, 0, :], sin, ALU.mult), r=R, w=['t2'])
            p.dve(lambda e, x4=x4, cos=cos: e.tensor_tensor(tt[3][:], x4[:, :, 1, :], cos, ALU.mult), r=R, w=['t3'])
            p.dve(lambda e, o4=o4: e.tensor_tensor(o4[:, :, 1, :], tt[2][:], tt[3][:], ALU.add), r=['t2', 't3', 'qkr'], w=['qkr'])
            qsrc, QK = qkr, 'qkr'
        else:
            qsrc, QK = pa[b], P
        p.dve(lambda e, qsrc=qsrc: e.scalar_tensor_tensor(qt[:], qsrc[:, 0:128], 0.125, Ep[:], ALU.mult, ALU.mult), r=[QK, 'Ep'], w=['qt'])
        p.dve(lambda e, qsrc=qsrc: e.tensor_tensor(kt[:], qsrc[:, 128:256], Em[:], ALU.mult), r=[QK, 'Em'], w=['kt'])
        p.dve(lambda e, qsrc=qsrc: e.tensor_tensor(kh[:], qsrc[:, 128:256], Eh[:], ALU.mult), r=[QK, 'Eh'], w=['kh'])
        p.act(lambda e, b=b: e.copy(vbf[:], pa[b][:, 256:512]), r=[P], w=['vbf'])
        for i in range(4):
            srcT = qt if i < 2 else kt
            p.pe(lambda e, i=i, srcT=srcT: e.transpose(psT[:, i, :], srcT[:, (i % 2) * 64:(i % 2) * 64 + 64], id64), r=['qt', 'kt', 'cst'], w=['PA'])
        p.act(lambda e: e.copy(qkT[:], psT[:]), r=[], w=['qkT', 'PA'])
        for h in range(2):
            p.pe(lambda e, h=h: e.matmul(att[:, h, :], qkT[:, 2 + h, :], qkT[:, h, :], start=True, stop=True), r=['qkT'], w=['PA'])
        p.dve(lambda e: e.tensor_tensor(attm[:], att[:], mask2[:], ALU.mult), r=['mask2'], w=['attm', 'PA'])
        for h in range(2):
            p.pe(lambda e, h=h: e.matmul(ops[:, h, :], attm[:, h, :], vbf[:, h * 128:(h + 1) * 128], start=True, stop=False), r=['attm', 'vbf'], w=['OK'])
            p.pe(lambda e, h=h: e.matmul(ops[:, h, :], qkT[:, h, :], Sbf[h][:], start=False, stop=True), r=['qkT', 'Sbf%d' % h], w=['OK'])
        for h in range(2):
            p.pe(lambda e, h=h: e.matmul(kvp[:, h, :], kh[:, h * 64:(h + 1) * 64], vbf[:, h * 128:(h + 1) * 128], start=True, stop=True), r=['kh', 'vbf'], w=['OK'])
        for h in range(2):
            p.dve(lambda e, h=h: e.scalar_tensor_tensor(S32[h][:], S32[h][:], decs[:, h:h + 1], kvp[:, h, :], ALU.mult, ALU.add), r=['S32%d' % h, 'decs'], w=['S32%d' % h, 'OK'])
            p.act(lambda e, h=h: e.copy(Sbf[h][:], S32[h][:]), r=['S32%d' % h], w=['Sbf%d' % h])
        if not epi:
            p.act(lambda e: e.copy(osb[:], ops[:].rearrange("p a b -> p (a b)")), r=[], w=['osb', 'OK'])
            p.dma(k.stash[srow:srow + 64, own0:own0 + 256], osb[:], r=['osb'], w=[('glob', 'st', 'g', srow)])
        else:
            p.dma(of[b][:], k.stash[srow:srow + 64, oth0:oth0 + 256], r=[('glob', 'st', 'g', srow)], w=['of%d' % b])
            p.dve(lambda e, b=b: e.tensor_tensor(osb[:], ops[:].rearrange("p a b -> p (a b)"), of[b][:], ALU.add), r=['of%d' % b], w=['osb', 'OK'])
            p.dve(lambda e: e.memset(ss[:], 0.0), w=['ss'])
            for h in range(2):
                p.act(lambda e, h=h: e.activation(junk[:], osb[:, h * 128:(h + 1) * 128], AF.Square, accum_out=ss[:, h:h + 1]), r=['osb', 'ss'], w=['junk', 'ss'])
            p.dve(lambda e: e.tensor_scalar(ss[:, 2:4], ss[:, 0:2], 1.0 / 128, EPS, ALU.mult, ALU.add), r=['ss'], w=['ss'])
            p.act(lambda e: e.activation(ss[:, 2:4], ss[:, 2:4], AF.Sqrt), r=['ss'], w=['ss'])
            p.dve(lambda e: e.reciprocal(ss[:, 2:4], ss[:, 2:4]), r=['ss'], w=['ss'])
            p.act(lambda e, b=b: e.activation(sz[:], pa[b][:, 512:768], AF.Silu), r=[P], w=['sz'])
            for h in range(2):
                p.dve(lambda e, h=h: e.scalar_tensor_tensor(tn[:, h * 128:(h + 1) * 128], osb[:, h * 128:(h + 1) * 128], ss[:, 2 + h:3 + h], gng[:, h * 128:(h + 1) * 128], ALU.mult, ALU.mult),
                      r=['osb', 'ss', 'gng'], w=['tn'])
            p.dve(lambda e: e.tensor_tensor(ya[:], tn[:], sz[:], ALU.mult), r=['tn', 'sz'], w=['ya'])
            ydst, ykey = yrows(k, kind, c)
            p.dma(ydst[:, 0:256], ya[:], r=['ya'], w=[('glob',) + ykey + ('a',)])


def phase_mlstm(k, l):
    s = Scope(k.p)
    fns = [lambda d=d: mlstm_sweep(k, l, d, s) for d in range(2)]
    if k.only is None:
        fns.append(lambda: proj_stream(k, l, s, [4, 5, 6, 7], 2, 'p2'))
    k.p.interleave(fns)
    s.end()
    dbg_y(k, l, 256, 512)


def mlstm_sweep(k, l, d, s):
    p = k.p
    mypos, otpos = chunk_pos(d), chunk_pos(1 - d)
    own0, oth0 = (256, 768) if d == 0 else (768, 256)
    cst = s.sb('l_cst', [64, 12, 64], F32)
    convw = s.sb('l_convw', [64, 3, 512], F32)
    convb = s.sb('l_convb', [64, 512], F32)
    bg = s.sb('l_bg', [64, 8], F32)
    ones = s.sb('l_ones', [64, 128], F32)
    nones = s.sb('l_nones', [64, 128], F32)
    CN32 = [s.sb('l_CN32%d' % h, [128, 129], F32) for h in range(2)]
    CNbf = [s.sb('l_CNbf%d' % h, [128, 129], BF16) for h in range(2)]
    mm = s.sb('l_mm', [128, 2], F32)
    v1 = s.sb('l_v1', [64, 2, 129], BF16)
    mn = [s.sb('l_mn%d' % i, [64, 1280], F32) for i in range(2)]
    pv = [s.sb('l_pv%d' % i, [64, 512], F32) for i in range(2)]
    nx = [s.sb('l_nx%d' % i, [64, 512], F32) for i in range(2)]
    gt = [s.sb('l_gt%d' % i, [64, 8], F32) for i in range(2)]
    hf = [s.sb('l_hf%d' % i, [64, 256], F32) for i in range(2)]
    ta = s.sb('l_ta', [64, 512], F32)
    tb = s.sb('l_tb', [64, 512], F32)
    sl = s.sb('l_sl', [64, 512], F32)
    qs = s.sb('l_qs', [64, 256], F32)
    ks = s.sb('l_ks', [64, 256], F32)
    qkT = s.sb('l_qkT', [128, 4, 64], BF16)
    g = s.sb('l_g', [64, 8], F32)
    sm = s.sb('l_sm', [64, 40], F32)
    sm128 = s.sb('l_sm128', [128, 16], F32)
    diag = [s.sb('l_diag%d' % h, [64, 64], F32) for h in range(2)]
    Dm = s.sb('l_D', [64, 2, 64], F32)
    Ew = s.sb('l_Ew', [64, 2, 64], F32)
    qkE = s.sb('l_qkE', [64, 2, 64], F32)
    qkET = s.sb('l_qkET', [64, 2, 64], BF16)
    ins_ = s.sb('l_ins', [64, 2, 128], F32)
    hout = s.sb('l_hout', [64, 256], F32)
    kw = s.sb('l_kw', [64, 2, 128], BF16)
    so = s.sb('l_so', [64, 256], F32)
    sz = s.sb('l_sz', [64, 256], F32)
    yb = s.sb('l_yb', [64, 256], BF16)
    bA = s.ps('l_bA', [128, 512], F32)
    bB = s.ps('l_bB', [128, 512], F32)
    bD = s.ps('l_bD', [128, 512], F32)
    Tg = bA[:, 0:16]
    Rps = bA[:, 16:144].rearrange("p (a b) -> p a b", a=2)
    psE = bA[0:64, 144:272].rearrange("p (a b) -> p a b", a=2)
    Sps = bA[0:64, 272:400].rearrange("p (a b) -> p a b", a=2)
    psT = bB[:, 0:256].rearrange("p (a b) -> p a b", a=4)
    nps = bB[0:64, 256:512].rearrange("p (a b) -> p a b", a=2)
    ips = bD[0:64, 0:258].rearrange("p (a b) -> p a b", a=2)
    ups1 = bD[:, 258:387]

    p.dma(cst[:], k.cst, w=['cst'])
    p.dma(convw[:], k.conv_w_s[l].partition_broadcast(64), w=['convw'])
    p.dma(convb[:], k.conv_b_s[l].partition_broadcast(64), w=['convb'])
    p.dma(bg[:], k.b_gates_s[l].partition_broadcast(64), w=['bg'])
    p.dve(lambda e: e.memset(ones[:], 1.0), w=['ones'])
    p.dve(lambda e: e.memset(nones[:], -1.0), w=['nones'])
    p.dve(lambda e: e.memset(mm[:], 0.0), w=['mm'])
    p.dve(lambda e: e.memset(v1[:], 1.0), w=['v1'])
    for h in range(2):
        p.dve(lambda e, h=h: e.memset(CN32[h][:], 0.0), w=['CN32%d' % h])
        p.dve(lambda e, h=h: e.memset(CNbf[h][:], 0.0), w=['CNbf%d' % h])
    TriM = cst[:, 5 + d, :]
    maskb = cst[:, 7 + d, :]
    id64 = cst[:, 9, :]
    C = lambda a: sm[:, a:a + 2]
    C8 = lambda a: sm128[:, a:a + 2]

    for it, (kind, c) in enumerate(chunk_order(d)):
        b = it % 2
        src = k.P_lat if kind == 'lat' else k.P_ctx
        last_c = 63 if kind == 'lat' else 3
        r0 = c * 64
        srow = r0 if kind == 'lat' else L + r0
        MN, PV, NX, GT = 'mn%d' % b, 'pv%d' % b, 'nx%d' % b, 'gt%d' % b
        p.dma(mn[b][:], src[r0:r0 + 64, C_ML:C_ML + 1280], w=[MN])
        if c == 0:
            p.dve(lambda e, b=b: e.memset(pv[b][:], 0.0), w=[PV])
            p.dma(pv[b][1:64, :], src[0:63, C_ML:C_ML + 512], w=[PV])
        else:
            p.dma(pv[b][:], src[r0 - 1:r0 + 63, C_ML:C_ML + 512], w=[PV])
        if c == last_c:
            p.dve(lambda e, b=b: e.memset(nx[b][:], 0.0), w=[NX])
            p.dma(nx[b][0:63, :], src[r0 + 1:r0 + 64, C_ML:C_ML + 512], w=[NX])
        else:
            p.dma(nx[b][:], src[r0 + 1:r0 + 65, C_ML:C_ML + 512], w=[NX])
        p.dma(gt[b][:], src[r0:r0 + 64, C_GAT:C_GAT + 8], w=[GT])
        epi = mypos[(kind, c)] > otpos[(kind, c)]
        p.dve(lambda e, b=b: e.tensor_tensor(ta[:], pv[b][:], convw[:, 0, :], ALU.mult), r=[PV, 'convw'], w=['ta'])
        p.dve(lambda e, b=b: e.tensor_tensor(tb[:], mn[b][:, 0:512], convw[:, 1, :], ALU.mult), r=[MN, 'convw'], w=['tb'])
        p.dve(lambda e: e.tensor_tensor(ta[:], ta[:], tb[:], ALU.add), r=['ta', 'tb'], w=['ta'])
        p.dve(lambda e, b=b: e.tensor_tensor(tb[:], nx[b][:], convw[:, 2, :], ALU.mult), r=[NX, 'convw', 'ta'], w=['tb'])
        p.dve(lambda e: e.tensor_tensor(ta[:], ta[:], tb[:], ALU.add), r=['ta', 'tb'], w=['ta'])
        p.dve(lambda e: e.tensor_tensor(ta[:], ta[:], convb[:], ALU.add), r=['ta', 'convb'], w=['ta'])
        p.act(lambda e: e.activation(sl[:], ta[:], AF.Silu), r=['ta'], w=['sl'])
        p.dve(lambda e: e.tensor_scalar(qs[:], sl[:, 0:256], 128.0 ** -0.5, None, ALU.mult), r=['sl'], w=['qs'])
        p.act(lambda e: e.copy(ks[:], sl[:, 256:512]), r=['sl'], w=['ks'])
        p.act(lambda e, b=b: e.copy(v1[:, :, 0:128], mn[b][:, 512:768].rearrange("p (h v) -> p h v", h=2)), r=[MN], w=['v1'])
        for i in range(4):
            srcT = qs if i < 2 else ks
            p.pe(lambda e, i=i, srcT=srcT: e.transpose(psT[:, i, :], srcT[:, (i % 2) * 128:(i % 2) * 128 + 128], id64), r=['qs', 'ks', 'cst'], w=['bB'])
        p.act(lambda e: e.copy(qkT[:], psT[:]), r=[], w=['qkT', 'bB'])
        p.dve(lambda e, b=b: e.tensor_tensor(g[:], gt[b][:], bg[:], ALU.add), r=[GT, 'bg'], w=['g'])
        ic = g[:, 4 * d:4 * d + 2]
        fp = g[:, 4 * d + 2:4 * d + 4]
        p.act(lambda e, fp=fp: e.activation(C(0), fp, AF.Exp, scale=-1.0), r=['g'], w=['sm'])
        p.act(lambda e: e.activation(C(2), C(0), AF.Ln, bias=1.0), r=['sm'], w=['sm'])
        p.pe(lambda e: e.matmul(Tg[0:64, 0:2], TriM, C(2), start=True, stop=True), r=['sm', 'cst'], w=['bA'])
        p.pe(lambda e: e.matmul(Tg[:, 8:10], nones[:], C(2), start=True, stop=True), r=['sm', 'nones'], w=['bA'])
        p.dve(lambda e: e.tensor_copy(C(4), Tg[0:64, 0:2]), r=[], w=['sm', 'bA'])
        p.dve(lambda e: e.tensor_copy(C8(0), Tg[:, 8:10]), r=[], w=['sm128', 'bA'])
        p.dve(lambda e, ic=ic: e.tensor_tensor(C(6), ic, C(4), ALU.subtract), r=['g', 'sm'], w=['sm'])
        for h in range(2):
            p.dve(lambda e, h=h: e.tensor_scalar(diag[h][:], id64, sm[:, 6 + h:7 + h], None, ALU.mult), r=['cst', 'sm'], w=['diag%d' % h])
            p.pe(lambda e, h=h: e.matmul(Rps[:, h, :], ones[:], diag[h][:], start=True, stop=True), r=['ones', 'diag%d' % h], w=['bA'])
        for h in range(2):
            p.dve(lambda e, h=h: e.scalar_tensor_tensor(Dm[:, h, :], Rps[0:64, h, :], sm[:, 4 + h:5 + h], maskb, ALU.add, ALU.add), r=['sm', 'cst'], w=['D', 'bA'])
        p.dve(lambda e: e.reduce_max(C(8), Dm[:], AX.X), r=['D'], w=['sm'])
        p.dve(lambda e: e.tensor_tensor(C(10), C(4), mm[0:64, :], ALU.add), r=['sm', 'mm'], w=['sm'])
        p.dve(lambda e: e.tensor_tensor(C(12), C(10), C(8), ALU.max), r=['sm'], w=['sm'])
        p.dve(lambda e: e.tensor_scalar(C(14), C(12), -1.0, None, ALU.mult), r=['sm'], w=['sm'])
        p.dve(lambda e: e.tensor_tensor(C(16), C(10), C(12), ALU.subtract), r=['sm'], w=['sm'])
        p.act(lambda e: e.activation(C(18), C(16), AF.Exp), r=['sm'], w=['sm'])
        p.act(lambda e: e.activation(C(20), C(14), AF.Exp), r=['sm'], w=['sm'])
        for h in range(2):
            p.act(lambda e, h=h: e.activation(Ew[:, h, :], Dm[:, h, :], AF.Exp, bias=sm[:, 14 + h:15 + h]), r=['D', 'sm'], w=['Ew'])
            p.pe(lambda e, h=h: e.matmul(Sps[:, h, :], qkT[:, h, :], qkT[:, 2 + h, :], start=True, stop=True), r=['qkT'], w=['bA'])
        p.dve(lambda e: e.memset(C(22), 0.0), r=['sm'], w=['sm'])
        for h in range(2):
            p.dve(lambda e, h=h: e.scalar_tensor_tensor(qkE[:, h, :], Sps[:, h, :], 1.0, Ew[:, h, :], ALU.mult, ALU.mult, accum_out=sm[:, 22 + h:23 + h]),
                  r=['Ew', 'sm'], w=['qkE', 'sm', 'bA'])
        for h in range(2):
            p.pe(lambda e, h=h: e.transpose(psE[:, h, :], qkE[:, h, :], id64), r=['qkE', 'cst'], w=['bA'])
        p.act(lambda e: e.copy(qkET[:], psE[:]), r=[], w=['qkET', 'bA'])
        for h in range(2):
            p.pe(lambda e, h=h: e.matmul(nps[:, h, :], qkET[:, h, :], v1[:, h, 0:128], start=True, stop=True), r=['qkET', 'v1'], w=['bB'])
            p.pe(lambda e, h=h: e.matmul(ips[:, h, :], qkT[:, h, :], CNbf[h][:], start=True, stop=True), r=['qkT', 'CNbf%d' % h], w=['bD'])
        for h in range(2):
            p.dve(lambda e, h=h: e.scalar_tensor_tensor(sm[:, 24 + h:25 + h], ips[:, h, 128:129], sm[:, 18 + h:19 + h], sm[:, 22 + h:23 + h], ALU.mult, ALU.add),
                  r=['sm'], w=['sm', 'bD'])
        p.dve(lambda e: e.tensor_scalar(C(32), C(24), -1.0, None, ALU.mult), r=['sm'], w=['sm'])
        p.dve(lambda e: e.tensor_tensor(C(24), C(24), C(32), ALU.max), r=['sm'], w=['sm'])
        p.dve(lambda e: e.tensor_tensor(C(24), C(24), C(20), ALU.max), r=['sm'], w=['sm'])
        p.dve(lambda e: e.reciprocal(C(26), C(24)), r=['sm'], w=['sm'])
        p.dve(lambda e: e.tensor_tensor(C(28), C(18), C(26), ALU.mult), r=['sm'], w=['sm'])
        for h in range(2):
            p.act(lambda e, h=h: e.activation(ins_[:, h, :], ips[:, h, 0:128], AF.Identity, scale=sm[:, 28 + h:29 + h]), r=['sm'], w=['ins', 'bD'])
            p.dve(lambda e, h=h: e.scalar_tensor_tensor(hout[:, h * 128:(h + 1) * 128], nps[:, h, :], sm[:, 26 + h:27 + h], ins_[:, h, :], ALU.mult, ALU.add),
                  r=['sm', 'ins'], w=['hout', 'bB'])
        p.dve(lambda e: e.reduce_max(C8(2), Rps[:], AX.X), r=[], w=['sm128', 'bA'])
        p.dve(lambda e: e.tensor_tensor(C8(4), C8(2), C8(0), ALU.add), r=['sm128'], w=['sm128'])
        p.dve(lambda e: e.tensor_tensor(C8(6), C8(0), mm[:], ALU.add), r=['sm128', 'mm'], w=['sm128'])
        p.dve(lambda e: e.tensor_tensor(C8(8), C8(6), C8(4), ALU.max), r=['sm128'], w=['sm128'])
        p.dve(lambda e: e.tensor_tensor(C8(10), C8(6), C8(8), ALU.subtract), r=['sm128'], w=['sm128'])
        p.act(lambda e: e.activation(C8(12), C8(10), AF.Exp), r=['sm128'], w=['sm128'])
        p.dve(lambda e: e.tensor_tensor(C8(14), C8(0), C8(8), ALU.subtract), r=['sm128'], w=['sm128'])
        p.dve(lambda e: e.tensor_tensor(C(30), C(6), sm128[0:64, 14:16], ALU.add), r=['sm', 'sm128'], w=['sm'])
        p.act(lambda e: e.activation(C(30), C(30), AF.Exp), r=['sm'], w=['sm'])
        for h in range(2):
            p.dve(lambda e, h=h: e.tensor_scalar(kw[:, h, :], ks[:, h * 128:(h + 1) * 128], sm[:, 30 + h:31 + h], None, ALU.mult), r=['ks', 'sm'], w=['kw'])
        for h in range(2):
            p.pe(lambda e, h=h: e.matmul(ups1, kw[:, h, :], v1[:, h, :], start=True, stop=True), r=['kw', 'v1'], w=['bD'])
            p.dve(lambda e, h=h: e.scalar_tensor_tensor(CN32[h][:], CN32[h][:], sm128[:, 12 + h:13 + h], ups1, ALU.mult, ALU.add),
                  r=['CN32%d' % h, 'sm128'], w=['CN32%d' % h, 'bD'])
            p.act(lambda e, h=h: e.copy(CNbf[h][:], CN32[h][:]), r=['CN32%d' % h], w=['CNbf%d' % h])
        p.dve(lambda e: e.tensor_copy(mm[:], C8(8)), r=['sm128'], w=['mm'])
        if not epi:
            p.dma(k.stash[srow:srow + 64, own0:own0 + 256], hout[:], r=['hout'], w=[('glob', 'st', 'l', srow)])
        else:
            p.dma(hf[b][:], k.stash[srow:srow + 64, oth0:oth0 + 256], r=[('glob', 'st', 'l', srow)], w=['hf%d' % b])
            p.dve(lambda e, b=b: e.tensor_tensor(hout[:], hout[:], hf[b][:], ALU.add), r=['hout', 'hf%d' % b], w=['hout'])
            p.act(lambda e, b=b: e.activation(so[:], mn[b][:, 1024:1280], AF.Sigmoid), r=[MN], w=['so'])
            p.act(lambda e, b=b: e.activation(sz[:], mn[b][:, 768:1024], AF.Silu), r=[MN], w=['sz'])
            p.dve(lambda e: e.tensor_tensor(so[:], so[:], sz[:], ALU.mult), r=['so', 'sz'], w=['so'])
            p.dve(lambda e: e.tensor_tensor(yb[:], hout[:], so[:], ALU.mult), r=['hout', 'so'], w=['yb'])
            ydst, ykey = yrows(k, kind, c)
            p.dma(ydst[:, 256:512], yb[:], r=['yb'], w=[('glob',) + ykey + ('b',)])


def phase_na(k, l):
    for pair in ((0, 1), (2, 3)):
        s = Scope(k.p)
        k.p.interleave([lambda n=n: na_head(k, l, n, s) for n in pair])
        s.end()
    dbg_y(k, l, 512, 1024)


def na_head(k, l, n, s):
    p = k.p
    SC = 128.0 ** -0.5
    idb = s.sb('c_idb', [128, 128], BF16)
    QKT = s.sb('c_QKT', [128, 2, L + LC], BF16)
    V = s.sb('c_V', [128, 34, 128], BF16)
    Vt32 = s.sb('c_Vt32', [128, 5, 128], F32)
    Vt = s.sb('c_Vt', [128, 5, 128], BF16)
    BT = s.sb('c_BT', [128, 5, 576], F32)
    qkv = [s.sb('c_qkv%d' % i, [128, 3, 128], F32) for i in range(2)]
    idf = s.sb('c_idf', [128, 128], F32)
    zt = [s.sb('c_zt%d' % i, [128, 128], F32) for i in range(2)]
    sm = s.sb('c_sm', [128, 832], F32)
    Pm = s.sb('c_Pm', [128, 832], BF16)
    PT = s.sb('c_PT', [128, 7, 128], BF16)
    st = s.sb('c_st', [128, 4], F32)
    sz = s.sb('c_sz', [128, 128], F32)
    yo = s.sb('c_yo', [128, 128], BF16)
    Sa = s.ps('c_Sa', [128, 512], F32)
    Sb = s.ps('c_Sb', [128, 512], F32)
    psP = s.ps('c_psP', [128, 7, 128], BF16)
    bO = s.ps('c_bO', [128, 512], F32)
    O = bO[:, 0:128]
    psT = bO[:, 128:384].rearrange("p (a b) -> p a b", a=2)

    p.dma(idb[:], k.ident, w=['idb'], eng='pool')
    p.dma(idf[:], k.ident, w=['idf'])
    p.dma(BT[:], k.natab[l, n].rearrange("t q c -> q t c"), w=['BT'])
    vcol = C_NA + 1024 + n * 128
    zcol = C_NA + 1536 + n * 128
    p.dma(Vt32[:, 0:4, :], k.P_lat[3520:4032, vcol:vcol + 128].rearrange("(t q) c -> q t c", q=128), w=['Vt32'])
    p.dma(Vt32[0:64, 4, :], k.P_lat[4032:4096, vcol:vcol + 128], w=['Vt32'])
    p.dve(lambda e: e.tensor_copy(Vt[:, 0:4, :], Vt32[:, 0:4, :]), r=['Vt32'], w=['Vt'])
    p.dve(lambda e: e.tensor_copy(Vt[0:64, 4, :], Vt32[0:64, 4, :]), r=['Vt32', 'Vt'], w=['Vt'])
    for i in range(34):
        b = i % 2
        src = k.P_lat[i * 128:(i + 1) * 128, :] if i < 32 else k.P_ctx[(i - 32) * 128:(i - 31) * 128, :]
        v3 = src[:, C_NA:C_NA + 1536].rearrange("q (a hh c) -> q a hh c", a=3, hh=4)[:, :, n, :]
        p.dma(qkv[b][:], v3, w=['qkv%d' % b])
        p.act(lambda e, b=b, i=i: e.copy(V[:, i, :], qkv[b][:, 2, :]), r=['qkv%d' % b], w=['V'])
        for a in range(2):
            p.pe(lambda e, a=a, b=b: e.transpose(psT[:, a, :], qkv[b][:, a, :], idf[:]), r=['qkv%d' % b, 'idf'], w=['O'])
        p.act(lambda e, i=i: e.copy(QKT[:, :, i * 128:(i + 1) * 128], psT[:]), r=[], w=['QKT', 'O'])

    ntile = 34 if l == 0 else 32
    for it, rp in enumerate(range(ntile)):
        b = it % 2
        lat = rp < 32
        q_ap = QKT[:, 0, rp * 128:(rp + 1) * 128]
        if lat:
            ti = {0: 0, 1: 1, 30: 3, 31: 4}.get(rp, 2)
            rs_lo = min(max(2 * rp - 4, 0), 55)
            ks0 = rs_lo * 64
            p.dma(zt[b][:], k.P_lat[rp * 128:(rp + 1) * 128, zcol:zcol + 128], w=['zt%d' % b])
            p.pe(lambda e, q_ap=q_ap, ks0=ks0: e.matmul(Sa[:], q_ap, QKT[:, 1, ks0:ks0 + 512], start=True, stop=True), r=['QKT'], w=['Sa'])
            p.pe(lambda e, q_ap=q_ap, ks0=ks0: e.matmul(Sb[:, 0:64], q_ap, QKT[:, 1, ks0 + 512:ks0 + 576], start=True, stop=True), r=['QKT'], w=['Sb'])
            p.pe(lambda e, q_ap=q_ap: e.matmul(Sb[:, 64:320], q_ap, QKT[:, 1, L:L + LC], start=True, stop=True), r=['QKT'], w=['Sb'])
            p.dve(lambda e, ti=ti: e.scalar_tensor_tensor(sm[:, 0:512], Sa[:], SC, BT[:, ti, 0:512], ALU.mult, ALU.add), r=['BT'], w=['sm', 'Sa'])
            p.dve(lambda e, ti=ti: e.scalar_tensor_tensor(sm[:, 512:576], Sb[:, 0:64], SC, BT[:, ti, 512:576], ALU.mult, ALU.add), r=['BT'], w=['sm', 'Sb'])
            p.act(lambda e: e.activation(sm[:, 576:832], Sb[:, 64:320], AF.Copy, scale=SC), r=[], w=['sm', 'Sb'])
            nk = 832
        else:
            cq = rp - 32
            p.dma(zt[b][:], k.P_ctx[cq * 128:(cq + 1) * 128, zcol:zcol + 128], w=['zt%d' % b])
            p.pe(lambda e, q_ap=q_ap: e.matmul(Sb[:, 64:320], q_ap, QKT[:, 1, L:L + LC], start=True, stop=True), r=['QKT'], w=['Sb'])
            p.act(lambda e: e.activation(sm[:, 0:256], Sb[:, 64:320], AF.Copy, scale=SC), r=[], w=['sm', 'Sb'])
            nk = 256
        p.dve(lambda e, nk=nk: e.reduce_max(st[:, 0:1], sm[:, 0:nk], AX.X), r=['sm'], w=['st'])
        p.dve(lambda e: e.tensor_scalar(st[:, 1:2], st[:, 0:1], -1.0, None, ALU.mult), r=['st'], w=['st'])
        p.dve(lambda e: e.memset(st[:, 2:3], 0.0), r=['st'], w=['st'])
        p.act(lambda e, nk=nk: e.activation(Pm[:, 0:nk], sm[:, 0:nk], AF.Exp, bias=st[:, 1:2], accum_out=st[:, 2:3]), r=['sm', 'st'], w=['Pm', 'st'])
        p.dve(lambda e: e.reciprocal(st[:, 3:4], st[:, 2:3]), r=['st'], w=['st'])
        if lat:
            blocks = [(i, i * 128, 128) for i in range(4)] + [(4, 512, 64), (5, 576, 128), (6, 704, 128)]
        else:
            blocks = [(5, 0, 128), (6, 128, 128)]
        for (bi, c0, w_) in blocks:
            p.pe(lambda e, bi=bi, c0=c0, w_=w_: e.transpose(psP[0:w_, bi, :], Pm[:, c0:c0 + w_], idb[:]), r=['Pm', 'idb'], w=['psP'])
        if lat:
            p.act(lambda e: e.copy(PT[:, 0:4, :], psP[:, 0:4, :]), r=[], w=['PT', 'psP'])
            p.dve(lambda e: e.tensor_copy(PT[0:64, 4, :], psP[0:64, 4, :]), r=[], w=['PT', 'psP'])
        p.dve(lambda e: e.tensor_copy(PT[:, 5:7, :], psP[:, 5:7, :]), r=[], w=['PT', 'psP'])
        if lat:
            aligned = (ks0 % 128 == 0)
            for i in range(4):
                rhs = V[:, ks0 // 128 + i, :] if aligned else Vt[:, i, :]
                p.pe(lambda e, i=i, rhs=rhs: e.matmul(O[:], PT[:, i, :], rhs, start=(i == 0), stop=False), r=['PT', 'V', 'Vt'], w=['O'])
            rhs = V[0:64, ks0 // 128 + 4, :] if aligned else Vt[0:64, 4, :]
            p.pe(lambda e, rhs=rhs: e.matmul(O[:], PT[0:64, 4, :], rhs, start=False, stop=False), r=['PT', 'V', 'Vt'], w=['O'])
        for i in (5, 6):
            p.pe(lambda e, i=i, lat=lat: e.matmul(O[:], PT[:, i, :], V[:, 32 + (i - 5), :], start=((not lat) and i == 5), stop=(i == 6)), r=['PT', 'V'], w=['O'])
        p.act(lambda e, b=b: e.activation(sz[:], zt[b][:], AF.Silu), r=['zt%d' % b], w=['sz'])
        p.dve(lambda e: e.scalar_tensor_tensor(yo[:], O[:], st[:, 3:4], sz[:], ALU.mult, ALU.mult), r=['st', 'sz'], w=['yo', 'O'])
        if lat:
            ydst = k.y_loc[rp % 8][(rp // 8) * 128:(rp // 8) * 128 + 128, 512 + n * 128:512 + (n + 1) * 128]
        else:
            ydst = k.y_loc[8][(rp - 32) * 128:(rp - 31) * 128, 512 + n * 128:512 + (n + 1) * 128]
        p.dma(ydst, yo[:], r=['yo'], w=[('glob', 'yc', rp, n)])


def transpose_tiles(k, s, loader, XT, ntiles, tag):
    p = k.p
    xt = [s.sb('tt_x%s%d' % (tag, i), [128, D], BF16) for i in range(2)]
    idb = s.sb('tt_idb' + tag, [128, 128], BF16)
    pst = [s.ps('tt_ps%s%d' % (tag, i), [128, 8, 128], BF16) for i in range(2)]
    p.dma(idb[:], k.ident, w=['tt_idb'], eng='pool')
    for t in range(ntiles):
        rows = 128 if t < 8 else 64
        b = t % 2
        loader(t, xt[b], 'tt_x%d' % b)
        for g in range(4):
            pb = (t * 4 + g) % 2
            for i in range(8):
                kk = g * 8 + i
                p.pe(lambda e, pb=pb, i=i, kk=kk, rows=rows, b=b: e.transpose(pst[pb][:, i, :rows], xt[b][:rows, kk * 128:(kk + 1) * 128], idb[:rows, :rows]),
                     r=['tt_x%d' % b, 'tt_idb'], w=['tt_ps%d' % pb])
            if g % 2 == 0:
                p.act(lambda e, pb=pb, g=g, t=t, rows=rows: e.copy(XT[:, g * 8:(g + 1) * 8, t * 128:t * 128 + rows], pst[pb][:, :, :rows]), r=[], w=['XT' + tag, 'tt_ps%d' % pb])
            else:
                p.dve(lambda e, pb=pb, g=g, t=t, rows=rows: e.tensor_copy(XT[:, g * 8:(g + 1) * 8, t * 128:t * 128 + rows], pst[pb][:, :, :rows]), r=[], w=['XT' + tag, 'tt_ps%d' % pb])


def phase_b(k, l, last):
    p = k.p
    ntiles = 8 if last else NT
    for t in range(NT):
        allgather(k, k.y_loc[t], k.y_all[t], [], [('y_all', t)])
    p.flush()

    s = Scope(p)
    yTs = s.sb('b_yTs', [128, 32, NT * 128], BF16)

    def load_y(t, dst, key):
        rows = 128 if t < 8 else 64
        view = k.y_all[t].rearrange("(r j i) c -> j i r c", r=4, j=4)

        def fn(e, view=view, dst=dst, rows=rows):
            if 'rank' not in p.body_cache:
                p.body_cache['rank'] = e.partition_id() % 4
            rank = p.body_cache['rank']
            return e.dma_start(out=dst[:rows, :].rearrange("q (r c) -> q r c", r=4), in_=view[rank])
        p.add('sp', fn, [], [key], dma=True)

    transpose_tiles(k, s, load_y, yTs, ntiles, 'y')
    wp = [s.sb('b_wp%d' % i, [128, 16, 512], BF16) for i in range(2)]
    ub = s.sb('b_ub', [128, NT, 512], F32)
    ubf = [s.sb('b_ubf%d' % i, [128, 512], BF16) for i in range(2)]
    Gl = [s.sb('b_Gl%d' % i, [128, 512], BF16) for i in range(3)]
    tmp = s.sb('b_tmp', [128, 512], F32)
    ps = [s.ps('b_pp%d' % i, [128, 512], F32) for i in range(4)]
    wsrc = (k.w_pa, k.w_pb, k.w_pc)
    kmap = ([((hd // 2) * 8 + hd % 2, hd) for hd in range(8)],
            [((hd // 2) * 8 + 2 + hd % 2, hd) for hd in range(8)],
            [((n // 4) * 8 + 4 + n % 4, n) for n in range(16)])
    it = 0
    wi = 0
    for cb in range(8):
        for kb in range(3):
            nk = len(kmap[kb])
            w_ = wi % 2
            wi += 1
            p.dma(wp[w_][:, 0:nk, :], wsrc[kb][l][:, cb * 512:(cb + 1) * 512].rearrange("(kk pp) c -> pp kk c", pp=128), w=['pw%d' % w_], eng='pool')
            for t in range(ntiles):
                rows = 128 if t < 8 else 64
                pi, gi = it % 4, it % 3
                it += 1
                g0 = ((t * 3 + kb) * 8 + cb) * 128
                p.dma(Gl[gi][:rows, :], k.G_scr[g0:g0 + rows, :], w=['Gl%d' % gi])
                for qi, (kk, wr) in enumerate(kmap[kb]):
                    p.pe(lambda e, pi=pi, w_=w_, kk=kk, wr=wr, t=t, rows=rows, qi=qi, nk=nk: e.matmul(ps[pi][:rows, :], yTs[:, kk, t * 128:t * 128 + rows], wp[w_][:, wr, :], start=(qi == 0), stop=(qi == nk - 1)),
                         r=['XTy', 'pw%d' % w_], w=['pps%d' % pi])
                if kb == 0:
                    p.dve(lambda e, pi=pi, gi=gi, t=t, rows=rows: e.tensor_tensor(ub[:rows, t, :], ps[pi][:rows, :], Gl[gi][:rows, :], ALU.mult), r=['Gl%d' % gi], w=[('ub', t), 'pps%d' % pi])
                else:
                    p.dve(lambda e, pi=pi, gi=gi, rows=rows: e.tensor_tensor(tmp[:rows, :], ps[pi][:rows, :], Gl[gi][:rows, :], ALU.mult), r=['Gl%d' % gi], w=['btmp', 'pps%d' % pi])
                    if kb == 1:
                        p.dve(lambda e, t=t, rows=rows: e.tensor_tensor(ub[:rows, t, :], ub[:rows, t, :], tmp[:rows, :], ALU.add), r=['btmp', ('ub', t)], w=[('ub', t)])
                    else:
                        ui = t % 2
                        p.dve(lambda e, t=t, rows=rows, ui=ui: e.tensor_tensor(ubf[ui][:rows, :], ub[:rows, t, :], tmp[:rows, :], ALU.add), r=['btmp', ('ub', t)], w=['ubf%d' % ui])
                        p.dma(k.u_scr[t * 128:t * 128 + rows, cb * 512:(cb + 1) * 512], ubf[ui][:rows, :], r=['ubf%d' % ui], w=[('u', t, cb)])
    s.end()

    s = Scope(p)
    uTs = s.sb('b_uTs', [128, 32, NT * 128], BF16)

    def load_u(t, dst, key):
        rows = 128 if t < 8 else 64
        p.dma(dst[:rows, :], k.u_scr[t * 128:t * 128 + rows, :], w=[key])

    transpose_tiles(k, s, load_u, uTs, ntiles, 'u')
    wo = [s.sb('b_wo%d' % i, [128, 32, 512], BF16) for i in range(2)]
    gate = [s.sb('b_gate%d' % m, [128, D], F32) for m in range(2)]
    xin = [s.sb('b_xin%d' % i, [128, 512], F32) for i in range(3)]
    xo = [s.sb('b_xo%d' % i, [128, 512], F32) for i in range(3)]
    ps = [s.ps('b_po%d' % i, [128, 512], F32) for i in range(4)]
    for m in range(2):
        p.dma(gate[m][:].rearrange("q (r c) -> q r c", r=4), modvec_bc(k, l, m, 2), w=['gate%d' % m])
    xsrc = k.x_tok if l == 0 else k.x_loc
    it = 0
    for cb in range(8):
        w_ = cb % 2
        p.dma(wo[w_][:], k.w_out[l][:, cb * 512:(cb + 1) * 512].rearrange("(kk pp) c -> pp kk c", pp=128), w=['ow%d' % w_], eng='pool')
        for t in range(ntiles):
            rows = 128 if t < 8 else 64
            m = 0 if t < 8 else 1
            pi, xi = it % 4, it % 3
            it += 1
            p.dma(xin[xi][:rows, :], xsrc[t * 128:t * 128 + rows, cb * 512:(cb + 1) * 512], r=[('x', t, cb)], w=['xin%d' % xi])
            for kk in range(32):
                p.pe(lambda e, pi=pi, w_=w_, kk=kk, t=t, rows=rows: e.matmul(ps[pi][:rows, :], uTs[:, kk, t * 128:t * 128 + rows], wo[w_][:, kk, :], start=(kk == 0), stop=(kk == 31)),
                     r=['XTu', 'ow%d' % w_], w=['ops%d' % pi])
            p.dve(lambda e, pi=pi, xi=xi, rows=rows, m=m, cb=cb: e.tensor_tensor(xo[xi][:rows, :], ps[pi][:rows, :], gate[m][:rows, cb * 512:(cb + 1) * 512], ALU.mult),
                  r=['gate%d' % m], w=['xo%d' % xi, 'ops%d' % pi])
            p.dve(lambda e, xi=xi, rows=rows: e.tensor_tensor(xo[xi][:rows, :], xo[xi][:rows, :], xin[xi][:rows, :], ALU.add), r=['xo%d' % xi, 'xin%d' % xi], w=['xo%d' % xi])
            p.dma(k.x_loc[t * 128:t * 128 + rows, cb * 512:(cb + 1) * 512], xo[xi][:rows, :], r=['xo%d' % xi], w=[('x', t, cb)])
    s.end()


def phase_final(k):
    p = k.p
    s = Scope(p)
    fg = s.sb('f_g', [128, D], F32)
    xt = [s.sb('f_x%d' % i, [128, D], F32) for i in range(2)]
    xo = [s.sb('f_o%d' % i, [128, D], F32) for i in range(2)]
    junk = s.sb('f_junk', [128, D], BF16)
    st = s.sb('f_st', [128, 4], F32)
    p.dma(fg[:], k.final_g.partition_broadcast(128).rearrange("q a d -> q (a d)"), w=['fg'])
    for t in range(8):
        b = t % 2
        p.dma(xt[b][:], k.x_loc[t * 128:(t + 1) * 128, :], w=['fx%d' % b])
        p.dve(lambda e: e.memset(st[:], 0.0), w=['fst'])
        p.act(lambda e, b=b: e.activation(junk[:], xt[b][:], AF.Square, accum_out=st[:, 0:1]), r=['fx%d' % b, 'fst'], w=['fjunk', 'fst'])
        p.dve(lambda e: e.tensor_scalar(st[:, 1:2], st[:, 0:1], 1.0 / D, EPS, ALU.mult, ALU.add), r=['fst'], w=['fst'])
        p.act(lambda e: e.activation(st[:, 1:2], st[:, 1:2], AF.Sqrt), r=['fst'], w=['fst'])
        p.dve(lambda e: e.reciprocal(st[:, 2:3], st[:, 1:2]), r=['fst'], w=['fst'])
        p.dve(lambda e, b=b: e.scalar_tensor_tensor(xo[b][:], xt[b][:], st[:, 2:3], fg[:], ALU.mult, ALU.mult), r=['fx%d' % b, 'fst', 'fg'], w=['fo%d' % b])
        p.dma(k.out[t * 128:(t + 1) * 128, :], xo[b][:], r=['fo%d' % b], w=[('out', t)])
    s.end()


def kernel(**inputs):
    inp = {kk: np.asarray(v) for kk, v in inputs.items()}
    maps = prep(inp)
    nc = build_nc()
    maps = [{kk: m[kk] for kk in nc.declared_inputs} for m in maps]
    res = run_bass_kernel_spmd(nc, maps, core_ids=list(range(8)))
    out = np.zeros((2, L, D), np.float32)
    for i in range(8):
        b, j = i // 4, i % 4
        out[b, j * 1024:(j + 1) * 1024] = res.results[i]['out']
    return out
```

```python
from contextlib import ExitStack
import numpy as np
import ml_dtypes
import concourse.bass as bass
import concourse.mybir as mybir
from concourse.bass_utils import run_bass_kernel_spmd

F32 = mybir.dt.float32
BF16 = mybir.dt.bfloat16
AF = mybir.ActivationFunctionType
ALU = mybir.AluOpType
AX = mybir.AxisListType

D = 4096
L = 4096
LC = 256
NT = 9
NTOK = 1088
EPS = 1e-6
D_IN = 16448
NCOL = 4136
C_GLA, C_ML, C_NA, C_DEC, C_GAT = 0, 768, 2048, 4096, 4128
NEG = -30000.0
GROUPS = [[0, 1, 2, 3], [4, 5, 6, 7]]

ENGS = ('pe', 'dve', 'act', 'pool', 'sp')
ENGOBJ = {'pe': 'tensor', 'dve': 'vector', 'act': 'scalar', 'pool': 'gpsimd', 'sp': 'sync'}
NDMASEM = 8
SEM_ROLL = 30000
import os
SAME_ENGINE_SYNC = not os.environ.get('NO_SES')


class Op:
    __slots__ = ('eng', 'fn', 'deps', 'signal', 'tok', 'is_dma', 'dsem', 'prev_dma', 'inc')

    def __init__(self, eng, fn, is_dma, inc):
        self.eng, self.fn, self.is_dma, self.inc = eng, fn, is_dma, inc
        self.deps = []
        self.signal = False
        self.tok = None
        self.dsem = None
        self.prev_dma = None


class Prog:
    def __init__(self, nc):
        self.nc = nc
        self.q = {e: [] for e in ENGS}
        self.state = {}
        self.ndma = {e: 0 for e in ENGS}
        self.dma_hist = {e: [] for e in ENGS}
        self.es = ExitStack()
        self.esem = {e: [] for e in ENGS}
        self.ecnt = {e: SEM_ROLL for e in ENGS}
        self.dsem = {}
        self.dcnt = {}
        self.seen = {e: {} for e in ENGS}
        self.last = {e: None for e in ENGS}
        self.nops = 0
        self.rec = None
        self.rec_prefix = None

    def sem(self, name):
        return self.es.enter_context(self.nc.semaphore(name))

    def add(self, eng, fn, reads=(), writes=(), dma=False, inc=16, semq=None):
        if self.rec is not None:
            pf = self.rec_prefix
            fix = lambda kk: kk if (isinstance(kk, tuple) and kk and kk[0] == 'glob') else (pf, kk)
            self.rec.append((eng, fn, tuple(fix(x) for x in reads), tuple(fix(x) for x in writes), dma, inc, semq))
            return None
        for kk in reads:
            if isinstance(kk, tuple) and kk and kk[0] == 'glob' and kk[1] == 'st':
                assert kk in self.state, ('stash read before write', kk)
        op = Op(eng, fn, dma, inc)
        deps = []
        for k in reads:
            st = self.state.get(k)
            if st is not None:
                deps.extend(st[0])
        for k in writes:
            st = self.state.get(k)
            if st is not None:
                deps.extend(st[0])
                deps.extend(st[1])
        seen = set()
        for d in deps:
            if id(d) in seen:
                continue
            seen.add(id(d))
            if (not d.is_dma) and d.eng == eng and (eng == 'pe' or not SAME_ENGINE_SYNC):
                continue
            op.deps.append(d)
            d.signal = True
        for k in reads:
            st = self.state.setdefault(k, [[], []])
            if not dma:
                st[1] = [r for r in st[1] if r.is_dma or r.eng != eng]
            st[1].append(op)
        for k in writes:
            self.state[k] = [[op], []]
        if dma:
            sq = semq or eng
            n = self.ndma.setdefault(sq, 0)
            self.ndma[sq] = n + 1
            op.dsem = ('dma_' + sq, n % NDMASEM)
            hist = self.dma_hist.setdefault(sq, [])
            if n >= NDMASEM:
                op.prev_dma = hist[n - NDMASEM]
            hist.append(op)
        self.q[eng].append(op)
        self.nops += 1
        return op

    def pe(self, fn, r=(), w=()):
        return self.add('pe', fn, r, w)

    def dve(self, fn, r=(), w=()):
        return self.add('dve', fn, r, w)

    def act(self, fn, r=(), w=()):
        return self.add('act', fn, r, w)

    def pool(self, fn, r=(), w=()):
        return self.add('pool', fn, r, w)

    def dma(self, out, in_, r=(), w=(), eng='sp'):
        return self.add(eng, lambda e: e.dma_start(out=out, in_=in_), r, w, dma=True)

    def flush(self):
        lasts = []
        for e in ENGS:
            ops = [o for o in self.q[e] if (not o.is_dma) and o.fn is not None]
            if ops:
                ops[-1].signal = True
                lasts.append(ops[-1])
        dmas = []
        for sq in self.dma_hist:
            hist = self.dma_hist[sq]
            dmas.extend(hist[-NDMASEM:])
        for e in ENGS:
            b = Op(e, None, False, 0)
            b.deps = [o for o in lasts if o.eng != e] + dmas
            self.q[e].append(b)
        nc = self.nc
        for e in ENGS:
            for op in self.q[e]:
                if op.fn is None:
                    continue
                if op.is_dma:
                    if op.dsem not in self.dsem:
                        self.dsem[op.dsem] = self.sem('d_%s_%d' % op.dsem)
                        self.dcnt[op.dsem] = 0
                    v = self.dcnt[op.dsem] + op.inc
                    self.dcnt[op.dsem] = v
                    op.tok = (op.dsem, v, self.dsem[op.dsem])
                elif op.signal:
                    if self.ecnt[e] >= SEM_ROLL:
                        self.esem[e].append(self.sem('s_%s_%d' % (e, len(self.esem[e]))))
                        self.ecnt[e] = 0
                    self.ecnt[e] += 1
                    op.tok = ((e, len(self.esem[e])), self.ecnt[e], self.esem[e][-1])
        with nc.Block() as block:
            for e in ENGS:
                ops = self.q[e]

                def body(eng, ops=ops, e=e):
                    self.body_cache = {}
                    seen = self.seen[e]
                    for op in ops:
                        deps = list(op.deps)
                        if op.prev_dma is not None:
                            deps.append(op.prev_dma)
                        for dd in deps:
                            key, val, sem = dd.tok
                            if seen.get(key, 0) >= val:
                                continue
                            seen[key] = val
                            eng.wait_ge(sem, val)
                        if op.fn is None:
                            continue
                        ins = op.fn(eng)
                        if op.tok is not None:
                            ins.then_inc(op.tok[2], op.inc if op.is_dma else 1)

                getattr(block, ENGOBJ[e])(body)
        self.q = {e: [] for e in ENGS}
        self.state = {}

    def interleave(self, fns):
        recs = []
        for i, fn in enumerate(fns):
            self.rec, self.rec_prefix = [], 'il%d' % i
            fn()
            recs.append(self.rec)
        self.rec = None
        n = max(len(r) for r in recs)
        for i in range(n):
            for r in recs:
                if i < len(r):
                    self.add(*r[i])

    def close(self):
        self.es.close()


class Scope:
    def __init__(self, p):
        self.p = p
        self.es = ExitStack()

    CNT = [0]

    def sb(self, name, shape, dt):
        Scope.CNT[0] += 1
        return self.es.enter_context(self.p.nc.sbuf_tensor('%s_%d' % (name, Scope.CNT[0]), list(shape), dt))

    def ps(self, name, shape, dt=F32):
        Scope.CNT[0] += 1
        full = 512 if dt == F32 else 1024
        t = self.es.enter_context(self.p.nc.psum_tensor('%s_%d' % (name, Scope.CNT[0]), [shape[0], full], dt))
        n = 1
        for d_ in shape[1:]:
            n *= d_
        v = t[:, 0:n]
        if len(shape) == 3:
            v = v.rearrange("p (a b) -> p a b", a=shape[1])
        return v

    def end(self):
        self.p.flush()
        self.es.close()


IN_SPLITS = (512, 512, 1024, 1024, 32, 1024, 1024, 1024, 1024, 1024, 32, 2048, 2048, 2048, 2048)
OFFS = np.concatenate([[0], np.cumsum(IN_SPLITS)])


def my_cols(j):
    A_q, A_k, A_v, A_z, A_d, B_q, B_k, B_v, B_z, B_o, B_g, C_q, C_k, C_v, C_z = OFFS[:15]
    cols = []
    hs = (2 * j, 2 * j + 1)
    for base in (A_q, A_k):
        for h in hs:
            cols.append(np.arange(64) + base + h * 64)
    for base in (A_v, A_z):
        for h in hs:
            cols.append(np.arange(128) + base + h * 128)
    for base in (B_q, B_k, B_v, B_z, B_o):
        for h in hs:
            cols.append(np.arange(128) + base + h * 128)
    for base in (C_q, C_k, C_v, C_z):
        for h in range(4 * j, 4 * j + 4):
            cols.append(np.arange(128) + base + h * 128)
    cols.append(np.arange(32) + A_d)
    for t in range(4):
        for h in hs:
            cols.append(np.array([B_g + t * 8 + h]))
    c = np.concatenate(cols)
    assert c.shape[0] == NCOL
    return c


def na_tables(rpb_l, j):
    out = np.full((4, 5, 128, 576), NEG, np.float32)
    reps = [0, 1, 2, 30, 31]
    cc = np.arange(64)
    cstart = np.clip(cc - 8, 0, 48)
    for ti, rp in enumerate(reps):
        rs_lo = int(np.clip(2 * rp - 4, 0, 55))
        for jj in range(2):
            r = 2 * rp + jj
            rs = int(np.clip(r - 4, 0, 56))
            for a in range(9):
                kr = rs_lo + a
                if kr < rs or kr >= rs + 8 or kr > 63:
                    continue
                dr = kr - r + 7
                for c in range(64):
                    cp = np.arange(cstart[c], cstart[c] + 16)
                    dc = cp - c + 15
                    for hh in range(4):
                        out[hh, ti, jj * 64 + c, a * 64 + cp] = rpb_l[4 * j + hh, dr, dc]
    return out


def rope_table():
    nf = 16
    inv = (10000.0 ** (-np.arange(nf, dtype=np.float32) / nf)).astype(np.float32)
    tab = np.zeros((64, 64, 2, 8, 16), np.float32)
    for c in range(64):
        ang_r = (np.float32(c) * inv).astype(np.float32)
        ang_c = (np.arange(64, dtype=np.float32)[:, None] * inv[None, :]).astype(np.float32)
        for b8 in range(8):
            if b8 % 2 == 0:
                tab[c, :, 0, b8, :] = np.cos(ang_r)[None, :]
                tab[c, :, 1, b8, :] = np.sin(ang_r)[None, :]
            else:
                tab[c, :, 0, b8, :] = np.cos(ang_c)
                tab[c, :, 1, b8, :] = np.sin(ang_c)
    return tab.reshape(64, 64, 256)


def const_tables():
    s = np.arange(64)[:, None]
    t = np.arange(64)[None, :]
    lo = (s <= t).astype(np.float32)
    up = (s >= t).astype(np.float32)
    c = np.zeros((64, 12, 64), np.float32)
    c[:, 0] = lo
    c[:, 1] = up
    c[:, 2] = lo * (-1.0 / 16)
    c[:, 3] = up * (-1.0 / 16)
    c[:, 4] = -1.0 / 16
    c[:, 5] = -lo
    c[:, 6] = -up
    c[:, 7] = np.where(t <= s, 0.0, NEG)
    c[:, 8] = np.where(t >= s, 0.0, NEG)
    c[:, 9] = np.eye(64, dtype=np.float32)
    c[:, 10] = 1.0
    c[:, 11] = -1.0
    return c


def prep(inp):
    f = np.float32
    x, c, ctx, c_ctx = inp['x'], inp['c'], inp['ctx'], inp['c_ctx']
    rope = rope_table()
    cst = const_tables()
    ident = np.eye(128, dtype=f)
    maps = []
    for i in range(8):
        b, j = i // 4, i % 4
        m = {}
        m['x_tok'] = np.ascontiguousarray(np.concatenate([x[b, j * 1024:(j + 1) * 1024], ctx[b, j * 64:(j + 1) * 64]], 0))
        c2 = np.stack([c[b], c_ctx], 0)
        m['c2T'] = np.ascontiguousarray(c2.reshape(2, 32, 128).transpose(2, 1, 0))
        mc = np.concatenate([np.arange(1024) + part * 4096 + j * 1024 for part in range(3)])
        m['w_mod_s'] = np.ascontiguousarray(inp['w_mod'][:, :, mc])
        m['b_mod_s'] = np.ascontiguousarray(np.repeat(inp['b_mod'][:, None, mc], 2, axis=1))
        m['norm_g'] = inp['norm_g']
        m['final_g'] = inp['final_g'].reshape(1, D)
        cols = my_cols(j)
        m['w_in_s'] = np.ascontiguousarray(inp['w_in'][:, :, cols])
        hc = np.concatenate([np.arange(64) + h * 64 for h in (2 * j, 2 * j + 1)])
        wal = np.zeros((2, 2, 17, 128), f)
        wal[:, :, :16, :] = inp['w_alpha2'][:, :, :, hc]
        wal[:, :, 16, :] = inp['b_alpha'][:, :, hc]
        m['w_al'] = wal
        m['gla_g'] = np.ascontiguousarray(inp['gla_norm_g'][:, 2 * j * 128:(2 * j + 2) * 128])
        m['b_gates_s'] = np.ascontiguousarray(inp['b_gates'][:, :, 2 * j:2 * j + 2].reshape(2, 8))
        qc = np.concatenate([np.arange(256) + 2 * j * 128, np.arange(256) + 1024 + 2 * j * 128])
        m['conv_w_s'] = np.ascontiguousarray(inp['conv_w'][:, :, qc])
        m['conv_b_s'] = np.ascontiguousarray(inp['conv_b'][:, qc])
        m['natab'] = np.stack([na_tables(inp['rpb'][l], j) for l in range(2)], 0)
        m['rope'] = rope
        m['cst'] = cst
        m['ident'] = ident
        m['w_merge'] = inp['w_merge']
        m['b_merge'] = inp['b_merge']
        m['w_pa'] = inp['w_proj_a']
        m['w_pb'] = inp['w_proj_b']
        m['w_pc'] = inp['w_proj_c']
        m['w_out'] = inp['w_out']
        maps.append(m)
    return maps


class K:
    def __getattr__(self, name):
        specs = self.__dict__.get('_specs', {})
        if name in specs:
            shape, dt = specs[name]
            ap = self.nc.dram_tensor(name, list(shape), dt, kind="ExternalInput").ap()
            self.__dict__[name] = ap
            self.declared.append(name)
            return ap
        raise AttributeError(name)


def build_nc(stop_after=None, dbg=False, nlayers=2, only=None):
    nc = bass.Bass("TRN2", target_bir_lowering=False)
    k = K()
    k.nc = nc
    k.dbg = dbg
    k.only = only

    order = ['M', 'N0', 'A0', 'G0', 'L0', 'C0', 'B0', 'N1', 'A1', 'G1', 'L1', 'C1', 'B1', 'F']
    upto = len(order) if stop_after is None else order.index(stop_after) + 1
    need = {'c2T': 'M', 'w_mod_s': 'M', 'b_mod_s': 'M', 'x_tok': 'N0', 'norm_g': 'N0', 'ident': 'N0', 'w_in_s': 'A0',
            'w_al': 'G0', 'gla_g': 'G0', 'rope': 'G0', 'cst': 'G0', 'b_gates_s': 'L0', 'conv_w_s': 'L0', 'conv_b_s': 'L0',
            'natab': 'C0', 'w_merge': 'B0', 'b_merge': 'B0', 'w_pa': 'B0', 'w_pb': 'B0', 'w_pc': 'B0', 'w_out': 'B0', 'final_g': 'F'}
    k.declared = []

    k._specs = {}

    def din(name, shape, dt=F32):
        k._specs[name] = (shape, dt)
        return None

    def dint(name, shape, dt=F32):
        return nc.dram_tensor(name, list(shape), dt, kind="ExternalOutput" if (dbg and name in DBG_OUT) else "Internal").ap()

    din('x_tok', [NTOK, D])
    din('c2T', [128, 32, 2])
    din('w_mod_s', [2, D, 3072])
    din('b_mod_s', [2, 2, 3072])
    din('norm_g', [2, D])
    din('final_g', [1, D])
    din('w_in_s', [2, D, NCOL])
    din('w_al', [2, 2, 17, 128])
    din('gla_g', [2, 256])
    din('b_gates_s', [2, 8])
    din('conv_w_s', [2, 3, 512])
    din('conv_b_s', [2, 512])
    din('natab', [2, 4, 5, 128, 576])
    din('rope', [64, 64, 256])
    din('cst', [64, 12, 64])
    din('ident', [128, 128])
    din('w_merge', [2, D, 3 * D])
    din('b_merge', [2, 3 * D])
    din('w_pa', [2, 1024, D])
    din('w_pb', [2, 1024, D])
    din('w_pc', [2, 2048, D])
    din('w_out', [2, D, D])
    k.out = nc.dram_tensor('out', [1024, D], F32, kind="ExternalOutput").ap()

    k.mod_loc = [dint('mod_loc%d' % l, [2, 3, 1024]) for l in range(2)]
    k.mod_all = [dint('mod_all%d' % l, [4, 2, 3, 1024]) for l in range(2)]
    k.hT_loc = [dint('hT_loc%d' % t, [128, 32 * 128], BF16) for t in range(NT)]
    k.hT_all = [dint('hT_all%d' % t, [4 * 128, 32 * 128], BF16) for t in range(NT)]
    if only is not None:
        k.P_lat = nc.dram_tensor('P_lat', [L, NCOL], F32, kind="ExternalInput").ap()
        k.P_ctx = nc.dram_tensor('P_ctx', [LC, NCOL], F32, kind="ExternalInput").ap()
        k.declared += ['P_lat', 'P_ctx']
    else:
        k.P_lat = dint('P_lat', [L, NCOL])
        k.P_ctx = dint('P_ctx', [LC, NCOL])
    k.stash = dint('stash', [L + LC, 1024])
    k.y_loc = [dint('y_loc%d' % t, [512 if t < 8 else 256, 1024], BF16) for t in range(NT)]
    k.y_all = [dint('y_all%d' % t, [4 * (512 if t < 8 else 256), 1024], BF16) for t in range(NT)]
    if dbg:
        k.y_dbg = nc.dram_tensor('y_dbg', [NT * 512, 1024], BF16, kind='ExternalOutput').ap()
    k.G_scr = dint('G_scr', [NT * 3 * 8 * 128, 512], BF16)
    k.x_loc = dint('x_loc', [NTOK, D])
    k.u_scr = dint('u_scr', [NTOK, D], BF16)
    if dbg:
        k.xn_dbg = nc.dram_tensor('xn_dbg', [128, D], F32, kind='ExternalOutput').ap()
        k.hn_dbg = nc.dram_tensor('hn_dbg', [128, D], BF16, kind='ExternalOutput').ap()
        k.hTt_dbg = nc.dram_tensor('hTt_dbg', [128, D], BF16, kind='ExternalOutput').ap()
        k.A_dbg = nc.dram_tensor('A_dbg', [128, D], F32, kind='ExternalOutput').ap()
        k.mod_dbg = nc.dram_tensor('mod_dbg', [48, 1024], F32, kind='ExternalOutput').ap()
        k.hT_dbg = nc.dram_tensor('hT_dbg', [NT * 512, 4096], BF16, kind='ExternalOutput').ap()

    p = Prog(nc)
    k.p = p
    phases = []
    phases.append(('M', lambda: phase_mod(k)))
    for l in range(nlayers):
        phases.append(('N%d' % l, lambda l=l: phase_norm(k, l)))
        phases.append(('A%d' % l, lambda l=l: phase_proj(k, l)))
        phases.append(('G%d' % l, lambda l=l: phase_gla(k, l)))
        phases.append(('L%d' % l, lambda l=l: phase_mlstm(k, l)))
        phases.append(('C%d' % l, lambda l=l: phase_na(k, l)))
        phases.append(('B%d' % l, lambda l=l: phase_b(k, l, last=(l == 1))))
    phases.append(('F', lambda: phase_final(k)))
    for name, fn in phases:
        if only is not None and name != only:
            continue
        fn()
        if stop_after == name:
            break
    p.flush()
    p.close()
    nc.declared_inputs = k.declared
    return nc


DBG_OUT = ('P_lat', 'P_ctx', 'x_loc', 'stash', 'y_dbg')


def allgather(k, src, dst, rkeys, wkeys):
    k.p.add('pool', lambda e: e.collective_compute("AllGather", ALU.bypass, replica_groups=GROUPS, ins=[src], outs=[dst]),
            rkeys, wkeys, dma=True, inc=1, semq='cc')


def mod_stream(k, s, layers):
    p = k.p
    c2 = s.sb('m_c2', [128, 32, 2], F32)
    cs = s.sb('m_cs', [128, 32, 2], BF16)
    bm = s.sb('m_bm', [2, 2 * 3072], F32)
    mo = s.sb('m_mo', [2, 2 * 3072], F32)
    l0 = layers[0]
    wm = [s.sb('m_wm%d' % i, [128, 32, 512], BF16) for i in range(2)]
    ps = [s.ps('m_ps%d' % i, [2, 512], F32) for i in range(2)]
    p.dma(c2[:], k.c2T, w=['c2'])
    p.dma(bm[:].rearrange("m (l c) -> m l c", l=2), k.b_mod_s.rearrange("l m c -> m l c"), w=['bm'])
    p.act(lambda e: e.activation(cs[:], c2[:], AF.Silu), r=['c2'], w=['cs'])
    it = 0
    for l in layers:
        for cb in range(6):
            b = it % 2
            it += 1
            p.dma(wm[b][:], k.w_mod_s[l][:, cb * 512:(cb + 1) * 512].rearrange("(kk pp) c -> pp kk c", pp=128),
                  w=['wm%d' % b], eng='pool')
            for kk in range(32):
                p.pe(lambda e, b=b, kk=kk: e.matmul(ps[b][:], cs[:, kk, :], wm[b][:, kk, :], start=(kk == 0), stop=(kk == 31)),
                     r=['cs', 'wm%d' % b], w=['mps%d' % b])
            o = l * 3072 + cb * 512
            p.dve(lambda e, b=b, o=o: e.tensor_tensor(mo[:, o:o + 512], ps[b][:], bm[:, o:o + 512], ALU.add),
                  r=['mps%d' % b, 'bm'], w=['mo'])
    for l in layers:
        p.dma(k.mod_loc[l].rearrange("m a c -> m (a c)"), mo[:, l * 3072:(l + 1) * 3072], r=['mo'], w=[('mod_loc', l)])
        allgather(k, k.mod_loc[l].rearrange("m a c -> (m a) c"), k.mod_all[l].rearrange("r m a c -> (r m a) c"), [('mod_loc', l)], [('mod_all', l)])


def phase_mod(k):
    s = Scope(k.p)
    mod_stream(k, s, [0])
    s.end()


def modvec_bc(k, l, m, a):
    v = k.mod_all[l].rearrange("r m a c -> m a r c")[m, a]
    return v.partition_broadcast(128)


def phase_norm(k, l):
    p = k.p
    s = Scope(p)
    A = [s.sb('n_A%d' % m, [128, D], F32) for m in range(2)]
    S = [s.sb('n_S%d' % m, [128, D], F32) for m in range(2)]
    gbc = s.sb('n_g', [128, D], F32)
    xt = [s.sb('n_x%d' % i, [128, D], F32) for i in range(2)]
    xn2 = [s.sb('n_xn%d' % i, [128, D], F32) for i in range(2)]
    hn2 = [s.sb('n_hn%d' % i, [128, D], BF16) for i in range(2)]
    junk = s.sb('n_junk', [128, D], BF16)
    st2 = [s.sb('n_st%d' % i, [128, 4], F32) for i in range(2)]
    hT = [s.sb('n_hT%d' % i, [128, 32, 128], BF16) for i in range(2)]
    idb = s.sb('n_idb', [128, 128], BF16)
    pst = [s.ps('n_ps%d' % i, [128, 8, 128], BF16) for i in range(2)]
    p.dma(idb[:], k.ident, w=['idb'], eng='pool')
    p.dma(gbc[:], k.norm_g[l].partition_broadcast(128), w=['gbc'])
    for m in range(2):
        p.dma(A[m][:].rearrange("p (r c) -> p r c", r=4), modvec_bc(k, l, m, 1), w=['A%d' % m])
        p.dma(S[m][:].rearrange("p (r c) -> p r c", r=4), modvec_bc(k, l, m, 0), w=['S%d' % m])
        p.dve(lambda e, m=m: e.scalar_tensor_tensor(A[m][:], A[m][:], 1.0, gbc[:], ALU.add, ALU.mult), r=['A%d' % m, 'gbc'], w=['A%d' % m])
    for i in range(2):
        p.dve(lambda e, i=i: e.memset(hT[i][:], 0.0), w=['hT%d' % i])
    xsrc = k.x_tok if l == 0 else k.x_loc
    for t in range(NT):
        rows = 128 if t < 8 else 64
        m = 0 if t < 8 else 1
        b = t % 2
        xn, hn, st = xn2[b], hn2[b], st2[b]
        XN, HN, ST = 'xn%d' % b, 'hn%d' % b, 'st%d' % b
        p.dma(xt[b][:rows], xsrc[t * 128:t * 128 + rows, :], w=['xt%d' % b])
        p.dve(lambda e, st=st: e.memset(st[:], 0.0), w=[ST])
        p.act(lambda e, b=b, rows=rows, st=st: e.activation(junk[:rows], xt[b][:rows], AF.Square, accum_out=st[:rows, 0:1]), r=['xt%d' % b, ST], w=['junk', ST])
        p.dve(lambda e, rows=rows, st=st: e.tensor_scalar(st[:rows, 1:2], st[:rows, 0:1], 1.0 / D, EPS, ALU.mult, ALU.add), r=[ST], w=[ST])
        p.act(lambda e, rows=rows, st=st: e.activation(st[:rows, 1:2], st[:rows, 1:2], AF.Sqrt), r=[ST], w=[ST])
        p.dve(lambda e, rows=rows, st=st: e.reciprocal(st[:rows, 2:3], st[:rows, 1:2]), r=[ST], w=[ST])
        p.dve(lambda e, b=b, rows=rows, m=m, st=st, xn=xn: e.scalar_tensor_tensor(xn[:rows], xt[b][:rows], st[:rows, 2:3], A[m][:rows], ALU.mult, ALU.mult),
              r=['xt%d' % b, ST, 'A%d' % m], w=[XN])
        p.dve(lambda e, rows=rows, m=m, hn=hn, xn=xn: e.tensor_tensor(hn[:rows], xn[:rows], S[m][:rows], ALU.add), r=[XN, 'S%d' % m], w=[HN])
        for g in range(4):
            pb = (t * 4 + g) % 2
            for i in range(8):
                kk = g * 8 + i
                p.pe(lambda e, pb=pb, i=i, kk=kk, rows=rows, hn=hn: e.transpose(pst[pb][:, i, :rows], hn[:rows, kk * 128:(kk + 1) * 128], idb[:rows, :rows]),
                     r=[HN, 'idb'], w=['pst%d' % pb])
            if g % 2 == 0:
                p.act(lambda e, pb=pb, g=g, b=b, rows=rows: e.copy(hT[b][:, g * 8:(g + 1) * 8, :rows], pst[pb][:, :, :rows]), r=['pst%d' % pb], w=['hT%d' % b])
            else:
                p.dve(lambda e, pb=pb, g=g, b=b, rows=rows: e.tensor_copy(hT[b][:, g * 8:(g + 1) * 8, :rows], pst[pb][:, :, :rows]), r=['pst%d' % pb], w=['hT%d' % b])
        p.dma(k.hT_loc[t], hT[b][:].rearrange("p a b -> p (a b)"), r=['hT%d' % b], w=[('hT_loc', t)])
        if k.dbg and l == 0 and t == 0:
            p.dma(k.xn_dbg, xn[:], r=[XN], w=['d1'])
            p.dma(k.hn_dbg, hn[:], r=[HN], w=['d2'])
            p.dma(k.hTt_dbg, hT[b][:].rearrange("p a b -> p (a b)"), r=['hT%d' % b], w=['d3'])
            p.dma(k.A_dbg, A[0][:], r=['A0'], w=['d4'])
        allgather(k, k.hT_loc[t], k.hT_all[t], [('hT_loc', t)], [('hT_all', t)])
        if k.dbg and l == 0:
            p.dma(k.hT_dbg[t * 512:(t + 1) * 512, :], k.hT_all[t], r=[('hT_all', t)], w=[('hTd', t)])
    if k.dbg and l == 0:
        pass

    s.end()


def proj_stream(k, l, s, blocks, nps, tag):
    p = k.p
    wb = [s.sb('a_w%s%d' % (tag, i), [128, 32, 512], BF16) for i in range(2)]
    ht = [s.sb('a_h%s%d' % (tag, i), [128, 32, 128], BF16) for i in range(3)]
    ob = [s.sb('a_o%s%d' % (tag, i), [128, 512], F32) for i in range(3)]
    ps = [s.ps('a_ps%s%d' % (tag, i), [128, 512], F32) for i in range(nps)]
    it = 0
    for bi, nb in enumerate(blocks):
        c0 = nb * 512
        n = 512 if nb < 8 else NCOL - 4096
        wi = bi % 2
        p.dma(wb[wi][:, :, :n], k.w_in_s[l][:, c0:c0 + n].rearrange("(kk pp) c -> pp kk c", pp=128), w=['aw%d' % wi], eng='pool')
        for r in range(4):
            for t in range(NT):
                rows = 128 if t < 8 else 64
                hi, oi, pi = it % 3, it % 3, it % nps
                it += 1
                p.dma(ht[hi][:].rearrange("p a b -> p (a b)"), k.hT_all[t][r * 128:(r + 1) * 128, :], w=['ah%d' % hi])
                for kk in range(32):
                    p.pe(lambda e, pi=pi, hi=hi, wi=wi, kk=kk, rows=rows, n=n: e.matmul(ps[pi][:rows, :n], ht[hi][:, kk, :rows], wb[wi][:, kk, :n], start=(kk == 0), stop=(kk == 31)),
                         r=['ah%d' % hi, 'aw%d' % wi], w=['aps%d' % pi])
                if it % 2 == 0:
                    p.act(lambda e, oi=oi, pi=pi, rows=rows, n=n: e.copy(ob[oi][:rows, :n], ps[pi][:rows, :n]), r=[], w=['ao%d' % oi, 'aps%d' % pi])
                else:
                    p.dve(lambda e, oi=oi, pi=pi, rows=rows, n=n: e.tensor_copy(ob[oi][:rows, :n], ps[pi][:rows, :n]), r=[], w=['ao%d' % oi, 'aps%d' % pi])
                if t < 8:
                    dst = k.P_lat[r * 1024 + t * 128: r * 1024 + t * 128 + 128, c0:c0 + n]
                else:
                    dst = k.P_ctx[r * 64:(r + 1) * 64, c0:c0 + n]
                p.dma(dst, ob[oi][:rows, :n], r=['ao%d' % oi], w=[('P', r, t, nb)], eng='act')


def phase_proj(k, l):
    s = Scope(k.p)
    fns = [lambda: proj_stream(k, l, s, [0, 1, 2, 3, 8], 4, 'p1')]
    if l == 0:
        fns.append(lambda: mod_stream(k, s, [1]))
    k.p.interleave(fns)
    s.end()


def gates_stream(k, l, s, ntiles):
    p = k.p
    hTs = s.sb('b_hTs', [128, 32, NT * 128], BF16)
    wb = [s.sb('b_wb%d' % i, [128, 32, 512], BF16) for i in range(2)]
    bms = [s.sb('b_bm%d' % i, [1, 512], BF16) for i in range(2)]
    onesr = s.sb('b_ones', [1, 128], BF16)
    Gsb = [s.sb('b_G%d' % i, [128, 512], BF16) for i in range(3)]
    ps = [s.ps('b_ps%d' % i, [128, 512], F32) for i in range(2)]
    p.dve(lambda e: e.memset(onesr[:], 1.0), w=['onesr'])
    for t in range(ntiles):
        p.dma(hTs[:, :, t * 128:(t + 1) * 128], k.hT_loc[t].rearrange("q (a b) -> q a b", a=32), w=[('hTs', t)])
    it = 0
    wi = 0
    for cb in range(8):
        for kb in range(3):
            c0 = kb * D + cb * 512
            w_ = wi % 2
            wi += 1
            p.dma(wb[w_][:], k.w_merge[l][:, c0:c0 + 512].rearrange("(kk pp) c -> pp kk c", pp=128), w=['bw%d' % w_], eng='pool')
            p.dma(bms[w_][:], k.b_merge[l:l + 1, c0:c0 + 512], w=['bm%d' % w_], eng='pool')
            for t in range(ntiles):
                rows = 128 if t < 8 else 64
                pi, gi = it % 2, it % 3
                it += 1
                for kk in range(32):
                    p.pe(lambda e, pi=pi, w_=w_, kk=kk, t=t, rows=rows: e.matmul(ps[pi][:rows, :], hTs[:, kk, t * 128:t * 128 + rows], wb[w_][:, kk, :], start=(kk == 0), stop=False),
                         r=[('hTs', t), 'bw%d' % w_], w=['bps%d' % pi])
                p.pe(lambda e, pi=pi, rows=rows, w_=w_: e.matmul(ps[pi][:rows, :], onesr[:, :rows], bms[w_][:], start=False, stop=True), r=['onesr', 'bm%d' % w_], w=['bps%d' % pi])
                p.act(lambda e, pi=pi, gi=gi, rows=rows: e.activation(Gsb[gi][:rows, :], ps[pi][:rows, :], AF.Sigmoid), r=[], w=['bG%d' % gi, 'bps%d' % pi])
                g0 = ((t * 3 + kb) * 8 + cb) * 128
                p.dma(k.G_scr[g0:g0 + rows, :], Gsb[gi][:rows, :], r=['bG%d' % gi], w=[('G', t, kb, cb)])


def yrows(k, kind, c, n=64):
    if kind == 'lat':
        t = (c % 16) // 2
        r0 = (c // 16) * 128 + (c % 2) * 64
        return k.y_loc[t][r0:r0 + n, :], ('y', t, c // 16, c % 2)
    return k.y_loc[8][c * 64:c * 64 + n, :], ('y', 8, c, 0)


def dbg_y(k, l, c0, c1):
    if k.dbg and l == 0:
        for t in range(NT):
            n = 512 if t < 8 else 256
            k.p.dma(k.y_dbg[t * 512:t * 512 + n, c0:c1], k.y_loc[t][:, c0:c1], w=[('yd', t)])
        k.p.flush()


def chunk_order(d):
    ctx = [('ctx', c) for c in range(4)]
    lat = [('lat', c) for c in range(64)]
    if d == 1:
        ctx.reverse()
        lat.reverse()
    return ctx + lat


def chunk_pos(d):
    return {kc: i for i, kc in enumerate(chunk_order(d))}


def phase_gla(k, l):
    s = Scope(k.p)
    fns = [lambda d=d: gla_sweep(k, l, d, s) for d in range(2)]
    if k.only is None:
        fns.append(lambda: gates_stream(k, l, s, 8 if l == 1 else NT))
    k.p.interleave(fns)
    s.end()
    dbg_y(k, l, 0, 256)


def gla_sweep(k, l, d, s):
    p = k.p
    mypos, otpos = chunk_pos(d), chunk_pos(1 - d)
    own0, oth0 = (0, 512) if d == 0 else (512, 0)
    cst = s.sb('g_cst', [64, 12, 64], F32)
    waug = s.sb('g_waug', [17, 128], F32)
    gng = s.sb('g_gng', [64, 256], F32)
    mask2 = s.sb('g_mask2', [64, 2, 64], F32)
    S32 = [s.sb('g_S32%d' % h, [64, 128], F32) for h in range(2)]
    Sbf = [s.sb('g_Sbf%d' % h, [64, 128], BF16) for h in range(2)]
    aaT = s.sb('g_aaT', [17, 64], F32)
    pa = [s.sb('g_pa%d' % i, [64, 768], F32) for i in range(2)]
    dec = [s.sb('g_dec%d' % i, [64, 16], F32) for i in range(2)]
    rt = [s.sb('g_rt%d' % i, [64, 256], F32) for i in range(2)]
    of = [s.sb('g_of%d' % i, [64, 256], F32) for i in range(2)]
    e1 = s.sb('g_e1', [64, 128], F32)
    sp = s.sb('g_sp', [64, 128], F32)
    bs = s.sb('g_bs', [64, 128], F32)
    Ep = s.sb('g_Ep', [64, 128], F32)
    Em = s.sb('g_Em', [64, 128], F32)
    dlt = s.sb('g_dlt', [64, 128], F32)
    Eh = s.sb('g_Eh', [64, 128], F32)
    decs = s.sb('g_decs', [64, 2], F32)
    tt = [s.sb('g_t%d' % i, [64, 8, 16], F32) for i in range(4)]
    qkr = s.sb('g_qkr', [64, 256], F32)
    qt = s.sb('g_qt', [64, 128], F32)
    kt = s.sb('g_kt', [64, 128], F32)
    kh = s.sb('g_kh', [64, 128], BF16)
    vbf = s.sb('g_vbf', [64, 256], BF16)
    qkT = s.sb('g_qkT', [64, 4, 64], BF16)
    attm = s.sb('g_attm', [64, 2, 64], BF16)
    osb = s.sb('g_osb', [64, 256], F32)
    junk = s.sb('g_junk', [64, 128], F32)
    ss = s.sb('g_ss', [64, 4], F32)
    sz = s.sb('g_sz', [64, 256], F32)
    tn = s.sb('g_tn', [64, 256], F32)
    ya = s.sb('g_ya', [64, 256], BF16)
    T1 = s.ps('g_T1', [64, 512], F32)
    PA = s.ps('g_PA', [64, 512], F32)
    psT = PA[:, 0:256].rearrange("p (a b) -> p a b", a=4)
    att = PA[:, 256:384].rearrange("p (a b) -> p a b", a=2)
    OK = s.ps('g_OK', [64, 512], F32)
    ops = OK[:, 0:256].rearrange("p (a b) -> p a b", a=2)
    kvp = OK[:, 256:512].rearrange("p (a b) -> p a b", a=2)

    p.dma(cst[:], k.cst, w=['cst'])
    p.dma(waug[:], k.w_al[l, d], w=['waug'])
    p.dma(gng[:], k.gla_g[l].partition_broadcast(64), w=['gng'])
    for h in range(2):
        p.dma(mask2[:, h, :], k.cst[:, d, :], w=['mask2'])
        p.dve(lambda e, h=h: e.memset(S32[h][:], 0.0), w=['S32%d' % h])
        p.dve(lambda e, h=h: e.memset(Sbf[h][:], 0.0), w=['Sbf%d' % h])
    p.dve(lambda e: e.memset(aaT[:], 1.0), w=['aaT'])
    TriD = cst[:, 2 + d, :]
    All16 = cst[:, 4, :]
    id64 = cst[:, 9, :]

    for it, (kind, c) in enumerate(chunk_order(d)):
        b = it % 2
        src = k.P_lat if kind == 'lat' else k.P_ctx
        r0 = c * 64
        srow = r0 if kind == 'lat' else L + r0
        P = 'pa%d' % b
        p.dma(pa[b][:], src[r0:r0 + 64, C_GLA:C_GLA + 768], w=[P])
        p.dma(dec[b][:], src[r0:r0 + 64, C_DEC + 16 * d:C_DEC + 16 * d + 16], w=['dec%d' % b])
        if kind == 'lat':
            p.dma(rt[b][:], k.rope[c], w=['rt%d' % b])
        epi = mypos[(kind, c)] > otpos[(kind, c)]
        p.pe(lambda e, b=b: e.transpose(T1[0:16, 448:512], dec[b][:], id64), r=['dec%d' % b, 'cst'], w=['T1'])
        p.act(lambda e: e.copy(aaT[0:16, :], T1[0:16, 448:512]), r=[], w=['aaT', 'T1'])
        p.pe(lambda e: e.matmul(T1[:, 0:128], aaT[:], waug[:], start=True, stop=True), r=['aaT', 'waug'], w=['T1'])
        p.act(lambda e: e.activation(e1[:], T1[:, 0:128], AF.Exp, scale=-1.0), r=[], w=['e1', 'T1'])
        p.act(lambda e: e.activation(sp[:], e1[:], AF.Ln, bias=1.0), r=['e1'], w=['sp'])
        p.pe(lambda e: e.matmul(T1[:, 128:256], TriD, sp[:], start=True, stop=True), r=['sp', 'cst'], w=['T1'])
        p.pe(lambda e: e.matmul(T1[:, 256:384], All16, sp[:], start=True, stop=True), r=['sp', 'cst'], w=['T1'])
        for h in range(2):
            p.pe(lambda e, h=h: e.matmul(T1[:, 384 + h:385 + h], sp[:, h * 64:(h + 1) * 64], cst[:, 4, 0:1], start=True, stop=True), r=['sp', 'cst'], w=['T1'])
        p.dve(lambda e: e.tensor_copy(bs[:], T1[:, 128:256]), r=[], w=['bs', 'T1'])
        p.act(lambda e: e.activation(Ep[:], bs[:], AF.Exp), r=['bs'], w=['Ep'])
        p.act(lambda e: e.activation(Em[:], bs[:], AF.Exp, scale=-1.0), r=['bs'], w=['Em'])
        p.dve(lambda e: e.tensor_tensor(dlt[:], T1[:, 256:384], bs[:], ALU.subtract), r=['bs'], w=['dlt', 'T1'])
        p.act(lambda e: e.activation(Eh[:], dlt[:], AF.Exp), r=['dlt'], w=['Eh'])
        p.act(lambda e: e.activation(decs[:], T1[:, 384:386], AF.Exp), r=[], w=['decs', 'T1'])
        if kind == 'lat':
            x4 = pa[b][:, 0:256].rearrange("p (a h f) -> p a h f", a=8, h=2)
            cos = rt[b][:, 0:128].rearrange("p (a f) -> p a f", a=8)
            sin = rt[b][:, 128:256].rearrange("p (a f) -> p a f", a=8)
            o4 = qkr[:].rearrange("p (a h f) -> p a h f", a=8, h=2)
            R = [P, 'rt%d' % b]
            p.dve(lambda e, x4=x4, cos=cos: e.tensor_tensor(tt[0][:], x4[:, :, 0, :], cos, ALU.mult), r=R, w=['t0'])
            p.dve(lambda e, x4=x4, sin=sin: e.tensor_tensor(tt[1][:], x4[:, :, 1, :], sin, ALU.mult), r=R, w=['t1'])
            p.dve(lambda e, o4=o4: e.tensor_tensor(o4[:, :, 0, :], tt[0][:], tt[1][:], ALU.subtract), r=['t0', 't1'], w=['qkr'])
            p.dve(lambda e, x4=x4, sin=sin: e.tensor_tensor(tt[2][:], x4[:, :, 0, :], sin, ALU.mult), r=R, w=['t2'])
            p.dve(lambda e, x4=x4, cos=cos: e.tensor_tensor(tt[3][:], x4[:, :, 1, :], cos, ALU.mult), r=R, w=['t3'])
            p.dve(lambda e, o4=o4: e.tensor_tensor(o4[:, :, 1, :], tt[2][:], tt[3][:], ALU.add), r=['t2', 't3', 'qkr'], w=['qkr'])
            qsrc, QK = qkr, 'qkr'
        else:
            qsrc, QK = pa[b], P
        p.dve(lambda e, qsrc=qsrc: e.scalar_tensor_tensor(qt[:], qsrc[:, 0:128], 0.125, Ep[:], ALU.mult, ALU.mult), r=[QK, 'Ep'], w=['qt'])
        p.dve(lambda e, qsrc=qsrc: e.tensor_tensor(kt[:], qsrc[:, 128:256], Em[:], ALU.mult), r=[QK, 'Em'], w=['kt'])
        p.dve(lambda e, qsrc=qsrc: e.tensor_tensor(kh[:], qsrc[:, 128:256], Eh[:], ALU.mult), r=[QK, 'Eh'], w=['kh'])
        p.act(lambda e, b=b: e.copy(vbf[:], pa[b][:, 256:512]), r=[P], w=['vbf'])
        for i in range(4):
            srcT = qt if i < 2 else kt
            p.pe(lambda e, i=i, srcT=srcT: e.transpose(psT[:, i, :], srcT[:, (i % 2) * 64:(i % 2) * 64 + 64], id64), r=['qt', 'kt', 'cst'], w=['PA'])
        p.act(lambda e: e.copy(qkT[:], psT[:]), r=[], w=['qkT', 'PA'])
        for h in range(2):
            p.pe(lambda e, h=h: e.matmul(att[:, h, :], qkT[:, 2 + h, :], qkT[:, h, :], start=True, stop=True), r=['qkT'], w=['PA'])
        p.dve(lambda e: e.tensor_tensor(attm[:], att[:], mask2[:], ALU.mult), r=['mask2'], w=['attm', 'PA'])
        for h in range(2):
            p.pe(lambda e, h=h: e.matmul(ops[:, h, :], attm[:, h, :], vbf[:, h * 128:(h + 1) * 128], start=True, stop=False), r=['attm', 'vbf'], w=['OK'])
            p.pe(lambda e, h=h: e.matmul(ops[:, h, :], qkT[:, h, :], Sbf[h][:], start=False, stop=True), r=['qkT', 'Sbf%d' % h], w=['OK'])
        for h in range(2):
            p.pe(lambda e, h=h: e.matmul(kvp[:, h, :], kh[:, h * 64:(h + 1) * 64], vbf[:, h * 128:(h + 1) * 128], start=True, stop=True), r=['kh', 'vbf'], w=['OK'])
        for h in range(2):
            p.dve(lambda e, h=h: e.scalar_tensor_tensor(S32[h][:], S32[h][:], decs[:, h:h + 1], kvp[:, h, :], ALU.mult, ALU.add), r=['S32%d' % h, 'decs'], w=['S32%d' % h, 'OK'])
            p.act(lambda e, h=h: e.copy(Sbf[h][:], S32[h][:]), r=['S32%d' % h], w=['Sbf%d' % h])
        if not epi:
            p.act(lambda e: e.copy(osb[:], ops[:].rearrange("p a b -> p (a b)")), r=[], w=['osb', 'OK'])
            p.dma(k.stash[srow:srow + 64, own0:own0 + 256], osb[:], r=['osb'], w=[('glob', 'st', 'g', srow)])
        else:
            p.dma(of[b][:], k.stash[srow:srow + 64, oth0:oth0 + 256], r=[('glob', 'st', 'g', srow)], w=['of%d' % b])
            p.dve(lambda e, b=b: e.tensor_tensor(osb[:], ops[:].rearrange("p a b -> p (a b)"), of[b][:], ALU.add), r=['of%d' % b], w=['osb', 'OK'])
            p.dve(lambda e: e.memset(ss[:], 0.0), w=['ss'])
            for h in range(2):
                p.act(lambda e, h=h: e.activation(junk[:], osb[:, h * 128:(h + 1) * 128], AF.Square, accum_out=ss[:, h:h + 1]), r=['osb', 'ss'], w=['junk', 'ss'])
            p.dve(lambda e: e.tensor_scalar(ss[:, 2:4], ss[:, 0:2], 1.0 / 128, EPS, ALU.mult, ALU.add), r=['ss'], w=['ss'])
            p.act(lambda e: e.activation(ss[:, 2:4], ss[:, 2:4], AF.Sqrt), r=['ss'], w=['ss'])
            p.dve(lambda e: e.reciprocal(ss[:, 2:4], ss[:, 2:4]), r=['ss'], w=['ss'])
            p.act(lambda e, b=b: e.activation(sz[:], pa[b][:, 512:768], AF.Silu), r=[P], w=['sz'])
            for h in range(2):
                p.dve(lambda e, h=h: e.scalar_tensor_tensor(tn[:, h * 128:(h + 1) * 128], osb[:, h * 128:(h + 1) * 128], ss[:, 2 + h:3 + h], gng[:, h * 128:(h + 1) * 128], ALU.mult, ALU.mult),
                      r=['osb', 'ss', 'gng'], w=['tn'])
            p.dve(lambda e: e.tensor_tensor(ya[:], tn[:], sz[:], ALU.mult), r=['tn', 'sz'], w=['ya'])
            ydst, ykey = yrows(k, kind, c)
            p.dma(ydst[:, 0:256], ya[:], r=['ya'], w=[('glob',) + ykey + ('a',)])


def phase_mlstm(k, l):
    s = Scope(k.p)
    fns = [lambda d=d: mlstm_sweep(k, l, d, s) for d in range(2)]
    if k.only is None:
        fns.append(lambda: proj_stream(k, l, s, [4, 5, 6, 7], 2, 'p2'))
    k.p.interleave(fns)
    s.end()
    dbg_y(k, l, 256, 512)


def mlstm_sweep(k, l, d, s):
    p = k.p
    mypos, otpos = chunk_pos(d), chunk_pos(1 - d)
    own0, oth0 = (256, 768) if d == 0 else (768, 256)
    cst = s.sb('l_cst', [64, 12, 64], F32)
    convw = s.sb('l_convw', [64, 3, 512], F32)
    convb = s.sb('l_convb', [64, 512], F32)
    bg = s.sb('l_bg', [64, 8], F32)
    ones = s.sb('l_ones', [64, 128], F32)
    nones = s.sb('l_nones', [64, 128], F32)
    CN32 = [s.sb('l_CN32%d' % h, [128, 129], F32) for h in range(2)]
    CNbf = [s.sb('l_CNbf%d' % h, [128, 129], BF16) for h in range(2)]
    mm = s.sb('l_mm', [128, 2], F32)
    v1 = s.sb('l_v1', [64, 2, 129], BF16)
    mn = [s.sb('l_mn%d' % i, [64, 1280], F32) for i in range(2)]
    pv = [s.sb('l_pv%d' % i, [64, 512], F32) for i in range(2)]
    nx = [s.sb('l_nx%d' % i, [64, 512], F32) for i in range(2)]
    gt = [s.sb('l_gt%d' % i, [64, 8], F32) for i in range(2)]
    hf = [s.sb('l_hf%d' % i, [64, 256], F32) for i in range(2)]
    ta = s.sb('l_ta', [64, 512], F32)
    tb = s.sb('l_tb', [64, 512], F32)
    sl = s.sb('l_sl', [64, 512], F32)
    qs = s.sb('l_qs', [64, 256], F32)
    ks = s.sb('l_ks', [64, 256], F32)
    qkT = s.sb('l_qkT', [128, 4, 64], BF16)
    g = s.sb('l_g', [64, 8], F32)
    sm = s.sb('l_sm', [64, 40], F32)
    sm128 = s.sb('l_sm128', [128, 16], F32)
    diag = [s.sb('l_diag%d' % h, [64, 64], F32) for h in range(2)]
    Dm = s.sb('l_D', [64, 2, 64], F32)
    Ew = s.sb('l_Ew', [64, 2, 64], F32)
    qkE = s.sb('l_qkE', [64, 2, 64], F32)
    qkET = s.sb('l_qkET', [64, 2, 64], BF16)
    ins_ = s.sb('l_ins', [64, 2, 128], F32)
    hout = s.sb('l_hout', [64, 256], F32)
    kw = s.sb('l_kw', [64, 2, 128], BF16)
    so = s.sb('l_so', [64, 256], F32)
    sz = s.sb('l_sz', [64, 256], F32)
    yb = s.sb('l_yb', [64, 256], BF16)
    bA = s.ps('l_bA', [128, 512], F32)
    bB = s.ps('l_bB', [128, 512], F32)
    bD = s.ps('l_bD', [128, 512], F32)
    Tg = bA[:, 0:16]
    Rps = bA[:, 16:144].rearrange("p (a b) -> p a b", a=2)
    psE = bA[0:64, 144:272].rearrange("p (a b) -> p a b", a=2)
    Sps = bA[0:64, 272:400].rearrange("p (a b) -> p a b", a=2)
    psT = bB[:, 0:256].rearrange("p (a b) -> p a b", a=4)
    nps = bB[0:64, 256:512].rearrange("p (a b) -> p a b", a=2)
    ips = bD[0:64, 0:258].rearrange("p (a b) -> p a b", a=2)
    ups1 = bD[:, 258:387]

    p.dma(cst[:], k.cst, w=['cst'])
    p.dma(convw[:], k.conv_w_s[l].partition_broadcast(64), w=['convw'])
    p.dma(convb[:], k.conv_b_s[l].partition_broadcast(64), w=['convb'])
    p.dma(bg[:], k.b_gates_s[l].partition_broadcast(64), w=['bg'])
    p.dve(lambda e: e.memset(ones[:], 1.0), w=['ones'])
    p.dve(lambda e: e.memset(nones[:], -1.0), w=['nones'])
    p.dve(lambda e: e.memset(mm[:], 0.0), w=['mm'])
    p.dve(lambda e: e.memset(v1[:], 1.0), w=['v1'])
    for h in range(2):
        p.dve(lambda e, h=h: e.memset(CN32[h][:], 0.0), w=['CN32%d' % h])
        p.dve(lambda e, h=h: e.memset(CNbf[h][:], 0.0), w=['CNbf%d' % h])
    TriM = cst[:, 5 + d, :]
    maskb = cst[:, 7 + d, :]
    id64 = cst[:, 9, :]
    C = lambda a: sm[:, a:a + 2]
    C8 = lambda a: sm128[:, a:a + 2]

    for it, (kind, c) in enumerate(chunk_order(d)):
        b = it % 2
        src = k.P_lat if kind == 'lat' else k.P_ctx
        last_c = 63 if kind == 'lat' else 3
        r0 = c * 64
        srow = r0 if kind == 'lat' else L + r0
        MN, PV, NX, GT = 'mn%d' % b, 'pv%d' % b, 'nx%d' % b, 'gt%d' % b
        p.dma(mn[b][:], src[r0:r0 + 64, C_ML:C_ML + 1280], w=[MN])
        if c == 0:
            p.dve(lambda e, b=b: e.memset(pv[b][:], 0.0), w=[PV])
            p.dma(pv[b][1:64, :], src[0:63, C_ML:C_ML + 512], w=[PV])
        else:
            p.dma(pv[b][:], src[r0 - 1:r0 + 63, C_ML:C_ML + 512], w=[PV])
        if c == last_c:
            p.dve(lambda e, b=b: e.memset(nx[b][:], 0.0), w=[NX])
            p.dma(nx[b][0:63, :], src[r0 + 1:r0 + 64, C_ML:C_ML + 512], w=[NX])
        else:
            p.dma(nx[b][:], src[r0 + 1:r0 + 65, C_ML:C_ML + 512], w=[NX])
        p.dma(gt[b][:], src[r0:r0 + 64, C_GAT:C_GAT + 8], w=[GT])
        epi = mypos[(kind, c)] > otpos[(kind, c)]
        p.dve(lambda e, b=b: e.tensor_tensor(ta[:], pv[b][:], convw[:, 0, :], ALU.mult), r=[PV, 'convw'], w=['ta'])
        p.dve(lambda e, b=b: e.tensor_tensor(tb[:], mn[b][:, 0:512], convw[:, 1, :], ALU.mult), r=[MN, 'convw'], w=['tb'])
        p.dve(lambda e: e.tensor_tensor(ta[:], ta[:], tb[:], ALU.add), r=['ta', 'tb'], w=['ta'])
        p.dve(lambda e, b=b: e.tensor_tensor(tb[:], nx[b][:], convw[:, 2, :], ALU.mult), r=[NX, 'convw', 'ta'], w=['tb'])
        p.dve(lambda e: e.tensor_tensor(ta[:], ta[:], tb[:], ALU.add), r=['ta', 'tb'], w=['ta'])
        p.dve(lambda e: e.tensor_tensor(ta[:], ta[:], convb[:], ALU.add), r=['ta', 'convb'], w=['ta'])
        p.act(lambda e: e.activation(sl[:], ta[:], AF.Silu), r=['ta'], w=['sl'])
        p.dve(lambda e: e.tensor_scalar(qs[:], sl[:, 0:256], 128.0 ** -0.5, None, ALU.mult), r=['sl'], w=['qs'])
        p.act(lambda e: e.copy(ks[:], sl[:, 256:512]), r=['sl'], w=['ks'])
        p.act(lambda e, b=b: e.copy(v1[:, :, 0:128], mn[b][:, 512:768].rearrange("p (h v) -> p h v", h=2)), r=[MN], w=['v1'])
        for i in range(4):
            srcT = qs if i < 2 else ks
            p.pe(lambda e, i=i, srcT=srcT: e.transpose(psT[:, i, :], srcT[:, (i % 2) * 128:(i % 2) * 128 + 128], id64), r=['qs', 'ks', 'cst'], w=['bB'])
        p.act(lambda e: e.copy(qkT[:], psT[:]), r=[], w=['qkT', 'bB'])
        p.dve(lambda e, b=b: e.tensor_tensor(g[:], gt[b][:], bg[:], ALU.add), r=[GT, 'bg'], w=['g'])
        ic = g[:, 4 * d:4 * d + 2]
        fp = g[:, 4 * d + 2:4 * d + 4]
        p.act(lambda e, fp=fp: e.activation(C(0), fp, AF.Exp, scale=-1.0), r=['g'], w=['sm'])
        p.act(lambda e: e.activation(C(2), C(0), AF.Ln, bias=1.0), r=['sm'], w=['sm'])
        p.pe(lambda e: e.matmul(Tg[0:64, 0:2], TriM, C(2), start=True, stop=True), r=['sm', 'cst'], w=['bA'])
        p.pe(lambda e: e.matmul(Tg[:, 8:10], nones[:], C(2), start=True, stop=True), r=['sm', 'nones'], w=['bA'])
        p.dve(lambda e: e.tensor_copy(C(4), Tg[0:64, 0:2]), r=[], w=['sm', 'bA'])
        p.dve(lambda e: e.tensor_copy(C8(0), Tg[:, 8:10]), r=[], w=['sm128', 'bA'])
        p.dve(lambda e, ic=ic: e.tensor_tensor(C(6), ic, C(4), ALU.subtract), r=['g', 'sm'], w=['sm'])
        for h in range(2):
            p.dve(lambda e, h=h: e.tensor_scalar(diag[h][:], id64, sm[:, 6 + h:7 + h], None, ALU.mult), r=['cst', 'sm'], w=['diag%d' % h])
            p.pe(lambda e, h=h: e.matmul(Rps[:, h, :], ones[:], diag[h][:], start=True, stop=True), r=['ones', 'diag%d' % h], w=['bA'])
        for h in range(2):
            p.dve(lambda e, h=h: e.scalar_tensor_tensor(Dm[:, h, :], Rps[0:64, h, :], sm[:, 4 + h:5 + h], maskb, ALU.add, ALU.add), r=['sm', 'cst'], w=['D', 'bA'])
        p.dve(lambda e: e.reduce_max(C(8), Dm[:], AX.X), r=['D'], w=['sm'])
        p.dve(lambda e: e.tensor_tensor(C(10), C(4), mm[0:64, :], ALU.add), r=['sm', 'mm'], w=['sm'])
        p.dve(lambda e: e.tensor_tensor(C(12), C(10), C(8), ALU.max), r=['sm'], w=['sm'])
        p.dve(lambda e: e.tensor_scalar(C(14), C(12), -1.0, None, ALU.mult), r=['sm'], w=['sm'])
        p.dve(lambda e: e.tensor_tensor(C(16), C(10), C(12), ALU.subtract), r=['sm'], w=['sm'])
        p.act(lambda e: e.activation(C(18), C(16), AF.Exp), r=['sm'], w=['sm'])
        p.act(lambda e: e.activation(C(20), C(14), AF.Exp), r=['sm'], w=['sm'])
        for h in range(2):
            p.act(lambda e, h=h: e.activation(Ew[:, h, :], Dm[:, h, :], AF.Exp, bias=sm[:, 14 + h:15 + h]), r=['D', 'sm'], w=['Ew'])
            p.pe(lambda e, h=h: e.matmul(Sps[:, h, :], qkT[:, h, :], qkT[:, 2 + h, :], start=True, stop=True), r=['qkT'], w=['bA'])
        p.dve(lambda e: e.memset(C(22), 0.0), r=['sm'], w=['sm'])
        for h in range(2):
            p.dve(lambda e, h=h: e.scalar_tensor_tensor(qkE[:, h, :], Sps[:, h, :], 1.0, Ew[:, h, :], ALU.mult, ALU.mult, accum_out=sm[:, 22 + h:23 + h]),
                  r=['Ew', 'sm'], w=['qkE', 'sm', 'bA'])
        for h in range(2):
            p.pe(lambda e, h=h: e.transpose(psE[:, h, :], qkE[:, h, :], id64), r=['qkE', 'cst'], w=['bA'])
        p.act(lambda e: e.copy(qkET[:], psE[:]), r=[], w=['qkET', 'bA'])
        for h in range(2):
            p.pe(lambda e, h=h: e.matmul(nps[:, h, :], qkET[:, h, :], v1[:, h, 0:128], start=True, stop=True), r=['qkET', 'v1'], w=['bB'])
            p.pe(lambda e, h=h: e.matmul(ips[:, h, :], qkT[:, h, :], CNbf[h][:], start=True, stop=True), r=['qkT', 'CNbf%d' % h], w=['bD'])
        for h in range(2):
            p.dve(lambda e, h=h: e.scalar_tensor_tensor(sm[:, 24 + h:25 + h], ips[:, h, 128:129], sm[:, 18 + h:19 + h], sm[:, 22 + h:23 + h], ALU.mult, ALU.add),
                  r=['sm'], w=['sm', 'bD'])
        p.dve(lambda e: e.tensor_scalar(C(32), C(24), -1.0, None, ALU.mult), r=['sm'], w=['sm'])
        p.dve(lambda e: e.tensor_tensor(C(24), C(24), C(32), ALU.max), r=['sm'], w=['sm'])
        p.dve(lambda e: e.tensor_tensor(C(24), C(24), C(20), ALU.max), r=['sm'], w=['sm'])
        p.dve(lambda e: e.reciprocal(C(26), C(24)), r=['sm'], w=['sm'])
        p.dve(lambda e: e.tensor_tensor(C(28), C(18), C(26), ALU.mult), r=['sm'], w=['sm'])
        for h in range(2):
            p.act(lambda e, h=h: e.activation(ins_[:, h, :], ips[:, h, 0:128], AF.Identity, scale=sm[:, 28 + h:29 + h]), r=['sm'], w=['ins', 'bD'])
            p.dve(lambda e, h=h: e.scalar_tensor_tensor(hout[:, h * 128:(h + 1) * 128], nps[:, h, :], sm[:, 26 + h:27 + h], ins_[:, h, :], ALU.mult, ALU.add),
                  r=['sm', 'ins'], w=['hout', 'bB'])
        p.dve(lambda e: e.reduce_max(C8(2), Rps[:], AX.X), r=[], w=['sm128', 'bA'])
        p.dve(lambda e: e.tensor_tensor(C8(4), C8(2), C8(0), ALU.add), r=['sm128'], w=['sm128'])
        p.dve(lambda e: e.tensor_tensor(C8(6), C8(0), mm[:], ALU.add), r=['sm128', 'mm'], w=['sm128'])
        p.dve(lambda e: e.tensor_tensor(C8(8), C8(6), C8(4), ALU.max), r=['sm128'], w=['sm128'])
        p.dve(lambda e: e.tensor_tensor(C8(10), C8(6), C8(8), ALU.subtract), r=['sm128'], w=['sm128'])
        p.act(lambda e: e.activation(C8(12), C8(10), AF.Exp), r=['sm128'], w=['sm128'])
        p.dve(lambda e: e.tensor_tensor(C8(14), C8(0), C8(8), ALU.subtract), r=['sm128'], w=['sm128'])
        p.dve(lambda e: e.tensor_tensor(C(30), C(6), sm128[0:64, 14:16], ALU.add), r=['sm', 'sm128'], w=['sm'])
        p.act(lambda e: e.activation(C(30), C(30), AF.Exp), r=['sm'], w=['sm'])
        for h in range(2):
            p.dve(lambda e, h=h: e.tensor_scalar(kw[:, h, :], ks[:, h * 128:(h + 1) * 128], sm[:, 30 + h:31 + h], None, ALU.mult), r=['ks', 'sm'], w=['kw'])
        for h in range(2):
            p.pe(lambda e, h=h: e.matmul(ups1, kw[:, h, :], v1[:, h, :], start=True, stop=True), r=['kw', 'v1'], w=['bD'])
            p.dve(lambda e, h=h: e.scalar_tensor_tensor(CN32[h][:], CN32[h][:], sm128[:, 12 + h:13 + h], ups1, ALU.mult, ALU.add),
                  r=['CN32%d' % h, 'sm128'], w=['CN32%d' % h, 'bD'])
            p.act(lambda e, h=h: e.copy(CNbf[h][:], CN32[h][:]), r=['CN32%d' % h], w=['CNbf%d' % h])
        p.dve(lambda e: e.tensor_copy(mm[:], C8(8)), r=['sm128'], w=['mm'])
        if not epi:
            p.dma(k.stash[srow:srow + 64, own0:own0 + 256], hout[:], r=['hout'], w=[('glob', 'st', 'l', srow)])
        else:
            p.dma(hf[b][:], k.stash[srow:srow + 64, oth0:oth0 + 256], r=[('glob', 'st', 'l', srow)], w=['hf%d' % b])
            p.dve(lambda e, b=b: e.tensor_tensor(hout[:], hout[:], hf[b][:], ALU.add), r=['hout', 'hf%d' % b], w=['hout'])
            p.act(lambda e, b=b: e.activation(so[:], mn[b][:, 1024:1280], AF.Sigmoid), r=[MN], w=['so'])
            p.act(lambda e, b=b: e.activation(sz[:], mn[b][:, 768:1024], AF.Silu), r=[MN], w=['sz'])
            p.dve(lambda e: e.tensor_tensor(so[:], so[:], sz[:], ALU.mult), r=['so', 'sz'], w=['so'])
            p.dve(lambda e: e.tensor_tensor(yb[:], hout[:], so[:], ALU.mult), r=['hout', 'so'], w=['yb'])
            ydst, ykey = yrows(k, kind, c)
            p.dma(ydst[:, 256:512], yb[:], r=['yb'], w=[('glob',) + ykey + ('b',)])


def phase_na(k, l):
    for pair in ((0, 1), (2, 3)):
        s = Scope(k.p)
        k.p.interleave([lambda n=n: na_head(k, l, n, s) for n in pair])
        s.end()
    dbg_y(k, l, 512, 1024)


def na_head(k, l, n, s):
    p = k.p
    SC = 128.0 ** -0.5
    idb = s.sb('c_idb', [128, 128], BF16)
    QKT = s.sb('c_QKT', [128, 2, L + LC], BF16)
    V = s.sb('c_V', [128, 34, 128], BF16)
    Vt32 = s.sb('c_Vt32', [128, 5, 128], F32)
    Vt = s.sb('c_Vt', [128, 5, 128], BF16)
    BT = s.sb('c_BT', [128, 5, 576], F32)
    qkv = [s.sb('c_qkv%d' % i, [128, 3, 128], F32) for i in range(2)]
    idf = s.sb('c_idf', [128, 128], F32)
    zt = [s.sb('c_zt%d' % i, [128, 128], F32) for i in range(2)]
    sm = s.sb('c_sm', [128, 832], F32)
    Pm = s.sb('c_Pm', [128, 832], BF16)
    PT = s.sb('c_PT', [128, 7, 128], BF16)
    st = s.sb('c_st', [128, 4], F32)
    sz = s.sb('c_sz', [128, 128], F32)
    yo = s.sb('c_yo', [128, 128], BF16)
    Sa = s.ps('c_Sa', [128, 512], F32)
    Sb = s.ps('c_Sb', [128, 512], F32)
    psP = s.ps('c_psP', [128, 7, 128], BF16)
    bO = s.ps('c_bO', [128, 512], F32)
    O = bO[:, 0:128]
    psT = bO[:, 128:384].rearrange("p (a b) -> p a b", a=2)

    p.dma(idb[:], k.ident, w=['idb'], eng='pool')
    p.dma(idf[:], k.ident, w=['idf'])
    p.dma(BT[:], k.natab[l, n].rearrange("t q c -> q t c"), w=['BT'])
    vcol = C_NA + 1024 + n * 128
    zcol = C_NA + 1536 + n * 128
    p.dma(Vt32[:, 0:4, :], k.P_lat[3520:4032, vcol:vcol + 128].rearrange("(t q) c -> q t c", q=128), w=['Vt32'])
    p.dma(Vt32[0:64, 4, :], k.P_lat[4032:4096, vcol:vcol + 128], w=['Vt32'])
    p.dve(lambda e: e.tensor_copy(Vt[:, 0:4, :], Vt32[:, 0:4, :]), r=['Vt32'], w=['Vt'])
    p.dve(lambda e: e.tensor_copy(Vt[0:64, 4, :], Vt32[0:64, 4, :]), r=['Vt32', 'Vt'], w=['Vt'])
    for i in range(34):
        b = i % 2
        src = k.P_lat[i * 128:(i + 1) * 128, :] if i < 32 else k.P_ctx[(i - 32) * 128:(i - 31) * 128, :]
        v3 = src[:, C_NA:C_NA + 1536].rearrange("q (a hh c) -> q a hh c", a=3, hh=4)[:, :, n, :]
        p.dma(qkv[b][:], v3, w=['qkv%d' % b])
        p.act(lambda e, b=b, i=i: e.copy(V[:, i, :], qkv[b][:, 2, :]), r=['qkv%d' % b], w=['V'])
        for a in range(2):
            p.pe(lambda e, a=a, b=b: e.transpose(psT[:, a, :], qkv[b][:, a, :], idf[:]), r=['qkv%d' % b, 'idf'], w=['O'])
        p.act(lambda e, i=i: e.copy(QKT[:, :, i * 128:(i + 1) * 128], psT[:]), r=[], w=['QKT', 'O'])

    ntile = 34 if l == 0 else 32
    for it, rp in enumerate(range(ntile)):
        b = it % 2
        lat = rp < 32
        q_ap = QKT[:, 0, rp * 128:(rp + 1) * 128]
        if lat:
            ti = {0: 0, 1: 1, 30: 3, 31: 4}.get(rp, 2)
            rs_lo = min(max(2 * rp - 4, 0), 55)
            ks0 = rs_lo * 64
            p.dma(zt[b][:], k.P_lat[rp * 128:(rp + 1) * 128, zcol:zcol + 128], w=['zt%d' % b])
            p.pe(lambda e, q_ap=q_ap, ks0=ks0: e.matmul(Sa[:], q_ap, QKT[:, 1, ks0:ks0 + 512], start=True, stop=True), r=['QKT'], w=['Sa'])
            p.pe(lambda e, q_ap=q_ap, ks0=ks0: e.matmul(Sb[:, 0:64], q_ap, QKT[:, 1, ks0 + 512:ks0 + 576], start=True, stop=True), r=['QKT'], w=['Sb'])
            p.pe(lambda e, q_ap=q_ap: e.matmul(Sb[:, 64:320], q_ap, QKT[:, 1, L:L + LC], start=True, stop=True), r=['QKT'], w=['Sb'])
            p.dve(lambda e, ti=ti: e.scalar_tensor_tensor(sm[:, 0:512], Sa[:], SC, BT[:, ti, 0:512], ALU.mult, ALU.add), r=['BT'], w=['sm', 'Sa'])
            p.dve(lambda e, ti=ti: e.scalar_tensor_tensor(sm[:, 512:576], Sb[:, 0:64], SC, BT[:, ti, 512:576], ALU.mult, ALU.add), r=['BT'], w=['sm', 'Sb'])
            p.act(lambda e: e.activation(sm[:, 576:832], Sb[:, 64:320], AF.Copy, scale=SC), r=[], w=['sm', 'Sb'])
            nk = 832
        else:
            cq = rp - 32
            p.dma(zt[b][:], k.P_ctx[cq * 128:(cq + 1) * 128, zcol:zcol + 128], w=['zt%d' % b])
            p.pe(lambda e, q_ap=q_ap: e.matmul(Sb[:, 64:320], q_ap, QKT[:, 1, L:L + LC], start=True, stop=True), r=['QKT'], w=['Sb'])
            p.act(lambda e: e.activation(sm[:, 0:256], Sb[:, 64:320], AF.Copy, scale=SC), r=[], w=['sm', 'Sb'])
            nk = 256
        p.dve(lambda e, nk=nk: e.reduce_max(st[:, 0:1], sm[:, 0:nk], AX.X), r=['sm'], w=['st'])
        p.dve(lambda e: e.tensor_scalar(st[:, 1:2], st[:, 0:1], -1.0, None, ALU.mult), r=['st'], w=['st'])
        p.dve(lambda e: e.memset(st[:, 2:3], 0.0), r=['st'], w=['st'])
        p.act(lambda e, nk=nk: e.activation(Pm[:, 0:nk], sm[:, 0:nk], AF.Exp, bias=st[:, 1:2], accum_out=st[:, 2:3]), r=['sm', 'st'], w=['Pm', 'st'])
        p.dve(lambda e: e.reciprocal(st[:, 3:4], st[:, 2:3]), r=['st'], w=['st'])
        if lat:
            blocks = [(i, i * 128, 128) for i in range(4)] + [(4, 512, 64), (5, 576, 128), (6, 704, 128)]
        else:
            blocks = [(5, 0, 128), (6, 128, 128)]
        for (bi, c0, w_) in blocks:
            p.pe(lambda e, bi=bi, c0=c0, w_=w_: e.transpose(psP[0:w_, bi, :], Pm[:, c0:c0 + w_], idb[:]), r=['Pm', 'idb'], w=['psP'])
        if lat:
            p.act(lambda e: e.copy(PT[:, 0:4, :], psP[:, 0:4, :]), r=[], w=['PT', 'psP'])
            p.dve(lambda e: e.tensor_copy(PT[0:64, 4, :], psP[0:64, 4, :]), r=[], w=['PT', 'psP'])
        p.dve(lambda e: e.tensor_copy(PT[:, 5:7, :], psP[:, 5:7, :]), r=[], w=['PT', 'psP'])
        if lat:
            aligned = (ks0 % 128 == 0)
            for i in range(4):
                rhs = V[:, ks0 // 128 + i, :] if aligned else Vt[:, i, :]
                p.pe(lambda e, i=i, rhs=rhs: e.matmul(O[:], PT[:, i, :], rhs, start=(i == 0), stop=False), r=['PT', 'V', 'Vt'], w=['O'])
            rhs = V[0:64, ks0 // 128 + 4, :] if aligned else Vt[0:64, 4, :]
            p.pe(lambda e, rhs=rhs: e.matmul(O[:], PT[0:64, 4, :], rhs, start=False, stop=False), r=['PT', 'V', 'Vt'], w=['O'])
        for i in (5, 6):
            p.pe(lambda e, i=i, lat=lat: e.matmul(O[:], PT[:, i, :], V[:, 32 + (i - 5), :], start=((not lat) and i == 5), stop=(i == 6)), r=['PT', 'V'], w=['O'])
        p.act(lambda e, b=b: e.activation(sz[:], zt[b][:], AF.Silu), r=['zt%d' % b], w=['sz'])
        p.dve(lambda e: e.scalar_tensor_tensor(yo[:], O[:], st[:, 3:4], sz[:], ALU.mult, ALU.mult), r=['st', 'sz'], w=['yo', 'O'])
        if lat:
            ydst = k.y_loc[rp % 8][(rp // 8) * 128:(rp // 8) * 128 + 128, 512 + n * 128:512 + (n + 1) * 128]
        else:
            ydst = k.y_loc[8][(rp - 32) * 128:(rp - 31) * 128, 512 + n * 128:512 + (n + 1) * 128]
        p.dma(ydst, yo[:], r=['yo'], w=[('glob', 'yc', rp, n)])


def transpose_tiles(k, s, loader, XT, ntiles, tag):
    p = k.p
    xt = [s.sb('tt_x%s%d' % (tag, i), [128, D], BF16) for i in range(2)]
    idb = s.sb('tt_idb' + tag, [128, 128], BF16)
    pst = [s.ps('tt_ps%s%d' % (tag, i), [128, 8, 128], BF16) for i in range(2)]
    p.dma(idb[:], k.ident, w=['tt_idb'], eng='pool')
    for t in range(ntiles):
        rows = 128 if t < 8 else 64
        b = t % 2
        loader(t, xt[b], 'tt_x%d' % b)
        for g in range(4):
            pb = (t * 4 + g) % 2
            for i in range(8):
                kk = g * 8 + i
                p.pe(lambda e, pb=pb, i=i, kk=kk, rows=rows, b=b: e.transpose(pst[pb][:, i, :rows], xt[b][:rows, kk * 128:(kk + 1) * 128], idb[:rows, :rows]),
                     r=['tt_x%d' % b, 'tt_idb'], w=['tt_ps%d' % pb])
            if g % 2 == 0:
                p.act(lambda e, pb=pb, g=g, t=t, rows=rows: e.copy(XT[:, g * 8:(g + 1) * 8, t * 128:t * 128 + rows], pst[pb][:, :, :rows]), r=[], w=['XT' + tag, 'tt_ps%d' % pb])
            else:
                p.dve(lambda e, pb=pb, g=g, t=t, rows=rows: e.tensor_copy(XT[:, g * 8:(g + 1) * 8, t * 128:t * 128 + rows], pst[pb][:, :, :rows]), r=[], w=['XT' + tag, 'tt_ps%d' % pb])


def phase_b(k, l, last):
    p = k.p
    ntiles = 8 if last else NT
    for t in range(NT):
        allgather(k, k.y_loc[t], k.y_all[t], [], [('y_all', t)])

    s = Scope(p)
    yTs = s.sb('b_yTs', [128, 32, NT * 128], BF16)

    def load_y(t, dst, key):
        rows = 128 if t < 8 else 64
        view = k.y_all[t].rearrange("(r j i) c -> j i r c", r=4, j=4)

        def fn(e, view=view, dst=dst, rows=rows):
            if 'rank' not in p.body_cache:
                p.body_cache['rank'] = e.partition_id() % 4
            rank = p.body_cache['rank']
            return e.dma_start(out=dst[:rows, :].rearrange("q (r c) -> q r c", r=4), in_=view[rank])
        p.add('sp', fn, [('y_all', t)], [key], dma=True)

    transpose_tiles(k, s, load_y, yTs, ntiles, 'y')
    wp = [s.sb('b_wp%d' % i, [128, 16, 512], BF16) for i in range(2)]
    ub = s.sb('b_ub', [128, NT, 512], F32)
    ubf = [s.sb('b_ubf%d' % i, [128, 512], BF16) for i in range(2)]
    Gl = [s.sb('b_Gl%d' % i, [128, 512], BF16) for i in range(3)]
    tmp = s.sb('b_tmp', [128, 512], F32)
    ps = [s.ps('b_pp%d' % i, [128, 512], F32) for i in range(4)]
    wsrc = (k.w_pa, k.w_pb, k.w_pc)
    kmap = ([((hd // 2) * 8 + hd % 2, hd) for hd in range(8)],
            [((hd // 2) * 8 + 2 + hd % 2, hd) for hd in range(8)],
            [((n // 4) * 8 + 4 + n % 4, n) for n in range(16)])
    it = 0
    wi = 0
    for cb in range(8):
        for kb in range(3):
            nk = len(kmap[kb])
            w_ = wi % 2
            wi += 1
            p.dma(wp[w_][:, 0:nk, :], wsrc[kb][l][:, cb * 512:(cb + 1) * 512].rearrange("(kk pp) c -> pp kk c", pp=128), w=['pw%d' % w_], eng='pool')
            for t in range(ntiles):
                rows = 128 if t < 8 else 64
                pi, gi = it % 4, it % 3
                it += 1
                g0 = ((t * 3 + kb) * 8 + cb) * 128
                p.dma(Gl[gi][:rows, :], k.G_scr[g0:g0 + rows, :], w=['Gl%d' % gi])
                for qi, (kk, wr) in enumerate(kmap[kb]):
                    p.pe(lambda e, pi=pi, w_=w_, kk=kk, wr=wr, t=t, rows=rows, qi=qi, nk=nk: e.matmul(ps[pi][:rows, :], yTs[:, kk, t * 128:t * 128 + rows], wp[w_][:, wr, :], start=(qi == 0), stop=(qi == nk - 1)),
                         r=['XTy', 'pw%d' % w_], w=['pps%d' % pi])
                if kb == 0:
                    p.dve(lambda e, pi=pi, gi=gi, t=t, rows=rows: e.tensor_tensor(ub[:rows, t, :], ps[pi][:rows, :], Gl[gi][:rows, :], ALU.mult), r=['Gl%d' % gi], w=[('ub', t), 'pps%d' % pi])
                else:
                    p.dve(lambda e, pi=pi, gi=gi, rows=rows: e.tensor_tensor(tmp[:rows, :], ps[pi][:rows, :], Gl[gi][:rows, :], ALU.mult), r=['Gl%d' % gi], w=['btmp', 'pps%d' % pi])
                    if kb == 1:
                        p.dve(lambda e, t=t, rows=rows: e.tensor_tensor(ub[:rows, t, :], ub[:rows, t, :], tmp[:rows, :], ALU.add), r=['btmp', ('ub', t)], w=[('ub', t)])
                    else:
                        ui = t % 2
                        p.dve(lambda e, t=t, rows=rows, ui=ui: e.tensor_tensor(ubf[ui][:rows, :], ub[:rows, t, :], tmp[:rows, :], ALU.add), r=['btmp', ('ub', t)], w=['ubf%d' % ui])
                        p.dma(k.u_scr[t * 128:t * 128 + rows, cb * 512:(cb + 1) * 512], ubf[ui][:rows, :], r=['ubf%d' % ui], w=[('u', t, cb)])
    s.end()

    s = Scope(p)
    uTs = s.sb('b_uTs', [128, 32, NT * 128], BF16)

    def load_u(t, dst, key):
        rows = 128 if t < 8 else 64
        p.dma(dst[:rows, :], k.u_scr[t * 128:t * 128 + rows, :], w=[key])

    transpose_tiles(k, s, load_u, uTs, ntiles, 'u')
    wo = [s.sb('b_wo%d' % i, [128, 32, 512], BF16) for i in range(2)]
    gate = [s.sb('b_gate%d' % m, [128, D], F32) for m in range(2)]
    xin = [s.sb('b_xin%d' % i, [128, 512], F32) for i in range(3)]
    xo = [s.sb('b_xo%d' % i, [128, 512], F32) for i in range(3)]
    ps = [s.ps('b_po%d' % i, [128, 512], F32) for i in range(4)]
    for m in range(2):
        p.dma(gate[m][:].rearrange("q (r c) -> q r c", r=4), modvec_bc(k, l, m, 2), w=['gate%d' % m])
    xsrc = k.x_tok if l == 0 else k.x_loc
    it = 0
    for cb in range(8):
        w_ = cb % 2
        p.dma(wo[w_][:], k.w_out[l][:, cb * 512:(cb + 1) * 512].rearrange("(kk pp) c -> pp kk c", pp=128), w=['ow%d' % w_], eng='pool')
        for t in range(ntiles):
            rows = 128 if t < 8 else 64
            m = 0 if t < 8 else 1
            pi, xi = it % 4, it % 3
            it += 1
            p.dma(xin[xi][:rows, :], xsrc[t * 128:t * 128 + rows, cb * 512:(cb + 1) * 512], r=[('x', t, cb)], w=['xin%d' % xi])
            for kk in range(32):
                p.pe(lambda e, pi=pi, w_=w_, kk=kk, t=t, rows=rows: e.matmul(ps[pi][:rows, :], uTs[:, kk, t * 128:t * 128 + rows], wo[w_][:, kk, :], start=(kk == 0), stop=(kk == 31)),
                     r=['XTu', 'ow%d' % w_], w=['ops%d' % pi])
            p.dve(lambda e, pi=pi, xi=xi, rows=rows, m=m, cb=cb: e.tensor_tensor(xo[xi][:rows, :], ps[pi][:rows, :], gate[m][:rows, cb * 512:(cb + 1) * 512], ALU.mult),
                  r=['gate%d' % m], w=['xo%d' % xi, 'ops%d' % pi])
            p.dve(lambda e, xi=xi, rows=rows: e.tensor_tensor(xo[xi][:rows, :], xo[xi][:rows, :], xin[xi][:rows, :], ALU.add), r=['xo%d' % xi, 'xin%d' % xi], w=['xo%d' % xi])
            p.dma(k.x_loc[t * 128:t * 128 + rows, cb * 512:(cb + 1) * 512], xo[xi][:rows, :], r=['xo%d' % xi], w=[('x', t, cb)])
    s.end()


def phase_final(k):
    p = k.p
    s = Scope(p)
    fg = s.sb('f_g', [128, D], F32)
    xt = [s.sb('f_x%d' % i, [128, D], F32) for i in range(2)]
    xo = [s.sb('f_o%d' % i, [128, D], F32) for i in range(2)]
    junk = s.sb('f_junk', [128, D], BF16)
    st = s.sb('f_st', [128, 4], F32)
    p.dma(fg[:], k.final_g.partition_broadcast(128).rearrange("q a d -> q (a d)"), w=['fg'])
    for t in range(8):
        b = t % 2
        p.dma(xt[b][:], k.x_loc[t * 128:(t + 1) * 128, :], w=['fx%d' % b])
        p.dve(lambda e: e.memset(st[:], 0.0), w=['fst'])
        p.act(lambda e, b=b: e.activation(junk[:], xt[b][:], AF.Square, accum_out=st[:, 0:1]), r=['fx%d' % b, 'fst'], w=['fjunk', 'fst'])
        p.dve(lambda e: e.tensor_scalar(st[:, 1:2], st[:, 0:1], 1.0 / D, EPS, ALU.mult, ALU.add), r=['fst'], w=['fst'])
        p.act(lambda e: e.activation(st[:, 1:2], st[:, 1:2], AF.Sqrt), r=['fst'], w=['fst'])
        p.dve(lambda e: e.reciprocal(st[:, 2:3], st[:, 1:2]), r=['fst'], w=['fst'])
        p.dve(lambda e, b=b: e.scalar_tensor_tensor(xo[b][:], xt[b][:], st[:, 2:3], fg[:], ALU.mult, ALU.mult), r=['fx%d' % b, 'fst', 'fg'], w=['fo%d' % b])
        p.dma(k.out[t * 128:(t + 1) * 128, :], xo[b][:], r=['fo%d' % b], w=[('out', t)])
    s.end()


def kernel(**inputs):
    inp = {kk: np.asarray(v) for kk, v in inputs.items()}
    maps = prep(inp)
    nc = build_nc()
    maps = [{kk: m[kk] for kk in nc.declared_inputs} for m in maps]
    res = run_bass_kernel_spmd(nc, maps, core_ids=list(range(8)))
    out = np.zeros((2, L, D), np.float32)
    for i in range(8):
        b, j = i // 4, i % 4
        out[b, j * 1024:(j + 1) * 1024] = res.results[i]['out']
    return out
```

```python
from contextlib import ExitStack
import numpy as np
import ml_dtypes
import concourse.bass as bass
import concourse.mybir as mybir
from concourse.bass_utils import run_bass_kernel_spmd

F32 = mybir.dt.float32
BF16 = mybir.dt.bfloat16
AF = mybir.ActivationFunctionType
ALU = mybir.AluOpType
AX = mybir.AxisListType

D = 4096
L = 4096
LC = 256
NT = 9
NTOK = 1088
EPS = 1e-6
D_IN = 16448
NCOL = 4136
C_GLA, C_ML, C_NA, C_DEC, C_GAT = 0, 768, 2048, 4096, 4128
NEG = -30000.0
GROUPS = [[0, 1, 2, 3], [4, 5, 6, 7]]

ENGS = ('pe', 'dve', 'act', 'pool', 'sp')
ENGOBJ = {'pe': 'tensor', 'dve': 'vector', 'act': 'scalar', 'pool': 'gpsimd', 'sp': 'sync'}
NDMASEM = 8
SEM_ROLL = 30000
import os
SAME_ENGINE_SYNC = not os.environ.get('NO_SES')


class Op:
    __slots__ = ('eng', 'fn', 'deps', 'signal', 'tok', 'is_dma', 'dsem', 'prev_dma', 'inc')

    def __init__(self, eng, fn, is_dma, inc):
        self.eng, self.fn, self.is_dma, self.inc = eng, fn, is_dma, inc
        self.deps = []
        self.signal = False
        self.tok = None
        self.dsem = None
        self.prev_dma = None


class Prog:
    def __init__(self, nc):
        self.nc = nc
        self.q = {e: [] for e in ENGS}
        self.state = {}
        self.ndma = {e: 0 for e in ENGS}
        self.dma_hist = {e: [] for e in ENGS}
        self.es = ExitStack()
        self.esem = {e: [] for e in ENGS}
        self.ecnt = {e: SEM_ROLL for e in ENGS}
        self.dsem = {}
        self.dcnt = {}
        self.seen = {e: {} for e in ENGS}
        self.last = {e: None for e in ENGS}
        self.nops = 0
        self.rec = None
        self.rec_prefix = None

    def sem(self, name):
        return self.es.enter_context(self.nc.semaphore(name))

    def add(self, eng, fn, reads=(), writes=(), dma=False, inc=16, semq=None):
        if self.rec is not None:
            pf = self.rec_prefix
            fix = lambda kk: kk if (isinstance(kk, tuple) and kk and kk[0] == 'glob') else (pf, kk)
            self.rec.append((eng, fn, tuple(fix(x) for x in reads), tuple(fix(x) for x in writes), dma, inc, semq))
            return None
        for kk in reads:
            if isinstance(kk, tuple) and kk and kk[0] == 'glob' and kk[1] == 'st':
                assert kk in self.state, ('stash read before write', kk)
        op = Op(eng, fn, dma, inc)
        deps = []
        for k in reads:
            st = self.state.get(k)
            if st is not None:
                deps.extend(st[0])
        for k in writes:
            st = self.state.get(k)
            if st is not None:
                deps.extend(st[0])
                deps.extend(st[1])
        seen = set()
        for d in deps:
            if id(d) in seen:
                continue
            seen.add(id(d))
            if (not d.is_dma) and d.eng == eng and (eng == 'pe' or not SAME_ENGINE_SYNC):
                continue
            op.deps.append(d)
            d.signal = True
        for k in reads:
            st = self.state.setdefault(k, [[], []])
            if not dma:
                st[1] = [r for r in st[1] if r.is_dma or r.eng != eng]
            st[1].append(op)
        for k in writes:
            self.state[k] = [[op], []]
        if dma:
            sq = semq or eng
            n = self.ndma.setdefault(sq, 0)
            self.ndma[sq] = n + 1
            op.dsem = ('dma_' + sq, n % NDMASEM)
            hist = self.dma_hist.setdefault(sq, [])
            if n >= NDMASEM:
                op.prev_dma = hist[n - NDMASEM]
            hist.append(op)
        self.q[eng].append(op)
        self.nops += 1
        return op

    def pe(self, fn, r=(), w=()):
        return self.add('pe', fn, r, w)

    def dve(self, fn, r=(), w=()):
        return self.add('dve', fn, r, w)

    def act(self, fn, r=(), w=()):
        return self.add('act', fn, r, w)

    def pool(self, fn, r=(), w=()):
        return self.add('pool', fn, r, w)

    def dma(self, out, in_, r=(), w=(), eng='sp'):
        return self.add(eng, lambda e: e.dma_start(out=out, in_=in_), r, w, dma=True)

    def flush(self):
        lasts = []
        for e in ENGS:
            ops = [o for o in self.q[e] if (not o.is_dma) and o.fn is not None]
            if ops:
                ops[-1].signal = True
                lasts.append(ops[-1])
        dmas = []
        for sq in self.dma_hist:
            hist = self.dma_hist[sq]
            dmas.extend(hist[-NDMASEM:])
        for e in ENGS:
            b = Op(e, None, False, 0)
            b.deps = [o for o in lasts if o.eng != e] + dmas
            self.q[e].append(b)
        nc = self.nc
        for e in ENGS:
            for op in self.q[e]:
                if op.fn is None:
                    continue
                if op.is_dma:
                    if op.dsem not in self.dsem:
                        self.dsem[op.dsem] = self.sem('d_%s_%d' % op.dsem)
                        self.dcnt[op.dsem] = 0
                    v = self.dcnt[op.dsem] + op.inc
                    self.dcnt[op.dsem] = v
                    op.tok = (op.dsem, v, self.dsem[op.dsem])
                elif op.signal:
                    if self.ecnt[e] >= SEM_ROLL:
                        self.esem[e].append(self.sem('s_%s_%d' % (e, len(self.esem[e]))))
                        self.ecnt[e] = 0
                    self.ecnt[e] += 1
                    op.tok = ((e, len(self.esem[e])), self.ecnt[e], self.esem[e][-1])
        with nc.Block() as block:
            for e in ENGS:
                ops = self.q[e]

                def body(eng, ops=ops, e=e):
                    self.body_cache = {}
                    seen = self.seen[e]
                    for op in ops:
                        deps = list(op.deps)
                        if op.prev_dma is not None:
                            deps.append(op.prev_dma)
                        for dd in deps:
                            key, val, sem = dd.tok
                            if seen.get(key, 0) >= val:
                                continue
                            seen[key] = val
                            eng.wait_ge(sem, val)
                        if op.fn is None:
                            continue
                        ins = op.fn(eng)
                        if op.tok is not None:
                            ins.then_inc(op.tok[2], op.inc if op.is_dma else 1)

                getattr(block, ENGOBJ[e])(body)
        self.q = {e: [] for e in ENGS}
        self.state = {}

    def interleave(self, fns, weights=None):
        recs = []
        for i, fn in enumerate(fns):
            self.rec, self.rec_prefix = [], 'il%d' % i
            fn()
            recs.append(self.rec)
        self.rec = None
        weights = weights or [1] * len(recs)
        pos = [0] * len(recs)
        while any(pos[j] < len(recs[j]) for j in range(len(recs))):
            for j, r in enumerate(recs):
                for _ in range(weights[j]):
                    if pos[j] < len(r):
                        self.add(*r[pos[j]])
                        pos[j] += 1

    def close(self):
        self.es.close()


class Scope:
    def __init__(self, p):
        self.p = p
        self.es = ExitStack()

    CNT = [0]

    def sb(self, name, shape, dt):
        Scope.CNT[0] += 1
        return self.es.enter_context(self.p.nc.sbuf_tensor('%s_%d' % (name, Scope.CNT[0]), list(shape), dt))

    def ps(self, name, shape, dt=F32):
        Scope.CNT[0] += 1
        full = 512 if dt == F32 else 1024
        t = self.es.enter_context(self.p.nc.psum_tensor('%s_%d' % (name, Scope.CNT[0]), [shape[0], full], dt))
        n = 1
        for d_ in shape[1:]:
            n *= d_
        v = t[:, 0:n]
        if len(shape) == 3:
            v = v.rearrange("p (a b) -> p a b", a=shape[1])
        return v

    def end(self):
        self.p.flush()
        self.es.close()


IN_SPLITS = (512, 512, 1024, 1024, 32, 1024, 1024, 1024, 1024, 1024, 32, 2048, 2048, 2048, 2048)
OFFS = np.concatenate([[0], np.cumsum(IN_SPLITS)])


def my_cols(j):
    A_q, A_k, A_v, A_z, A_d, B_q, B_k, B_v, B_z, B_o, B_g, C_q, C_k, C_v, C_z = OFFS[:15]
    cols = []
    hs = (2 * j, 2 * j + 1)
    for base in (A_q, A_k):
        for h in hs:
            cols.append(np.arange(64) + base + h * 64)
    for base in (A_v, A_z):
        for h in hs:
            cols.append(np.arange(128) + base + h * 128)
    for base in (B_q, B_k, B_v, B_z, B_o):
        for h in hs:
            cols.append(np.arange(128) + base + h * 128)
    for base in (C_q, C_k, C_v, C_z):
        for h in range(4 * j, 4 * j + 4):
            cols.append(np.arange(128) + base + h * 128)
    cols.append(np.arange(32) + A_d)
    for t in range(4):
        for h in hs:
            cols.append(np.array([B_g + t * 8 + h]))
    c = np.concatenate(cols)
    assert c.shape[0] == NCOL
    return c


def na_tables(rpb_l, j):
    out = np.full((4, 5, 128, 576), NEG, np.float32)
    reps = [0, 1, 2, 30, 31]
    cc = np.arange(64)
    cstart = np.clip(cc - 8, 0, 48)
    for ti, rp in enumerate(reps):
        rs_lo = int(np.clip(2 * rp - 4, 0, 55))
        for jj in range(2):
            r = 2 * rp + jj
            rs = int(np.clip(r - 4, 0, 56))
            for a in range(9):
                kr = rs_lo + a
                if kr < rs or kr >= rs + 8 or kr > 63:
                    continue
                dr = kr - r + 7
                for c in range(64):
                    cp = np.arange(cstart[c], cstart[c] + 16)
                    dc = cp - c + 15
                    for hh in range(4):
                        out[hh, ti, jj * 64 + c, a * 64 + cp] = rpb_l[4 * j + hh, dr, dc]
    return out


def rope_table():
    nf = 16
    inv = (10000.0 ** (-np.arange(nf, dtype=np.float32) / nf)).astype(np.float32)
    tab = np.zeros((64, 64, 2, 8, 16), np.float32)
    for c in range(64):
        ang_r = (np.float32(c) * inv).astype(np.float32)
        ang_c = (np.arange(64, dtype=np.float32)[:, None] * inv[None, :]).astype(np.float32)
        for b8 in range(8):
            if b8 % 2 == 0:
                tab[c, :, 0, b8, :] = np.cos(ang_r)[None, :]
                tab[c, :, 1, b8, :] = np.sin(ang_r)[None, :]
            else:
                tab[c, :, 0, b8, :] = np.cos(ang_c)
                tab[c, :, 1, b8, :] = np.sin(ang_c)
    return tab.reshape(64, 64, 256)


def const_tables():
    s = np.arange(64)[:, None]
    t = np.arange(64)[None, :]
    lo = (s <= t).astype(np.float32)
    up = (s >= t).astype(np.float32)
    c = np.zeros((64, 12, 64), np.float32)
    c[:, 0] = lo
    c[:, 1] = up
    c[:, 2] = lo * (-1.0 / 16)
    c[:, 3] = up * (-1.0 / 16)
    c[:, 4] = -1.0 / 16
    c[:, 5] = -lo
    c[:, 6] = -up
    c[:, 7] = np.where(t <= s, 0.0, NEG)
    c[:, 8] = np.where(t >= s, 0.0, NEG)
    c[:, 9] = np.eye(64, dtype=np.float32)
    c[:, 10] = 1.0
    c[:, 11] = -1.0
    return c


def prep(inp):
    f = np.float32
    x, c, ctx, c_ctx = inp['x'], inp['c'], inp['ctx'], inp['c_ctx']
    rope = rope_table()
    cst = const_tables()
    ident = np.eye(128, dtype=f)
    maps = []
    for i in range(8):
        b, j = i // 4, i % 4
        m = {}
        m['x_tok'] = np.ascontiguousarray(np.concatenate([x[b, j * 1024:(j + 1) * 1024], ctx[b, j * 64:(j + 1) * 64]], 0))
        c2 = np.stack([c[b], c_ctx], 0)
        m['c2T'] = np.ascontiguousarray(c2.reshape(2, 32, 128).transpose(2, 1, 0))
        mc = np.concatenate([np.arange(1024) + part * 4096 + j * 1024 for part in range(3)])
        m['w_mod_s'] = np.ascontiguousarray(inp['w_mod'][:, :, mc])
        m['b_mod_s'] = np.ascontiguousarray(np.repeat(inp['b_mod'][:, None, mc], 2, axis=1))
        m['norm_g'] = inp['norm_g']
        m['final_g'] = inp['final_g'].reshape(1, D)
        cols = my_cols(j)
        m['w_in_s'] = np.ascontiguousarray(inp['w_in'][:, :, cols])
        hc = np.concatenate([np.arange(64) + h * 64 for h in (2 * j, 2 * j + 1)])
        wal = np.zeros((2, 2, 17, 128), f)
        wal[:, :, :16, :] = inp['w_alpha2'][:, :, :, hc]
        wal[:, :, 16, :] = inp['b_alpha'][:, :, hc]
        m['w_al'] = wal
        m['gla_g'] = np.ascontiguousarray(inp['gla_norm_g'][:, 2 * j * 128:(2 * j + 2) * 128])
        m['b_gates_s'] = np.ascontiguousarray(inp['b_gates'][:, :, 2 * j:2 * j + 2].reshape(2, 8))
        qc = np.concatenate([np.arange(256) + 2 * j * 128, np.arange(256) + 1024 + 2 * j * 128])
        m['conv_w_s'] = np.ascontiguousarray(inp['conv_w'][:, :, qc])
        m['conv_b_s'] = np.ascontiguousarray(inp['conv_b'][:, qc])
        m['natab'] = np.stack([na_tables(inp['rpb'][l], j) for l in range(2)], 0)
        m['rope'] = rope
        m['cst'] = cst
        m['ident'] = ident
        m['w_merge'] = inp['w_merge']
        m['b_merge'] = inp['b_merge']
        m['w_pa'] = inp['w_proj_a']
        m['w_pb'] = inp['w_proj_b']
        m['w_pc'] = inp['w_proj_c']
        m['w_out'] = inp['w_out']
        maps.append(m)
    return maps


class K:
    def __getattr__(self, name):
        specs = self.__dict__.get('_specs', {})
        if name in specs:
            shape, dt = specs[name]
            ap = self.nc.dram_tensor(name, list(shape), dt, kind="ExternalInput").ap()
            self.__dict__[name] = ap
            self.declared.append(name)
            return ap
        raise AttributeError(name)


def build_nc(stop_after=None, dbg=False, nlayers=2, only=None):
    nc = bass.Bass("TRN2", target_bir_lowering=False)
    k = K()
    k.nc = nc
    k.dbg = dbg
    k.only = only

    order = ['M', 'N0', 'A0', 'G0', 'L0', 'C0', 'B0', 'N1', 'A1', 'G1', 'L1', 'C1', 'B1', 'F']
    upto = len(order) if stop_after is None else order.index(stop_after) + 1
    need = {'c2T': 'M', 'w_mod_s': 'M', 'b_mod_s': 'M', 'x_tok': 'N0', 'norm_g': 'N0', 'ident': 'N0', 'w_in_s': 'A0',
            'w_al': 'G0', 'gla_g': 'G0', 'rope': 'G0', 'cst': 'G0', 'b_gates_s': 'L0', 'conv_w_s': 'L0', 'conv_b_s': 'L0',
            'natab': 'C0', 'w_merge': 'B0', 'b_merge': 'B0', 'w_pa': 'B0', 'w_pb': 'B0', 'w_pc': 'B0', 'w_out': 'B0', 'final_g': 'F'}
    k.declared = []

    k._specs = {}

    def din(name, shape, dt=F32):
        k._specs[name] = (shape, dt)
        return None

    def dint(name, shape, dt=F32):
        return nc.dram_tensor(name, list(shape), dt, kind="ExternalOutput" if (dbg and name in DBG_OUT) else "Internal").ap()

    din('x_tok', [NTOK, D])
    din('c2T', [128, 32, 2])
    din('w_mod_s', [2, D, 3072])
    din('b_mod_s', [2, 2, 3072])
    din('norm_g', [2, D])
    din('final_g', [1, D])
    din('w_in_s', [2, D, NCOL])
    din('w_al', [2, 2, 17, 128])
    din('gla_g', [2, 256])
    din('b_gates_s', [2, 8])
    din('conv_w_s', [2, 3, 512])
    din('conv_b_s', [2, 512])
    din('natab', [2, 4, 5, 128, 576])
    din('rope', [64, 64, 256])
    din('cst', [64, 12, 64])
    din('ident', [128, 128])
    din('w_merge', [2, D, 3 * D])
    din('b_merge', [2, 3 * D])
    din('w_pa', [2, 1024, D])
    din('w_pb', [2, 1024, D])
    din('w_pc', [2, 2048, D])
    din('w_out', [2, D, D])
    k.out = nc.dram_tensor('out', [1024, D], F32, kind="ExternalOutput").ap()

    k.mod_loc = [dint('mod_loc%d' % l, [2, 3, 1024]) for l in range(2)]
    k.mod_all = [dint('mod_all%d' % l, [4, 2, 3, 1024]) for l in range(2)]
    k.hT_loc = [dint('hT_loc%d' % t, [128, 32 * 128], BF16) for t in range(NT)]
    k.hT_all = [dint('hT_all%d' % t, [4 * 128, 32 * 128], BF16) for t in range(NT)]
    if only is not None:
        k.P_lat = nc.dram_tensor('P_lat', [L, NCOL], F32, kind="ExternalInput").ap()
        k.P_ctx = nc.dram_tensor('P_ctx', [LC, NCOL], F32, kind="ExternalInput").ap()
        k.declared += ['P_lat', 'P_ctx']
    else:
        k.P_lat = dint('P_lat', [L, NCOL])
        k.P_ctx = dint('P_ctx', [LC, NCOL])
    k.stash = dint('stash', [L + LC, 1024])
    k.y_loc = [dint('y_loc%d' % t, [512 if t < 8 else 256, 1024], BF16) for t in range(NT)]
    k.y_all = [dint('y_all%d' % t, [4 * (512 if t < 8 else 256), 1024], BF16) for t in range(NT)]
    if dbg:
        k.y_dbg = nc.dram_tensor('y_dbg', [NT * 512, 1024], BF16, kind='ExternalOutput').ap()
    k.G_scr = dint('G_scr', [NT * 3 * 8 * 128, 512], BF16)
    k.x_loc = dint('x_loc', [NTOK, D])
    k.u_scr = dint('u_scr', [NTOK, D], BF16)
    if dbg:
        k.xn_dbg = nc.dram_tensor('xn_dbg', [128, D], F32, kind='ExternalOutput').ap()
        k.hn_dbg = nc.dram_tensor('hn_dbg', [128, D], BF16, kind='ExternalOutput').ap()
        k.hTt_dbg = nc.dram_tensor('hTt_dbg', [128, D], BF16, kind='ExternalOutput').ap()
        k.A_dbg = nc.dram_tensor('A_dbg', [128, D], F32, kind='ExternalOutput').ap()
        k.mod_dbg = nc.dram_tensor('mod_dbg', [48, 1024], F32, kind='ExternalOutput').ap()
        k.hT_dbg = nc.dram_tensor('hT_dbg', [NT * 512, 4096], BF16, kind='ExternalOutput').ap()

    p = Prog(nc)
    k.p = p
    phases = []
    phases.append(('M', lambda: phase_mod(k)))
    for l in range(nlayers):
        phases.append(('N%d' % l, lambda l=l: phase_norm(k, l)))
        phases.append(('A%d' % l, lambda l=l: phase_proj(k, l)))
        phases.append(('G%d' % l, lambda l=l: phase_gla(k, l)))
        phases.append(('L%d' % l, lambda l=l: phase_mlstm(k, l)))
        phases.append(('C%d' % l, lambda l=l: phase_na(k, l)))
        phases.append(('B%d' % l, lambda l=l: phase_b(k, l, last=(l == 1))))
    phases.append(('F', lambda: phase_final(k)))
    for name, fn in phases:
        if only is not None and name != only:
            continue
        fn()
        if stop_after == name:
            break
    p.flush()
    p.close()
    nc.declared_inputs = k.declared
    return nc


DBG_OUT = ('P_lat', 'P_ctx', 'x_loc', 'stash', 'y_dbg')


def allgather(k, src, dst, rkeys, wkeys):
    k.p.add('pool', lambda e: e.collective_compute("AllGather", ALU.bypass, replica_groups=GROUPS, ins=[src], outs=[dst]),
            rkeys, wkeys, dma=True, inc=1, semq='cc')


def mod_stream(k, s, layers):
    p = k.p
    c2 = s.sb('m_c2', [128, 32, 2], F32)
    cs = s.sb('m_cs', [128, 32, 2], BF16)
    bm = s.sb('m_bm', [2, 2 * 3072], F32)
    mo = s.sb('m_mo', [2, 2 * 3072], F32)
    l0 = layers[0]
    wm = [s.sb('m_wm%d' % i, [128, 32, 512], BF16) for i in range(2)]
    ps = [s.ps('m_ps%d' % i, [2, 512], F32) for i in range(2)]
    p.dma(c2[:], k.c2T, w=['c2'])
    p.dma(bm[:].rearrange("m (l c) -> m l c", l=2), k.b_mod_s.rearrange("l m c -> m l c"), w=['bm'])
    p.act(lambda e: e.activation(cs[:], c2[:], AF.Silu), r=['c2'], w=['cs'])
    it = 0
    for l in layers:
        for cb in range(6):
            b = it % 2
            it += 1
            p.dma(wm[b][:], k.w_mod_s[l][:, cb * 512:(cb + 1) * 512].rearrange("(kk pp) c -> pp kk c", pp=128),
                  w=['wm%d' % b], eng='pool')
            for kk in range(32):
                p.pe(lambda e, b=b, kk=kk: e.matmul(ps[b][:], cs[:, kk, :], wm[b][:, kk, :], start=(kk == 0), stop=(kk == 31)),
                     r=['cs', 'wm%d' % b], w=['mps%d' % b])
            o = l * 3072 + cb * 512
            p.dve(lambda e, b=b, o=o: e.tensor_tensor(mo[:, o:o + 512], ps[b][:], bm[:, o:o + 512], ALU.add),
                  r=['mps%d' % b, 'bm'], w=['mo'])
    for l in layers:
        p.dma(k.mod_loc[l].rearrange("m a c -> m (a c)"), mo[:, l * 3072:(l + 1) * 3072], r=['mo'], w=[('mod_loc', l)])
        allgather(k, k.mod_loc[l].rearrange("m a c -> (m a) c"), k.mod_all[l].rearrange("r m a c -> (r m a) c"), [('mod_loc', l)], [('mod_all', l)])


def phase_mod(k):
    s = Scope(k.p)
    mod_stream(k, s, [0])
    s.end()


def modvec_bc(k, l, m, a):
    v = k.mod_all[l].rearrange("r m a c -> m a r c")[m, a]
    return v.partition_broadcast(128)


def phase_norm(k, l):
    p = k.p
    s = Scope(p)
    A = [s.sb('n_A%d' % m, [128, D], F32) for m in range(2)]
    S = [s.sb('n_S%d' % m, [128, D], F32) for m in range(2)]
    gbc = s.sb('n_g', [128, D], F32)
    xt = [s.sb('n_x%d' % i, [128, D], F32) for i in range(2)]
    xn2 = [s.sb('n_xn%d' % i, [128, D], F32) for i in range(2)]
    hn2 = [s.sb('n_hn%d' % i, [128, D], BF16) for i in range(2)]
    junk = s.sb('n_junk', [128, D], BF16)
    st2 = [s.sb('n_st%d' % i, [128, 4], F32) for i in range(2)]
    hT = [s.sb('n_hT%d' % i, [128, 32, 128], BF16) for i in range(2)]
    idb = s.sb('n_idb', [128, 128], BF16)
    pst = [s.ps('n_ps%d' % i, [128, 8, 128], BF16) for i in range(2)]
    p.dma(idb[:], k.ident, w=['idb'], eng='pool')
    p.dma(gbc[:], k.norm_g[l].partition_broadcast(128), w=['gbc'])
    for m in range(2):
        p.dma(A[m][:].rearrange("p (r c) -> p r c", r=4), modvec_bc(k, l, m, 1), w=['A%d' % m])
        p.dma(S[m][:].rearrange("p (r c) -> p r c", r=4), modvec_bc(k, l, m, 0), w=['S%d' % m])
        p.dve(lambda e, m=m: e.scalar_tensor_tensor(A[m][:], A[m][:], 1.0, gbc[:], ALU.add, ALU.mult), r=['A%d' % m, 'gbc'], w=['A%d' % m])
    for i in range(2):
        p.dve(lambda e, i=i: e.memset(hT[i][:], 0.0), w=['hT%d' % i])
    xsrc = k.x_tok if l == 0 else k.x_loc
    for t in range(NT):
        rows = 128 if t < 8 else 64
        m = 0 if t < 8 else 1
        b = t % 2
        xn, hn, st = xn2[b], hn2[b], st2[b]
        XN, HN, ST = 'xn%d' % b, 'hn%d' % b, 'st%d' % b
        p.dma(xt[b][:rows], xsrc[t * 128:t * 128 + rows, :], w=['xt%d' % b])
        p.dve(lambda e, st=st: e.memset(st[:], 0.0), w=[ST])
        p.act(lambda e, b=b, rows=rows, st=st: e.activation(junk[:rows], xt[b][:rows], AF.Square, accum_out=st[:rows, 0:1]), r=['xt%d' % b, ST], w=['junk', ST])
        p.dve(lambda e, rows=rows, st=st: e.tensor_scalar(st[:rows, 1:2], st[:rows, 0:1], 1.0 / D, EPS, ALU.mult, ALU.add), r=[ST], w=[ST])
        p.act(lambda e, rows=rows, st=st: e.activation(st[:rows, 1:2], st[:rows, 1:2], AF.Sqrt), r=[ST], w=[ST])
        p.dve(lambda e, rows=rows, st=st: e.reciprocal(st[:rows, 2:3], st[:rows, 1:2]), r=[ST], w=[ST])
        p.dve(lambda e, b=b, rows=rows, m=m, st=st, xn=xn: e.scalar_tensor_tensor(xn[:rows], xt[b][:rows], st[:rows, 2:3], A[m][:rows], ALU.mult, ALU.mult),
              r=['xt%d' % b, ST, 'A%d' % m], w=[XN])
        p.dve(lambda e, rows=rows, m=m, hn=hn, xn=xn: e.tensor_tensor(hn[:rows], xn[:rows], S[m][:rows], ALU.add), r=[XN, 'S%d' % m], w=[HN])
        for g in range(4):
            pb = (t * 4 + g) % 2
            for i in range(8):
                kk = g * 8 + i
                p.pe(lambda e, pb=pb, i=i, kk=kk, rows=rows, hn=hn: e.transpose(pst[pb][:, i, :rows], hn[:rows, kk * 128:(kk + 1) * 128], idb[:rows, :rows]),
                     r=[HN, 'idb'], w=['pst%d' % pb])
            if g % 2 == 0:
                p.act(lambda e, pb=pb, g=g, b=b, rows=rows: e.copy(hT[b][:, g * 8:(g + 1) * 8, :rows], pst[pb][:, :, :rows]), r=['pst%d' % pb], w=['hT%d' % b])
            else:
                p.dve(lambda e, pb=pb, g=g, b=b, rows=rows: e.tensor_copy(hT[b][:, g * 8:(g + 1) * 8, :rows], pst[pb][:, :, :rows]), r=['pst%d' % pb], w=['hT%d' % b])
        p.dma(k.hT_loc[t], hT[b][:].rearrange("p a b -> p (a b)"), r=['hT%d' % b], w=[('hT_loc', t)])
        if k.dbg and l == 0 and t == 0:
            p.dma(k.xn_dbg, xn[:], r=[XN], w=['d1'])
            p.dma(k.hn_dbg, hn[:], r=[HN], w=['d2'])
            p.dma(k.hTt_dbg, hT[b][:].rearrange("p a b -> p (a b)"), r=['hT%d' % b], w=['d3'])
            p.dma(k.A_dbg, A[0][:], r=['A0'], w=['d4'])
        allgather(k, k.hT_loc[t], k.hT_all[t], [('hT_loc', t)], [('hT_all', t)])
        if k.dbg and l == 0:
            p.dma(k.hT_dbg[t * 512:(t + 1) * 512, :], k.hT_all[t], r=[('hT_all', t)], w=[('hTd', t)])
    if k.dbg and l == 0:
        pass

    s.end()


def proj_stream(k, l, s, blocks, nps, tag):
    p = k.p
    wb = [s.sb('a_w%s%d' % (tag, i), [128, 32, 512], BF16) for i in range(2)]
    ht = [s.sb('a_h%s%d' % (tag, i), [128, 32, 128], BF16) for i in range(3)]
    ob = [s.sb('a_o%s%d' % (tag, i), [128, 512], F32) for i in range(3)]
    ps = [s.ps('a_ps%s%d' % (tag, i), [128, 512], F32) for i in range(nps)]
    it = 0
    for bi, nb in enumerate(blocks):
        c0 = nb * 512
        n = 512 if nb < 8 else NCOL - 4096
        wi = bi % 2
        p.dma(wb[wi][:, :, :n], k.w_in_s[l][:, c0:c0 + n].rearrange("(kk pp) c -> pp kk c", pp=128), w=['aw%d' % wi], eng='pool')
        for r in range(4):
            for t in range(NT):
                rows = 128 if t < 8 else 64
                hi, oi, pi = it % 3, it % 3, it % nps
                it += 1
                p.dma(ht[hi][:].rearrange("p a b -> p (a b)"), k.hT_all[t][r * 128:(r + 1) * 128, :], w=['ah%d' % hi])
                for kk in range(32):
                    p.pe(lambda e, pi=pi, hi=hi, wi=wi, kk=kk, rows=rows, n=n: e.matmul(ps[pi][:rows, :n], ht[hi][:, kk, :rows], wb[wi][:, kk, :n], start=(kk == 0), stop=(kk == 31)),
                         r=['ah%d' % hi, 'aw%d' % wi], w=['aps%d' % pi])
                if it % 2 == 0:
                    p.act(lambda e, oi=oi, pi=pi, rows=rows, n=n: e.copy(ob[oi][:rows, :n], ps[pi][:rows, :n]), r=[], w=['ao%d' % oi, 'aps%d' % pi])
                else:
                    p.dve(lambda e, oi=oi, pi=pi, rows=rows, n=n: e.tensor_copy(ob[oi][:rows, :n], ps[pi][:rows, :n]), r=[], w=['ao%d' % oi, 'aps%d' % pi])
                if t < 8:
                    dst = k.P_lat[r * 1024 + t * 128: r * 1024 + t * 128 + 128, c0:c0 + n]
                else:
                    dst = k.P_ctx[r * 64:(r + 1) * 64, c0:c0 + n]
                p.dma(dst, ob[oi][:rows, :n], r=['ao%d' % oi], w=[('P', r, t, nb)], eng='act')


def phase_proj(k, l):
    s = Scope(k.p)
    fns = [lambda: proj_stream(k, l, s, [0, 1, 2, 3, 8], 4, 'p1')]
    if l == 0:
        fns.append(lambda: mod_stream(k, s, [1]))
    k.p.interleave(fns)
    s.end()


def gates_stream(k, l, s, ntiles):
    p = k.p
    hTs = s.sb('b_hTs', [128, 32, NT * 128], BF16)
    wb = [s.sb('b_wb%d' % i, [128, 32, 512], BF16) for i in range(2)]
    bms = [s.sb('b_bm%d' % i, [1, 512], BF16) for i in range(2)]
    onesr = s.sb('b_ones', [1, 128], BF16)
    Gsb = [s.sb('b_G%d' % i, [128, 512], BF16) for i in range(3)]
    ps = [s.ps('b_ps%d' % i, [128, 512], F32) for i in range(2)]
    p.dve(lambda e: e.memset(onesr[:], 1.0), w=['onesr'])
    for t in range(ntiles):
        p.dma(hTs[:, :, t * 128:(t + 1) * 128], k.hT_loc[t].rearrange("q (a b) -> q a b", a=32), w=[('hTs', t)])
    it = 0
    wi = 0
    for cb in range(8):
        for kb in range(3):
            c0 = kb * D + cb * 512
            w_ = wi % 2
            wi += 1
            p.dma(wb[w_][:], k.w_merge[l][:, c0:c0 + 512].rearrange("(kk pp) c -> pp kk c", pp=128), w=['bw%d' % w_], eng='pool')
            p.dma(bms[w_][:], k.b_merge[l:l + 1, c0:c0 + 512], w=['bm%d' % w_], eng='pool')
            for t in range(ntiles):
                rows = 128 if t < 8 else 64
                pi, gi = it % 2, it % 3
                it += 1
                for kk in range(32):
                    p.pe(lambda e, pi=pi, w_=w_, kk=kk, t=t, rows=rows: e.matmul(ps[pi][:rows, :], hTs[:, kk, t * 128:t * 128 + rows], wb[w_][:, kk, :], start=(kk == 0), stop=False),
                         r=[('hTs', t), 'bw%d' % w_], w=['bps%d' % pi])
                p.pe(lambda e, pi=pi, rows=rows, w_=w_: e.matmul(ps[pi][:rows, :], onesr[:, :rows], bms[w_][:], start=False, stop=True), r=['onesr', 'bm%d' % w_], w=['bps%d' % pi])
                p.act(lambda e, pi=pi, gi=gi, rows=rows: e.activation(Gsb[gi][:rows, :], ps[pi][:rows, :], AF.Sigmoid), r=[], w=['bG%d' % gi, 'bps%d' % pi])
                g0 = ((t * 3 + kb) * 8 + cb) * 128
                p.dma(k.G_scr[g0:g0 + rows, :], Gsb[gi][:rows, :], r=['bG%d' % gi], w=[('G', t, kb, cb)])


def yrows(k, kind, c, n=64):
    if kind == 'lat':
        t = (c % 16) // 2
        r0 = (c // 16) * 128 + (c % 2) * 64
        return k.y_loc[t][r0:r0 + n, :], ('y', t, c // 16, c % 2)
    return k.y_loc[8][c * 64:c * 64 + n, :], ('y', 8, c, 0)


def dbg_y(k, l, c0, c1):
    if k.dbg and l == 0:
        for t in range(NT):
            n = 512 if t < 8 else 256
            k.p.dma(k.y_dbg[t * 512:t * 512 + n, c0:c1], k.y_loc[t][:, c0:c1], w=[('yd', t)])
        k.p.flush()


def chunk_order(d):
    ctx = [('ctx', c) for c in range(4)]
    lat = [('lat', c) for c in range(64)]
    if d == 1:
        ctx.reverse()
        lat.reverse()
    return ctx + lat


def chunk_pos(d):
    return {kc: i for i, kc in enumerate(chunk_order(d))}


def phase_gla(k, l):
    s = Scope(k.p)
    fns = [lambda d=d: gla_sweep(k, l, d, s) for d in range(2)]
    if k.only is None:
        fns.append(lambda: gates_stream(k, l, s, 8 if l == 1 else NT))
    k.p.interleave(fns, [1, 1, 2] if len(fns) == 3 else None)
    s.end()
    dbg_y(k, l, 0, 256)


def gla_sweep(k, l, d, s):
    p = k.p
    mypos, otpos = chunk_pos(d), chunk_pos(1 - d)
    own0, oth0 = (0, 512) if d == 0 else (512, 0)
    cst = s.sb('g_cst', [64, 12, 64], F32)
    waug = s.sb('g_waug', [17, 128], F32)
    gng = s.sb('g_gng', [64, 256], F32)
    mask2 = s.sb('g_mask2', [64, 2, 64], F32)
    S32 = [s.sb('g_S32%d' % h, [64, 128], F32) for h in range(2)]
    Sbf = [s.sb('g_Sbf%d' % h, [64, 128], BF16) for h in range(2)]
    aaT = s.sb('g_aaT', [17, 64], F32)
    pa = [s.sb('g_pa%d' % i, [64, 768], F32) for i in range(2)]
    dec = [s.sb('g_dec%d' % i, [64, 16], F32) for i in range(2)]
    rt = [s.sb('g_rt%d' % i, [64, 256], F32) for i in range(2)]
    of = [s.sb('g_of%d' % i, [64, 256], F32) for i in range(2)]
    e1 = s.sb('g_e1', [64, 128], F32)
    sp = s.sb('g_sp', [64, 128], F32)
    bs = s.sb('g_bs', [64, 128], F32)
    Ep = s.sb('g_Ep', [64, 128], F32)
    Em = s.sb('g_Em', [64, 128], F32)
    dlt = s.sb('g_dlt', [64, 128], F32)
    Eh = s.sb('g_Eh', [64, 128], F32)
    decs = s.sb('g_decs', [64, 2], F32)
    tt = [s.sb('g_t%d' % i, [64, 8, 16], F32) for i in range(4)]
    qkr = s.sb('g_qkr', [64, 256], F32)
    qt = s.sb('g_qt', [64, 128], F32)
    kt = s.sb('g_kt', [64, 128], F32)
    kh = s.sb('g_kh', [64, 128], BF16)
    vbf = s.sb('g_vbf', [64, 256], BF16)
    qkT = s.sb('g_qkT', [64, 4, 64], BF16)
    attm = s.sb('g_attm', [64, 2, 64], BF16)
    osb = s.sb('g_osb', [64, 256], F32)
    junk = s.sb('g_junk', [64, 128], F32)
    ss = s.sb('g_ss', [64, 4], F32)
    sz = s.sb('g_sz', [64, 256], F32)
    tn = s.sb('g_tn', [64, 256], F32)
    ya = s.sb('g_ya', [64, 256], BF16)
    T1 = s.ps('g_T1', [64, 512], F32)
    PA = s.ps('g_PA', [64, 512], F32)
    psT = PA[:, 0:256].rearrange("p (a b) -> p a b", a=4)
    att = PA[:, 256:384].rearrange("p (a b) -> p a b", a=2)
    OK = s.ps('g_OK', [64, 512], F32)
    ops = OK[:, 0:256].rearrange("p (a b) -> p a b", a=2)
    kvp = OK[:, 256:512].rearrange("p (a b) -> p a b", a=2)

    p.dma(cst[:], k.cst, w=['cst'])
    p.dma(waug[:], k.w_al[l, d], w=['waug'])
    p.dma(gng[:], k.gla_g[l].partition_broadcast(64), w=['gng'])
    for h in range(2):
        p.dma(mask2[:, h, :], k.cst[:, d, :], w=['mask2'])
        p.dve(lambda e, h=h: e.memset(S32[h][:], 0.0), w=['S32%d' % h])
        p.dve(lambda e, h=h: e.memset(Sbf[h][:], 0.0), w=['Sbf%d' % h])
    p.dve(lambda e: e.memset(aaT[:], 1.0), w=['aaT'])
    TriD = cst[:, 2 + d, :]
    All16 = cst[:, 4, :]
    id64 = cst[:, 9, :]

    for it, (kind, c) in enumerate(chunk_order(d)):
        b = it % 2
        src = k.P_lat if kind == 'lat' else k.P_ctx
        r0 = c * 64
        srow = r0 if kind == 'lat' else L + r0
        P = 'pa%d' % b
        p.dma(pa[b][:], src[r0:r0 + 64, C_GLA:C_GLA + 768], w=[P])
        p.dma(dec[b][:], src[r0:r0 + 64, C_DEC + 16 * d:C_DEC + 16 * d + 16], w=['dec%d' % b])
        if kind == 'lat':
            p.dma(rt[b][:], k.rope[c], w=['rt%d' % b])
        epi = mypos[(kind, c)] > otpos[(kind, c)]
        p.pe(lambda e, b=b: e.transpose(T1[0:16, 448:512], dec[b][:], id64), r=['dec%d' % b, 'cst'], w=['T1'])
        p.act(lambda e: e.copy(aaT[0:16, :], T1[0:16, 448:512]), r=[], w=['aaT', 'T1'])
        p.pe(lambda e: e.matmul(T1[:, 0:128], aaT[:], waug[:], start=True, stop=True), r=['aaT', 'waug'], w=['T1'])
        p.act(lambda e: e.activation(e1[:], T1[:, 0:128], AF.Exp, scale=-1.0), r=[], w=['e1', 'T1'])
        p.act(lambda e: e.activation(sp[:], e1[:], AF.Ln, bias=1.0), r=['e1'], w=['sp'])
        p.pe(lambda e: e.matmul(T1[:, 128:256], TriD, sp[:], start=True, stop=True), r=['sp', 'cst'], w=['T1'])
        p.pe(lambda e: e.matmul(T1[:, 256:384], All16, sp[:], start=True, stop=True), r=['sp', 'cst'], w=['T1'])
        for h in range(2):
            p.pe(lambda e, h=h: e.matmul(T1[:, 384 + h:385 + h], sp[:, h * 64:(h + 1) * 64], cst[:, 4, 0:1], start=True, stop=True), r=['sp', 'cst'], w=['T1'])
        p.dve(lambda e: e.tensor_copy(bs[:], T1[:, 128:256]), r=[], w=['bs', 'T1'])
        p.act(lambda e: e.activation(Ep[:], bs[:], AF.Exp), r=['bs'], w=['Ep'])
        p.act(lambda e: e.activation(Em[:], bs[:], AF.Exp, scale=-1.0), r=['bs'], w=['Em'])
        p.dve(lambda e: e.tensor_tensor(dlt[:], T1[:, 256:384], bs[:], ALU.subtract), r=['bs'], w=['dlt', 'T1'])
        p.act(lambda e: e.activation(Eh[:], dlt[:], AF.Exp), r=['dlt'], w=['Eh'])
        p.act(lambda e: e.activation(decs[:], T1[:, 384:386], AF.Exp), r=[], w=['decs', 'T1'])
        if kind == 'lat':
            x4 = pa[b][:, 0:256].rearrange("p (a h f) -> p a h f", a=8, h=2)
            cos = rt[b][:, 0:128].rearrange("p (a f) -> p a f", a=8)
            sin = rt[b][:, 128:256].rearrange("p (a f) -> p a f", a=8)
            o4 = qkr[:].rearrange("p (a h f) -> p a h f", a=8, h=2)
            R = [P, 'rt%d' % b]
            p.dve(lambda e, x4=x4, cos=cos: e.tensor_tensor(tt[0][:], x4[:, :, 0, :], cos, ALU.mult), r=R, w=['t0'])
            p.dve(lambda e, x4=x4, sin=sin: e.tensor_tensor(tt[1][:], x4[:, :, 1, :], sin, ALU.mult), r=R, w=['t1'])
            p.dve(lambda e, o4=o4: e.tensor_tensor(o4[:, :, 0, :], tt[0][:], tt[1][:], ALU.subtract), r=['t0', 't1'], w=['qkr'])
            p.dve(lambda e, x4=x4, sin=sin: e.tensor_tensor(tt[2][:], x4[:, :, 0, :], sin, ALU.mult), r=R, w=['t2'])
            p.dve(lambda e, x4=x4, cos=cos: e.tensor_tensor(tt[3][:], x4[:, :, 1, :], cos, ALU.mult), r=R, w=['t3'])
            p.dve(lambda e, o4=o4: e.tensor_tensor(o4[:, :, 1, :], tt[2][:], tt[3][:], ALU.add), r=['t2', 't3', 'qkr'], w=['qkr'])
            qsrc, QK = qkr, 'qkr'
        else:
            qsrc, QK = pa[b], P
        p.dve(lambda e, qsrc=qsrc: e.scalar_tensor_tensor(qt[:], qsrc[:, 0:128], 0.125, Ep[:], ALU.mult, ALU.mult), r=[QK, 'Ep'], w=['qt'])
        p.dve(lambda e, qsrc=qsrc: e.tensor_tensor(kt[:], qsrc[:, 128:256], Em[:], ALU.mult), r=[QK, 'Em'], w=['kt'])
        p.dve(lambda e, qsrc=qsrc: e.tensor_tensor(kh[:], qsrc[:, 128:256], Eh[:], ALU.mult), r=[QK, 'Eh'], w=['kh'])
        p.act(lambda e, b=b: e.copy(vbf[:], pa[b][:, 256:512]), r=[P], w=['vbf'])
        for i in range(4):
            srcT = qt if i < 2 else kt
            p.pe(lambda e, i=i, srcT=srcT: e.transpose(psT[:, i, :], srcT[:, (i % 2) * 64:(i % 2) * 64 + 64], id64), r=['qt', 'kt', 'cst'], w=['PA'])
        p.act(lambda e: e.copy(qkT[:], psT[:]), r=[], w=['qkT', 'PA'])
        for h in range(2):
            p.pe(lambda e, h=h: e.matmul(att[:, h, :], qkT[:, 2 + h, :], qkT[:, h, :], start=True, stop=True), r=['qkT'], w=['PA'])
        p.dve(lambda e: e.tensor_tensor(attm[:], att[:], mask2[:], ALU.mult), r=['mask2'], w=['attm', 'PA'])
        for h in range(2):
            p.pe(lambda e, h=h: e.matmul(ops[:, h, :], attm[:, h, :], vbf[:, h * 128:(h + 1) * 128], start=True, stop=False), r=['attm', 'vbf'], w=['OK'])
            p.pe(lambda e, h=h: e.matmul(ops[:, h, :], qkT[:, h, :], Sbf[h][:], start=False, stop=True), r=['qkT', 'Sbf%d' % h], w=['OK'])
        for h in range(2):
            p.pe(lambda e, h=h: e.matmul(kvp[:, h, :], kh[:, h * 64:(h + 1) * 64], vbf[:, h * 128:(h + 1) * 128], start=True, stop=True), r=['kh', 'vbf'], w=['OK'])
        for h in range(2):
            p.dve(lambda e, h=h: e.scalar_tensor_tensor(S32[h][:], S32[h][:], decs[:, h:h + 1], kvp[:, h, :], ALU.mult, ALU.add), r=['S32%d' % h, 'decs'], w=['S32%d' % h, 'OK'])
            p.act(lambda e, h=h: e.copy(Sbf[h][:], S32[h][:]), r=['S32%d' % h], w=['Sbf%d' % h])
        if not epi:
            p.act(lambda e: e.copy(osb[:], ops[:].rearrange("p a b -> p (a b)")), r=[], w=['osb', 'OK'])
            p.dma(k.stash[srow:srow + 64, own0:own0 + 256], osb[:], r=['osb'], w=[('glob', 'st', 'g', srow)])
        else:
            p.dma(of[b][:], k.stash[srow:srow + 64, oth0:oth0 + 256], r=[('glob', 'st', 'g', srow)], w=['of%d' % b])
            p.dve(lambda e, b=b: e.tensor_tensor(osb[:], ops[:].rearrange("p a b -> p (a b)"), of[b][:], ALU.add), r=['of%d' % b], w=['osb', 'OK'])
            p.dve(lambda e: e.memset(ss[:], 0.0), w=['ss'])
            for h in range(2):
                p.act(lambda e, h=h: e.activation(junk[:], osb[:, h * 128:(h + 1) * 128], AF.Square, accum_out=ss[:, h:h + 1]), r=['osb', 'ss'], w=['junk', 'ss'])
            p.dve(lambda e: e.tensor_scalar(ss[:, 2:4], ss[:, 0:2], 1.0 / 128, EPS, ALU.mult, ALU.add), r=['ss'], w=['ss'])
            p.act(lambda e: e.activation(ss[:, 2:4], ss[:, 2:4], AF.Sqrt), r=['ss'], w=['ss'])
            p.dve(lambda e: e.reciprocal(ss[:, 2:4], ss[:, 2:4]), r=['ss'], w=['ss'])
            p.act(lambda e, b=b: e.activation(sz[:], pa[b][:, 512:768], AF.Silu), r=[P], w=['sz'])
            for h in range(2):
                p.dve(lambda e, h=h: e.scalar_tensor_tensor(tn[:, h * 128:(h + 1) * 128], osb[:, h * 128:(h + 1) * 128], ss[:, 2 + h:3 + h], gng[:, h * 128:(h + 1) * 128], ALU.mult, ALU.mult),
                      r=['osb', 'ss', 'gng'], w=['tn'])
            p.dve(lambda e: e.tensor_tensor(ya[:], tn[:], sz[:], ALU.mult), r=['tn', 'sz'], w=['ya'])
            ydst, ykey = yrows(k, kind, c)
            p.dma(ydst[:, 0:256], ya[:], r=['ya'], w=[('glob',) + ykey + ('a',)])


def phase_mlstm(k, l):
    s = Scope(k.p)
    fns = [lambda d=d: mlstm_sweep(k, l, d, s) for d in range(2)]
    if k.only is None:
        fns.append(lambda: proj_stream(k, l, s, [4, 5, 6, 7], 2, 'p2'))
    k.p.interleave(fns)
    s.end()
    dbg_y(k, l, 256, 512)


def mlstm_sweep(k, l, d, s):
    p = k.p
    mypos, otpos = chunk_pos(d), chunk_pos(1 - d)
    own0, oth0 = (256, 768) if d == 0 else (768, 256)
    cst = s.sb('l_cst', [64, 12, 64], F32)
    convw = s.sb('l_convw', [64, 3, 512], F32)
    convb = s.sb('l_convb', [64, 512], F32)
    bg = s.sb('l_bg', [64, 8], F32)
    ones = s.sb('l_ones', [64, 128], F32)
    nones = s.sb('l_nones', [64, 128], F32)
    CN32 = [s.sb('l_CN32%d' % h, [128, 129], F32) for h in range(2)]
    CNbf = [s.sb('l_CNbf%d' % h, [128, 129], BF16) for h in range(2)]
    mm = s.sb('l_mm', [128, 2], F32)
    v1 = s.sb('l_v1', [64, 2, 129], BF16)
    mn = [s.sb('l_mn%d' % i, [64, 1280], F32) for i in range(2)]
    pv = [s.sb('l_pv%d' % i, [64, 512], F32) for i in range(2)]
    nx = [s.sb('l_nx%d' % i, [64, 512], F32) for i in range(2)]
    gt = [s.sb('l_gt%d' % i, [64, 8], F32) for i in range(2)]
    hf = [s.sb('l_hf%d' % i, [64, 256], F32) for i in range(2)]
    ta = s.sb('l_ta', [64, 512], F32)
    tb = s.sb('l_tb', [64, 512], F32)
    sl = s.sb('l_sl', [64, 512], F32)
    qs = s.sb('l_qs', [64, 256], F32)
    ks = s.sb('l_ks', [64, 256], F32)
    qkT = s.sb('l_qkT', [128, 4, 64], BF16)
    g = s.sb('l_g', [64, 8], F32)
    sm = s.sb('l_sm', [64, 40], F32)
    sm128 = s.sb('l_sm128', [128, 16], F32)
    diag = [s.sb('l_diag%d' % h, [64, 64], F32) for h in range(2)]
    Dm = s.sb('l_D', [64, 2, 64], F32)
    Ew = s.sb('l_Ew', [64, 2, 64], F32)
    qkE = s.sb('l_qkE', [64, 2, 64], F32)
    qkET = s.sb('l_qkET', [64, 2, 64], BF16)
    ins_ = s.sb('l_ins', [64, 2, 128], F32)
    hout = s.sb('l_hout', [64, 256], F32)
    kw = s.sb('l_kw', [64, 2, 128], BF16)
    so = s.sb('l_so', [64, 256], F32)
    sz = s.sb('l_sz', [64, 256], F32)
    yb = s.sb('l_yb', [64, 256], BF16)
    bA = s.ps('l_bA', [128, 512], F32)
    bB = s.ps('l_bB', [128, 512], F32)
    bD = s.ps('l_bD', [128, 512], F32)
    Tg = bA[:, 0:16]
    Rps = bA[:, 16:144].rearrange("p (a b) -> p a b", a=2)
    psE = bA[0:64, 144:272].rearrange("p (a b) -> p a b", a=2)
    Sps = bA[0:64, 272:400].rearrange("p (a b) -> p a b", a=2)
    psT = bB[:, 0:256].rearrange("p (a b) -> p a b", a=4)
    nps = bB[0:64, 256:512].rearrange("p (a b) -> p a b", a=2)
    ips = bD[0:64, 0:258].rearrange("p (a b) -> p a b", a=2)
    ups1 = bD[:, 258:387]

    p.dma(cst[:], k.cst, w=['cst'])
    p.dma(convw[:], k.conv_w_s[l].partition_broadcast(64), w=['convw'])
    p.dma(convb[:], k.conv_b_s[l].partition_broadcast(64), w=['convb'])
    p.dma(bg[:], k.b_gates_s[l].partition_broadcast(64), w=['bg'])
    p.dve(lambda e: e.memset(ones[:], 1.0), w=['ones'])
    p.dve(lambda e: e.memset(nones[:], -1.0), w=['nones'])
    p.dve(lambda e: e.memset(mm[:], 0.0), w=['mm'])
    p.dve(lambda e: e.memset(v1[:], 1.0), w=['v1'])
    for h in range(2):
        p.dve(lambda e, h=h: e.memset(CN32[h][:], 0.0), w=['CN32%d' % h])
        p.dve(lambda e, h=h: e.memset(CNbf[h][:], 0.0), w=['CNbf%d' % h])
    TriM = cst[:, 5 + d, :]
    maskb = cst[:, 7 + d, :]
    id64 = cst[:, 9, :]
    C = lambda a: sm[:, a:a + 2]
    C8 = lambda a: sm128[:, a:a + 2]

    for it, (kind, c) in enumerate(chunk_order(d)):
        b = it % 2
        src = k.P_lat if kind == 'lat' else k.P_ctx
        last_c = 63 if kind == 'lat' else 3
        r0 = c * 64
        srow = r0 if kind == 'lat' else L + r0
        MN, PV, NX, GT = 'mn%d' % b, 'pv%d' % b, 'nx%d' % b, 'gt%d' % b
        p.dma(mn[b][:], src[r0:r0 + 64, C_ML:C_ML + 1280], w=[MN])
        if c == 0:
            p.dve(lambda e, b=b: e.memset(pv[b][:], 0.0), w=[PV])
            p.dma(pv[b][1:64, :], src[0:63, C_ML:C_ML + 512], w=[PV])
        else:
            p.dma(pv[b][:], src[r0 - 1:r0 + 63, C_ML:C_ML + 512], w=[PV])
        if c == last_c:
            p.dve(lambda e, b=b: e.memset(nx[b][:], 0.0), w=[NX])
            p.dma(nx[b][0:63, :], src[r0 + 1:r0 + 64, C_ML:C_ML + 512], w=[NX])
        else:
            p.dma(nx[b][:], src[r0 + 1:r0 + 65, C_ML:C_ML + 512], w=[NX])
        p.dma(gt[b][:], src[r0:r0 + 64, C_GAT:C_GAT + 8], w=[GT])
        epi = mypos[(kind, c)] > otpos[(kind, c)]
        p.dve(lambda e, b=b: e.tensor_tensor(ta[:], pv[b][:], convw[:, 0, :], ALU.mult), r=[PV, 'convw'], w=['ta'])
        p.dve(lambda e, b=b: e.tensor_tensor(tb[:], mn[b][:, 0:512], convw[:, 1, :], ALU.mult), r=[MN, 'convw'], w=['tb'])
        p.dve(lambda e: e.tensor_tensor(ta[:], ta[:], tb[:], ALU.add), r=['ta', 'tb'], w=['ta'])
        p.dve(lambda e, b=b: e.tensor_tensor(tb[:], nx[b][:], convw[:, 2, :], ALU.mult), r=[NX, 'convw', 'ta'], w=['tb'])
        p.dve(lambda e: e.tensor_tensor(ta[:], ta[:], tb[:], ALU.add), r=['ta', 'tb'], w=['ta'])
        p.dve(lambda e: e.tensor_tensor(ta[:], ta[:], convb[:], ALU.add), r=['ta', 'convb'], w=['ta'])
        p.act(lambda e: e.activation(sl[:], ta[:], AF.Silu), r=['ta'], w=['sl'])
        p.dve(lambda e: e.tensor_scalar(qs[:], sl[:, 0:256], 128.0 ** -0.5, None, ALU.mult), r=['sl'], w=['qs'])
        p.act(lambda e: e.copy(ks[:], sl[:, 256:512]), r=['sl'], w=['ks'])
        p.act(lambda e, b=b: e.copy(v1[:, :, 0:128], mn[b][:, 512:768].rearrange("p (h v) -> p h v", h=2)), r=[MN], w=['v1'])
        for i in range(4):
            srcT = qs if i < 2 else ks
            p.pe(lambda e, i=i, srcT=srcT: e.transpose(psT[:, i, :], srcT[:, (i % 2) * 128:(i % 2) * 128 + 128], id64), r=['qs', 'ks', 'cst'], w=['bB'])
        p.act(lambda e: e.copy(qkT[:], psT[:]), r=[], w=['qkT', 'bB'])
        p.dve(lambda e, b=b: e.tensor_tensor(g[:], gt[b][:], bg[:], ALU.add), r=[GT, 'bg'], w=['g'])
        ic = g[:, 4 * d:4 * d + 2]
        fp = g[:, 4 * d + 2:4 * d + 4]
        p.act(lambda e, fp=fp: e.activation(C(0), fp, AF.Exp, scale=-1.0), r=['g'], w=['sm'])
        p.act(lambda e: e.activation(C(2), C(0), AF.Ln, bias=1.0), r=['sm'], w=['sm'])
        p.pe(lambda e: e.matmul(Tg[0:64, 0:2], TriM, C(2), start=True, stop=True), r=['sm', 'cst'], w=['bA'])
        p.pe(lambda e: e.matmul(Tg[:, 8:10], nones[:], C(2), start=True, stop=True), r=['sm', 'nones'], w=['bA'])
        p.dve(lambda e: e.tensor_copy(C(4), Tg[0:64, 0:2]), r=[], w=['sm', 'bA'])
        p.dve(lambda e: e.tensor_copy(C8(0), Tg[:, 8:10]), r=[], w=['sm128', 'bA'])
        p.dve(lambda e, ic=ic: e.tensor_tensor(C(6), ic, C(4), ALU.subtract), r=['g', 'sm'], w=['sm'])
        for h in range(2):
            p.dve(lambda e, h=h: e.tensor_scalar(diag[h][:], id64, sm[:, 6 + h:7 + h], None, ALU.mult), r=['cst', 'sm'], w=['diag%d' % h])
            p.pe(lambda e, h=h: e.matmul(Rps[:, h, :], ones[:], diag[h][:], start=True, stop=True), r=['ones', 'diag%d' % h], w=['bA'])
        for h in range(2):
            p.dve(lambda e, h=h: e.scalar_tensor_tensor(Dm[:, h, :], Rps[0:64, h, :], sm[:, 4 + h:5 + h], maskb, ALU.add, ALU.add), r=['sm', 'cst'], w=['D', 'bA'])
        p.dve(lambda e: e.reduce_max(C(8), Dm[:], AX.X), r=['D'], w=['sm'])
        p.dve(lambda e: e.tensor_tensor(C(10), C(4), mm[0:64, :], ALU.add), r=['sm', 'mm'], w=['sm'])
        p.dve(lambda e: e.tensor_tensor(C(12), C(10), C(8), ALU.max), r=['sm'], w=['sm'])
        p.dve(lambda e: e.tensor_scalar(C(14), C(12), -1.0, None, ALU.mult), r=['sm'], w=['sm'])
        p.dve(lambda e: e.tensor_tensor(C(16), C(10), C(12), ALU.subtract), r=['sm'], w=['sm'])
        p.act(lambda e: e.activation(C(18), C(16), AF.Exp), r=['sm'], w=['sm'])
        p.act(lambda e: e.activation(C(20), C(14), AF.Exp), r=['sm'], w=['sm'])
        for h in range(2):
            p.act(lambda e, h=h: e.activation(Ew[:, h, :], Dm[:, h, :], AF.Exp, bias=sm[:, 14 + h:15 + h]), r=['D', 'sm'], w=['Ew'])
            p.pe(lambda e, h=h: e.matmul(Sps[:, h, :], qkT[:, h, :], qkT[:, 2 + h, :], start=True, stop=True), r=['qkT'], w=['bA'])
        p.dve(lambda e: e.memset(C(22), 0.0), r=['sm'], w=['sm'])
        for h in range(2):
            p.dve(lambda e, h=h: e.scalar_tensor_tensor(qkE[:, h, :], Sps[:, h, :], 1.0, Ew[:, h, :], ALU.mult, ALU.mult, accum_out=sm[:, 22 + h:23 + h]),
                  r=['Ew', 'sm'], w=['qkE', 'sm', 'bA'])
        for h in range(2):
            p.pe(lambda e, h=h: e.transpose(psE[:, h, :], qkE[:, h, :], id64), r=['qkE', 'cst'], w=['bA'])
        p.act(lambda e: e.copy(qkET[:], psE[:]), r=[], w=['qkET', 'bA'])
        for h in range(2):
            p.pe(lambda e, h=h: e.matmul(nps[:, h, :], qkET[:, h, :], v1[:, h, 0:128], start=True, stop=True), r=['qkET', 'v1'], w=['bB'])
            p.pe(lambda e, h=h: e.matmul(ips[:, h, :], qkT[:, h, :], CNbf[h][:], start=True, stop=True), r=['qkT', 'CNbf%d' % h], w=['bD'])
        for h in range(2):
            p.dve(lambda e, h=h: e.scalar_tensor_tensor(sm[:, 24 + h:25 + h], ips[:, h, 128:129], sm[:, 18 + h:19 + h], sm[:, 22 + h:23 + h], ALU.mult, ALU.add),
                  r=['sm'], w=['sm', 'bD'])
        p.dve(lambda e: e.tensor_scalar(C(32), C(24), -1.0, None, ALU.mult), r=['sm'], w=['sm'])
        p.dve(lambda e: e.tensor_tensor(C(24), C(24), C(32), ALU.max), r=['sm'], w=['sm'])
        p.dve(lambda e: e.tensor_tensor(C(24), C(24), C(20), ALU.max), r=['sm'], w=['sm'])
        p.dve(lambda e: e.reciprocal(C(26), C(24)), r=['sm'], w=['sm'])
        p.dve(lambda e: e.tensor_tensor(C(28), C(18), C(26), ALU.mult), r=['sm'], w=['sm'])
        for h in range(2):
            p.act(lambda e, h=h: e.activation(ins_[:, h, :], ips[:, h, 0:128], AF.Identity, scale=sm[:, 28 + h:29 + h]), r=['sm'], w=['ins', 'bD'])
            p.dve(lambda e, h=h: e.scalar_tensor_tensor(hout[:, h * 128:(h + 1) * 128], nps[:, h, :], sm[:, 26 + h:27 + h], ins_[:, h, :], ALU.mult, ALU.add),
                  r=['sm', 'ins'], w=['hout', 'bB'])
        p.dve(lambda e: e.reduce_max(C8(2), Rps[:], AX.X), r=[], w=['sm128', 'bA'])
        p.dve(lambda e: e.tensor_tensor(C8(4), C8(2), C8(0), ALU.add), r=['sm128'], w=['sm128'])
        p.dve(lambda e: e.tensor_tensor(C8(6), C8(0), mm[:], ALU.add), r=['sm128', 'mm'], w=['sm128'])
        p.dve(lambda e: e.tensor_tensor(C8(8), C8(6), C8(4), ALU.max), r=['sm128'], w=['sm128'])
        p.dve(lambda e: e.tensor_tensor(C8(10), C8(6), C8(8), ALU.subtract), r=['sm128'], w=['sm128'])
        p.act(lambda e: e.activation(C8(12), C8(10), AF.Exp), r=['sm128'], w=['sm128'])
        p.dve(lambda e: e.tensor_tensor(C8(14), C8(0), C8(8), ALU.subtract), r=['sm128'], w=['sm128'])
        p.dve(lambda e: e.tensor_tensor(C(30), C(6), sm128[0:64, 14:16], ALU.add), r=['sm', 'sm128'], w=['sm'])
        p.act(lambda e: e.activation(C(30), C(30), AF.Exp), r=['sm'], w=['sm'])
        for h in range(2):
            p.dve(lambda e, h=h: e.tensor_scalar(kw[:, h, :], ks[:, h * 128:(h + 1) * 128], sm[:, 30 + h:31 + h], None, ALU.mult), r=['ks', 'sm'], w=['kw'])
        for h in range(2):
            p.pe(lambda e, h=h: e.matmul(ups1, kw[:, h, :], v1[:, h, :], start=True, stop=True), r=['kw', 'v1'], w=['bD'])
            p.dve(lambda e, h=h: e.scalar_tensor_tensor(CN32[h][:], CN32[h][:], sm128[:, 12 + h:13 + h], ups1, ALU.mult, ALU.add),
                  r=['CN32%d' % h, 'sm128'], w=['CN32%d' % h, 'bD'])
            p.act(lambda e, h=h: e.copy(CNbf[h][:], CN32[h][:]), r=['CN32%d' % h], w=['CNbf%d' % h])
        p.dve(lambda e: e.tensor_copy(mm[:], C8(8)), r=['sm128'], w=['mm'])
        if not epi:
            p.dma(k.stash[srow:srow + 64, own0:own0 + 256], hout[:], r=['hout'], w=[('glob', 'st', 'l', srow)])
        else:
            p.dma(hf[b][:], k.stash[srow:srow + 64, oth0:oth0 + 256], r=[('glob', 'st', 'l', srow)], w=['hf%d' % b])
            p.dve(lambda e, b=b: e.tensor_tensor(hout[:], hout[:], hf[b][:], ALU.add), r=['hout', 'hf%d' % b], w=['hout'])
            p.act(lambda e, b=b: e.activation(so[:], mn[b][:, 1024:1280], AF.Sigmoid), r=[MN], w=['so'])
            p.act(lambda e, b=b: e.activation(sz[:], mn[b][:, 768:1024], AF.Silu), r=[MN], w=['sz'])
            p.dve(lambda e: e.tensor_tensor(so[:], so[:], sz[:], ALU.mult), r=['so', 'sz'], w=['so'])
            p.dve(lambda e: e.tensor_tensor(yb[:], hout[:], so[:], ALU.mult), r=['hout', 'so'], w=['yb'])
            ydst, ykey = yrows(k, kind, c)
            p.dma(ydst[:, 256:512], yb[:], r=['yb'], w=[('glob',) + ykey + ('b',)])


def phase_na(k, l):
    for pair in ((0, 1), (2, 3)):
        s = Scope(k.p)
        k.p.interleave([lambda n=n: na_head(k, l, n, s) for n in pair])
        s.end()
    dbg_y(k, l, 512, 1024)


def na_head(k, l, n, s):
    p = k.p
    SC = 128.0 ** -0.5
    idb = s.sb('c_idb', [128, 128], BF16)
    QKT = s.sb('c_QKT', [128, 2, L + LC], BF16)
    V = s.sb('c_V', [128, 34, 128], BF16)
    Vt32 = s.sb('c_Vt32', [128, 5, 128], F32)
    Vt = s.sb('c_Vt', [128, 5, 128], BF16)
    BT = s.sb('c_BT', [128, 5, 576], F32)
    qkv = [s.sb('c_qkv%d' % i, [128, 3, 128], F32) for i in range(2)]
    idf = s.sb('c_idf', [128, 128], F32)
    zt = [s.sb('c_zt%d' % i, [128, 128], F32) for i in range(2)]
    sm = s.sb('c_sm', [128, 832], F32)
    Pm = s.sb('c_Pm', [128, 832], BF16)
    PT = s.sb('c_PT', [128, 7, 128], BF16)
    st = s.sb('c_st', [128, 4], F32)
    sz = s.sb('c_sz', [128, 128], F32)
    yo = s.sb('c_yo', [128, 128], BF16)
    Sa = s.ps('c_Sa', [128, 512], F32)
    Sb = s.ps('c_Sb', [128, 512], F32)
    psP = s.ps('c_psP', [128, 7, 128], BF16)
    bO = s.ps('c_bO', [128, 512], F32)
    O = bO[:, 0:128]
    psT = bO[:, 128:384].rearrange("p (a b) -> p a b", a=2)

    p.dma(idb[:], k.ident, w=['idb'], eng='pool')
    p.dma(idf[:], k.ident, w=['idf'])
    p.dma(BT[:], k.natab[l, n].rearrange("t q c -> q t c"), w=['BT'])
    vcol = C_NA + 1024 + n * 128
    zcol = C_NA + 1536 + n * 128
    p.dma(Vt32[:, 0:4, :], k.P_lat[3520:4032, vcol:vcol + 128].rearrange("(t q) c -> q t c", q=128), w=['Vt32'])
    p.dma(Vt32[0:64, 4, :], k.P_lat[4032:4096, vcol:vcol + 128], w=['Vt32'])
    p.dve(lambda e: e.tensor_copy(Vt[:, 0:4, :], Vt32[:, 0:4, :]), r=['Vt32'], w=['Vt'])
    p.dve(lambda e: e.tensor_copy(Vt[0:64, 4, :], Vt32[0:64, 4, :]), r=['Vt32', 'Vt'], w=['Vt'])
    for i in range(34):
        b = i % 2
        src = k.P_lat[i * 128:(i + 1) * 128, :] if i < 32 else k.P_ctx[(i - 32) * 128:(i - 31) * 128, :]
        v3 = src[:, C_NA:C_NA + 1536].rearrange("q (a hh c) -> q a hh c", a=3, hh=4)[:, :, n, :]
        p.dma(qkv[b][:], v3, w=['qkv%d' % b])
        p.act(lambda e, b=b, i=i: e.copy(V[:, i, :], qkv[b][:, 2, :]), r=['qkv%d' % b], w=['V'])
        for a in range(2):
            p.pe(lambda e, a=a, b=b: e.transpose(psT[:, a, :], qkv[b][:, a, :], idf[:]), r=['qkv%d' % b, 'idf'], w=['O'])
        p.act(lambda e, i=i: e.copy(QKT[:, :, i * 128:(i + 1) * 128], psT[:]), r=[], w=['QKT', 'O'])

    ntile = 34 if l == 0 else 32
    for it, rp in enumerate(range(ntile)):
        b = it % 2
        lat = rp < 32
        q_ap = QKT[:, 0, rp * 128:(rp + 1) * 128]
        if lat:
            ti = {0: 0, 1: 1, 30: 3, 31: 4}.get(rp, 2)
            rs_lo = min(max(2 * rp - 4, 0), 55)
            ks0 = rs_lo * 64
            p.dma(zt[b][:], k.P_lat[rp * 128:(rp + 1) * 128, zcol:zcol + 128], w=['zt%d' % b])
            p.pe(lambda e, q_ap=q_ap, ks0=ks0: e.matmul(Sa[:], q_ap, QKT[:, 1, ks0:ks0 + 512], start=True, stop=True), r=['QKT'], w=['Sa'])
            p.pe(lambda e, q_ap=q_ap, ks0=ks0: e.matmul(Sb[:, 0:64], q_ap, QKT[:, 1, ks0 + 512:ks0 + 576], start=True, stop=True), r=['QKT'], w=['Sb'])
            p.pe(lambda e, q_ap=q_ap: e.matmul(Sb[:, 64:320], q_ap, QKT[:, 1, L:L + LC], start=True, stop=True), r=['QKT'], w=['Sb'])
            p.dve(lambda e, ti=ti: e.scalar_tensor_tensor(sm[:, 0:512], Sa[:], SC, BT[:, ti, 0:512], ALU.mult, ALU.add), r=['BT'], w=['sm', 'Sa'])
            p.dve(lambda e, ti=ti: e.scalar_tensor_tensor(sm[:, 512:576], Sb[:, 0:64], SC, BT[:, ti, 512:576], ALU.mult, ALU.add), r=['BT'], w=['sm', 'Sb'])
            p.act(lambda e: e.activation(sm[:, 576:832], Sb[:, 64:320], AF.Copy, scale=SC), r=[], w=['sm', 'Sb'])
            nk = 832
        else:
            cq = rp - 32
            p.dma(zt[b][:], k.P_ctx[cq * 128:(cq + 1) * 128, zcol:zcol + 128], w=['zt%d' % b])
            p.pe(lambda e, q_ap=q_ap: e.matmul(Sb[:, 64:320], q_ap, QKT[:, 1, L:L + LC], start=True, stop=True), r=['QKT'], w=['Sb'])
            p.act(lambda e: e.activation(sm[:, 0:256], Sb[:, 64:320], AF.Copy, scale=SC), r=[], w=['sm', 'Sb'])
            nk = 256
        p.dve(lambda e, nk=nk: e.reduce_max(st[:, 0:1], sm[:, 0:nk], AX.X), r=['sm'], w=['st'])
        p.dve(lambda e: e.tensor_scalar(st[:, 1:2], st[:, 0:1], -1.0, None, ALU.mult), r=['st'], w=['st'])
        p.dve(lambda e: e.memset(st[:, 2:3], 0.0), r=['st'], w=['st'])
        p.act(lambda e, nk=nk: e.activation(Pm[:, 0:nk], sm[:, 0:nk], AF.Exp, bias=st[:, 1:2], accum_out=st[:, 2:3]), r=['sm', 'st'], w=['Pm', 'st'])
        p.dve(lambda e: e.reciprocal(st[:, 3:4], st[:, 2:3]), r=['st'], w=['st'])
        if lat:
            blocks = [(i, i * 128, 128) for i in range(4)] + [(4, 512, 64), (5, 576, 128), (6, 704, 128)]
        else:
            blocks = [(5, 0, 128), (6, 128, 128)]
        for (bi, c0, w_) in blocks:
            p.pe(lambda e, bi=bi, c0=c0, w_=w_: e.transpose(psP[0:w_, bi, :], Pm[:, c0:c0 + w_], idb[:]), r=['Pm', 'idb'], w=['psP'])
        if lat:
            p.act(lambda e: e.copy(PT[:, 0:4, :], psP[:, 0:4, :]), r=[], w=['PT', 'psP'])
            p.dve(lambda e: e.tensor_copy(PT[0:64, 4, :], psP[0:64, 4, :]), r=[], w=['PT', 'psP'])
        p.dve(lambda e: e.tensor_copy(PT[:, 5:7, :], psP[:, 5:7, :]), r=[], w=['PT', 'psP'])
        if lat:
            aligned = (ks0 % 128 == 0)
            for i in range(4):
                rhs = V[:, ks0 // 128 + i, :] if aligned else Vt[:, i, :]
                p.pe(lambda e, i=i, rhs=rhs: e.matmul(O[:], PT[:, i, :], rhs, start=(i == 0), stop=False), r=['PT', 'V', 'Vt'], w=['O'])
            rhs = V[0:64, ks0 // 128 + 4, :] if aligned else Vt[0:64, 4, :]
            p.pe(lambda e, rhs=rhs: e.matmul(O[:], PT[0:64, 4, :], rhs, start=False, stop=False), r=['PT', 'V', 'Vt'], w=['O'])
        for i in (5, 6):
            p.pe(lambda e, i=i, lat=lat: e.matmul(O[:], PT[:, i, :], V[:, 32 + (i - 5), :], start=((not lat) and i == 5), stop=(i == 6)), r=['PT', 'V'], w=['O'])
        p.act(lambda e, b=b: e.activation(sz[:], zt[b][:], AF.Silu), r=['zt%d' % b], w=['sz'])
        p.dve(lambda e: e.scalar_tensor_tensor(yo[:], O[:], st[:, 3:4], sz[:], ALU.mult, ALU.mult), r=['st', 'sz'], w=['yo', 'O'])
        if lat:
            ydst = k.y_loc[rp % 8][(rp // 8) * 128:(rp // 8) * 128 + 128, 512 + n * 128:512 + (n + 1) * 128]
        else:
            ydst = k.y_loc[8][(rp - 32) * 128:(rp - 31) * 128, 512 + n * 128:512 + (n + 1) * 128]
        p.dma(ydst, yo[:], r=['yo'], w=[('glob', 'yc', rp, n)])


def transpose_tiles(k, s, loader, XT, ntiles, tag):
    p = k.p
    xt = [s.sb('tt_x%s%d' % (tag, i), [128, D], BF16) for i in range(2)]
    idb = s.sb('tt_idb' + tag, [128, 128], BF16)
    pst = [s.ps('tt_ps%s%d' % (tag, i), [128, 8, 128], BF16) for i in range(2)]
    p.dma(idb[:], k.ident, w=['tt_idb'], eng='pool')
    for t in range(ntiles):
        rows = 128 if t < 8 else 64
        b = t % 2
        loader(t, xt[b], 'tt_x%d' % b)
        for g in range(4):
            pb = (t * 4 + g) % 2
            for i in range(8):
                kk = g * 8 + i
                p.pe(lambda e, pb=pb, i=i, kk=kk, rows=rows, b=b: e.transpose(pst[pb][:, i, :rows], xt[b][:rows, kk * 128:(kk + 1) * 128], idb[:rows, :rows]),
                     r=['tt_x%d' % b, 'tt_idb'], w=['tt_ps%d' % pb])
            if g % 2 == 0:
                p.act(lambda e, pb=pb, g=g, t=t, rows=rows: e.copy(XT[:, g * 8:(g + 1) * 8, t * 128:t * 128 + rows], pst[pb][:, :, :rows]), r=[], w=['XT' + tag, 'tt_ps%d' % pb])
            else:
                p.dve(lambda e, pb=pb, g=g, t=t, rows=rows: e.tensor_copy(XT[:, g * 8:(g + 1) * 8, t * 128:t * 128 + rows], pst[pb][:, :, :rows]), r=[], w=['XT' + tag, 'tt_ps%d' % pb])


def phase_b(k, l, last):
    p = k.p
    ntiles = 8 if last else NT
    for t in range(NT):
        allgather(k, k.y_loc[t], k.y_all[t], [], [('y_all', t)])

    s = Scope(p)
    yTs = s.sb('b_yTs', [128, 32, NT * 128], BF16)

    def load_y(t, dst, key):
        rows = 128 if t < 8 else 64
        view = k.y_all[t].rearrange("(r j i) c -> j i r c", r=4, j=4)

        def fn(e, view=view, dst=dst, rows=rows):
            if 'rank' not in p.body_cache:
                p.body_cache['rank'] = e.partition_id() % 4
            rank = p.body_cache['rank']
            return e.dma_start(out=dst[:rows, :].rearrange("q (r c) -> q r c", r=4), in_=view[rank])
        p.add('sp', fn, [('y_all', t)], [key], dma=True)

    transpose_tiles(k, s, load_y, yTs, ntiles, 'y')
    wp = [s.sb('b_wp%d' % i, [128, 16, 512], BF16) for i in range(2)]
    ub = s.sb('b_ub', [128, NT, 512], F32)
    ubf = [s.sb('b_ubf%d' % i, [128, 512], BF16) for i in range(2)]
    Gl = [s.sb('b_Gl%d' % i, [128, 512], BF16) for i in range(3)]
    tmp = s.sb('b_tmp', [128, 512], F32)
    ps = [s.ps('b_pp%d' % i, [128, 512], F32) for i in range(4)]
    wsrc = (k.w_pa, k.w_pb, k.w_pc)
    kmap = ([((hd // 2) * 8 + hd % 2, hd) for hd in range(8)],
            [((hd // 2) * 8 + 2 + hd % 2, hd) for hd in range(8)],
            [((n // 4) * 8 + 4 + n % 4, n) for n in range(16)])
    it = 0
    wi = 0
    for cb in range(8):
        for kb in range(3):
            nk = len(kmap[kb])
            w_ = wi % 2
            wi += 1
            p.dma(wp[w_][:, 0:nk, :], wsrc[kb][l][:, cb * 512:(cb + 1) * 512].rearrange("(kk pp) c -> pp kk c", pp=128), w=['pw%d' % w_], eng='pool')
            for t in range(ntiles):
                rows = 128 if t < 8 else 64
                pi, gi = it % 4, it % 3
                it += 1
                g0 = ((t * 3 + kb) * 8 + cb) * 128
                p.dma(Gl[gi][:rows, :], k.G_scr[g0:g0 + rows, :], w=['Gl%d' % gi])
                for qi, (kk, wr) in enumerate(kmap[kb]):
                    p.pe(lambda e, pi=pi, w_=w_, kk=kk, wr=wr, t=t, rows=rows, qi=qi, nk=nk: e.matmul(ps[pi][:rows, :], yTs[:, kk, t * 128:t * 128 + rows], wp[w_][:, wr, :], start=(qi == 0), stop=(qi == nk - 1)),
                         r=['XTy', 'pw%d' % w_], w=['pps%d' % pi])
                if kb == 0:
                    p.dve(lambda e, pi=pi, gi=gi, t=t, rows=rows: e.tensor_tensor(ub[:rows, t, :], ps[pi][:rows, :], Gl[gi][:rows, :], ALU.mult), r=['Gl%d' % gi], w=[('ub', t), 'pps%d' % pi])
                else:
                    p.dve(lambda e, pi=pi, gi=gi, rows=rows: e.tensor_tensor(tmp[:rows, :], ps[pi][:rows, :], Gl[gi][:rows, :], ALU.mult), r=['Gl%d' % gi], w=['btmp', 'pps%d' % pi])
                    if kb == 1:
                        p.dve(lambda e, t=t, rows=rows: e.tensor_tensor(ub[:rows, t, :], ub[:rows, t, :], tmp[:rows, :], ALU.add), r=['btmp', ('ub', t)], w=[('ub', t)])
                    else:
                        ui = t % 2
                        p.dve(lambda e, t=t, rows=rows, ui=ui: e.tensor_tensor(ubf[ui][:rows, :], ub[:rows, t, :], tmp[:rows, :], ALU.add), r=['btmp', ('ub', t)], w=['ubf%d' % ui])
                        p.dma(k.u_scr[t * 128:t * 128 + rows, cb * 512:(cb + 1) * 512], ubf[ui][:rows, :], r=['ubf%d' % ui], w=[('u', t, cb)])
    s.end()

    s = Scope(p)
    uTs = s.sb('b_uTs', [128, 32, NT * 128], BF16)

    def load_u(t, dst, key):
        rows = 128 if t < 8 else 64
        p.dma(dst[:rows, :], k.u_scr[t * 128:t * 128 + rows, :], w=[key])

    transpose_tiles(k, s, load_u, uTs, ntiles, 'u')
    wo = [s.sb('b_wo%d' % i, [128, 32, 512], BF16) for i in range(2)]
    gate = [s.sb('b_gate%d' % m, [128, D], F32) for m in range(2)]
    xin = [s.sb('b_xin%d' % i, [128, 512], F32) for i in range(3)]
    xo = [s.sb('b_xo%d' % i, [128, 512], F32) for i in range(3)]
    ps = [s.ps('b_po%d' % i, [128, 512], F32) for i in range(4)]
    for m in range(2):
        p.dma(gate[m][:].rearrange("q (r c) -> q r c", r=4), modvec_bc(k, l, m, 2), w=['gate%d' % m])
    xsrc = k.x_tok if l == 0 else k.x_loc
    it = 0
    for cb in range(8):
        w_ = cb % 2
        p.dma(wo[w_][:], k.w_out[l][:, cb * 512:(cb + 1) * 512].rearrange("(kk pp) c -> pp kk c", pp=128), w=['ow%d' % w_], eng='pool')
        for t in range(ntiles):
            rows = 128 if t < 8 else 64
            m = 0 if t < 8 else 1
            pi, xi = it % 4, it % 3
            it += 1
            p.dma(xin[xi][:rows, :], xsrc[t * 128:t * 128 + rows, cb * 512:(cb + 1) * 512], r=[('x', t, cb)], w=['xin%d' % xi])
            for kk in range(32):
                p.pe(lambda e, pi=pi, w_=w_, kk=kk, t=t, rows=rows: e.matmul(ps[pi][:rows, :], uTs[:, kk, t * 128:t * 128 + rows], wo[w_][:, kk, :], start=(kk == 0), stop=(kk == 31)),
                     r=['XTu', 'ow%d' % w_], w=['ops%d' % pi])
            p.dve(lambda e, pi=pi, xi=xi, rows=rows, m=m, cb=cb: e.tensor_tensor(xo[xi][:rows, :], ps[pi][:rows, :], gate[m][:rows, cb * 512:(cb + 1) * 512], ALU.mult),
                  r=['gate%d' % m], w=['xo%d' % xi, 'ops%d' % pi])
            p.dve(lambda e, xi=xi, rows=rows: e.tensor_tensor(xo[xi][:rows, :], xo[xi][:rows, :], xin[xi][:rows, :], ALU.add), r=['xo%d' % xi, 'xin%d' % xi], w=['xo%d' % xi])
            p.dma(k.x_loc[t * 128:t * 128 + rows, cb * 512:(cb + 1) * 512], xo[xi][:rows, :], r=['xo%d' % xi], w=[('x', t, cb)])
    s.end()


def phase_final(k):
    p = k.p
    s = Scope(p)
    fg = s.sb('f_g', [128, D], F32)
    xt = [s.sb('f_x%d' % i, [128, D], F32) for i in range(2)]
    xo = [s.sb('f_o%d' % i, [128, D], F32) for i in range(2)]
    junk = s.sb('f_junk', [128, D], BF16)
    st = s.sb('f_st', [128, 4], F32)
    p.dma(fg[:], k.final_g.partition_broadcast(128).rearrange("q a d -> q (a d)"), w=['fg'])
    for t in range(8):
        b = t % 2
        p.dma(xt[b][:], k.x_loc[t * 128:(t + 1) * 128, :], w=['fx%d' % b])
        p.dve(lambda e: e.memset(st[:], 0.0), w=['fst'])
        p.act(lambda e, b=b: e.activation(junk[:], xt[b][:], AF.Square, accum_out=st[:, 0:1]), r=['fx%d' % b, 'fst'], w=['fjunk', 'fst'])
        p.dve(lambda e: e.tensor_scalar(st[:, 1:2], st[:, 0:1], 1.0 / D, EPS, ALU.mult, ALU.add), r=['fst'], w=['fst'])
        p.act(lambda e: e.activation(st[:, 1:2], st[:, 1:2], AF.Sqrt), r=['fst'], w=['fst'])
        p.dve(lambda e: e.reciprocal(st[:, 2:3], st[:, 1:2]), r=['fst'], w=['fst'])
        p.dve(lambda e, b=b: e.scalar_tensor_tensor(xo[b][:], xt[b][:], st[:, 2:3], fg[:], ALU.mult, ALU.mult), r=['fx%d' % b, 'fst', 'fg'], w=['fo%d' % b])
        p.dma(k.out[t * 128:(t + 1) * 128, :], xo[b][:], r=['fo%d' % b], w=[('out', t)])
    s.end()


def kernel(**inputs):
    inp = {kk: np.asarray(v) for kk, v in inputs.items()}
    maps = prep(inp)
    nc = build_nc()
    maps = [{kk: m[kk] for kk in nc.declared_inputs} for m in maps]
    res = run_bass_kernel_spmd(nc, maps, core_ids=list(range(8)))
    out = np.zeros((2, L, D), np.float32)
    for i in range(8):
        b, j = i // 4, i % 4
        out[b, j * 1024:(j + 1) * 1024] = res.results[i]['out']
    return out
```
